# Optimizing a Trainium2 kernel written in Bass

```python
import numpy as np
import jax
import jax.numpy as jnp
from jax import lax

D_MODEL = 1024
BATCH = 16
SEQ = 2048
DEPTH = 4

GRID_W = 64
CTX_LEN = 256
EPS = 1e-6
N_BRANCH = 3

M_HEADS = 4
M_DH = 128
M_WIDTH = M_HEADS * M_DH
M_CHUNK = 128
M_CONV = 3
M_FGATE_BIAS_LO = 3.0
M_FGATE_BIAS_HI = 6.0

A_HEADS = 8
A_NOPE = 64
A_ROPE = 32
A_VDIM = 64
A_QRANK = 384
A_KVRANK = 256
A_WIDTH = A_HEADS * A_VDIM
ATT_SCALE = (A_NOPE + A_ROPE) ** -0.5
Q_BLOCK = 128
ROPE_THETA = 10000.0

G_GROUPS = 4
G_CHUNK = 128
G_WIDTH = 512
G_DG = G_WIDTH // G_GROUPS

N_GROUPS = 4
EXP_PER_GROUP = 4
N_EXPERTS = N_GROUPS * EXP_PER_GROUP
TOP_K = 2
D_EXPERT = 512

IN_SPLIT = (M_WIDTH, M_WIDTH, M_WIDTH, M_WIDTH, 4 * M_HEADS, A_QRANK, A_KVRANK, A_ROPE,
            G_WIDTH, G_WIDTH, D_MODEL, D_MODEL, D_MODEL)
D_IN = sum(IN_SPLIT)

kernel_name = 'hybrid_mlstm_mla_gmlp_hmoe_diffusion_block'


def rmsnorm(x, g):
    xf = x.astype(jnp.float32)
    y = xf * lax.rsqrt(jnp.mean(xf * xf, axis=-1, keepdims=True) + EPS)
    return (y * g.astype(jnp.float32)).astype(x.dtype)


def modulate(h, shift, scale):
    return h * (1.0 + scale) + shift


def axial_rope_tables(rows):
    half = A_ROPE // 2
    r = jnp.repeat(jnp.arange(rows, dtype=jnp.float32), GRID_W)
    col = jnp.tile(jnp.arange(GRID_W, dtype=jnp.float32), rows)
    inv = ROPE_THETA ** (-jnp.arange(0, half, 2, dtype=jnp.float32) / half)
    ang = jnp.concatenate([r[:, None] * inv, col[:, None] * inv], axis=-1)
    return jnp.cos(ang), jnp.sin(ang)


def apply_rope(x, cos, sin):
    x1, x2 = jnp.split(x, 2, axis=-1)
    return jnp.concatenate([x1 * cos - x2 * sin, x2 * cos + x1 * sin], axis=-1).astype(x.dtype)


def dwconv_centered(x, w):
    pad = M_CONV // 2
    return lax.conv_general_dilated(
        x, w[:, None, :].astype(x.dtype), window_strides=(1,), padding=[(pad, pad)],
        dimension_numbers=('NWC', 'WIO', 'NWC'), feature_group_count=x.shape[-1])


def mlstm_scan(q, k, v, ig, fg, state):
    B, H, L, dh = q.shape
    nc = L // M_CHUNK
    to_chunks = lambda a: jnp.moveaxis(a.reshape(B, H, nc, M_CHUNK, *a.shape[3:]), 2, 0)
    prefix = jnp.tril(jnp.ones((M_CHUNK, M_CHUNK), dtype=bool))

    def step(carry, inp):
        C, n, m = carry
        qc, kc, vc, ic, lfc = inp
        b = jnp.cumsum(lfc, axis=-1)
        dmat = jnp.where(prefix, b[..., :, None] - b[..., None, :] + ic[..., None, :], -jnp.inf)
        inter = b + m[..., None]
        mt = jnp.maximum(inter, jnp.max(dmat, axis=-1))
        w_intra = jnp.exp(dmat - mt[..., None])
        w_inter = jnp.exp(inter - mt)
        qk = jnp.einsum('bhtd,bhsd->bhts', qc, kc) * w_intra
        num = jnp.einsum('bhts,bhse->bhte', qk, vc) + w_inter[..., None] * jnp.einsum('bhed,bhtd->bhte', C, qc)
        den = jnp.sum(qk, axis=-1) + w_inter * jnp.einsum('bhd,bhtd->bht', n, qc)
        h = num / jnp.maximum(jnp.abs(den), jnp.exp(-mt))[..., None]
        b_end = b[..., -1:]
        d_end = b_end - b + ic
        m_new = jnp.maximum(b_end[..., 0] + m, jnp.max(d_end, axis=-1))
        w_s = jnp.exp(d_end - m_new[..., None])
        w_c = jnp.exp(b_end[..., 0] + m - m_new)
        C_new = w_c[..., None, None] * C + jnp.einsum('bhs,bhse,bhsd->bhed', w_s, vc, kc)
        n_new = w_c[..., None] * n + jnp.einsum('bhs,bhsd->bhd', w_s, kc)
        return (C_new, n_new, m_new), h

    xs = (to_chunks(q), to_chunks(k), to_chunks(v), to_chunks(ig.astype(jnp.float32)),
          to_chunks(jax.nn.log_sigmoid(fg.astype(jnp.float32))))
    state, hs = lax.scan(step, state, xs)
    h = jnp.moveaxis(hs, 0, 2).reshape(B, H, L, dh)
    return h.astype(q.dtype), state


def mla_attend(q_nope, q_rope, k_nope, k_rope, v):
    s = jnp.einsum('bqhd,bkhd->bhqk', q_nope, k_nope) + jnp.einsum('bqhd,bkd->bhqk', q_rope, k_rope)
    p = jax.nn.softmax(s.astype(jnp.float32) * ATT_SCALE, axis=-1).astype(v.dtype)
    return jnp.einsum('bhqk,bkhd->bqhd', p, v)


def latent_attention_blocks(q_nope, q_rope, k_nope, k_rope, v):
    B, T, H, _ = q_nope.shape
    nb = T // Q_BLOCK
    blocks = lambda a: jnp.moveaxis(a.reshape(B, nb, Q_BLOCK, *a.shape[2:]), 1, 0)
    out = lax.map(lambda qs: mla_attend(qs[0], qs[1], k_nope, k_rope, v), (blocks(q_nope), blocks(q_rope)))
    return jnp.moveaxis(out, 0, 1).reshape(B, T, H, A_VDIM)


def chunk_spatial_gating(v, ws, bs):
    B, L, _ = v.shape
    vr = v.reshape(B, L // G_CHUNK, G_CHUNK, G_GROUPS, G_DG)
    out = jnp.einsum('gts,bnsgc->bntgc', ws, vr) + bs.T[:, :, None]
    return out.reshape(B, L, G_WIDTH)


def token_mixer(hc, hl, cos, sin, w_in, m_conv, m_gate_b, m_norm, a_qnorm, a_wuq, a_kvnorm, a_wukv,
                g_ws, g_bs, g_vnorm, w_pa, w_pb, w_pc, w_out, ctx_out):
    B, Lc, _ = hc.shape
    L = Lc + hl.shape[1]
    sl = slice(None) if ctx_out else slice(Lc, None)
    z = jnp.concatenate([hc, hl], axis=1) @ w_in
    split_at = np.cumsum(IN_SPLIT)[:-1].tolist()
    mq, mk, mv, mo, mg, aq, akv, akr, gu, gv, br_a, br_b, br_c = jnp.split(z, split_at, axis=-1)

    qk = jnp.concatenate([mq, mk], axis=-1)
    qk = jax.nn.silu(jnp.concatenate([dwconv_centered(qk[:, :Lc], m_conv), dwconv_centered(qk[:, Lc:], m_conv)], axis=1))
    heads = lambda a: a.reshape(B, L, M_HEADS, M_DH).transpose(0, 2, 1, 3)
    q = heads(qk[..., :M_WIDTH]) * (M_DH ** -0.5)
    k = heads(qk[..., M_WIDTH:])
    v = heads(mv)
    g = (mg.reshape(B, L, 4, M_HEADS) + m_gate_b.reshape(4, M_HEADS)).transpose(2, 0, 3, 1)
    st0 = (jnp.zeros((B, M_HEADS, M_DH, M_DH), jnp.float32), jnp.zeros((B, M_HEADS, M_DH), jnp.float32),
           jnp.zeros((B, M_HEADS), jnp.float32))

    def direction(ig, fg, flip):
        tf = (lambda a: jnp.flip(a, axis=2)) if flip else (lambda a: a)
        pc = lambda a: tf(a[:, :, :Lc])
        pl = lambda a: tf(a[:, :, Lc:])
        h_c, st = mlstm_scan(pc(q), pc(k), pc(v), pc(ig), pc(fg), st0)
        h_l, _ = mlstm_scan(pl(q), pl(k), pl(v), pl(ig), pl(fg), st)
        return tf(h_c), tf(h_l)

    hc_f, hl_f = direction(g[0], g[1], False)
    hc_b, hl_b = direction(g[2], g[3], True)
    h_m = jnp.concatenate([hc_f + hc_b, hl_f + hl_b], axis=2) if ctx_out else hl_f + hl_b
    y_a = rmsnorm(h_m.transpose(0, 2, 1, 3), m_norm.reshape(M_HEADS, M_DH)).reshape(B, -1, M_WIDTH)
    y_a = y_a * jax.nn.sigmoid(mo[:, sl])

    qa = (rmsnorm(aq, a_qnorm) @ a_wuq).reshape(B, L, A_HEADS, A_NOPE + A_ROPE)
    kva = (rmsnorm(akv, a_kvnorm) @ a_wukv).reshape(B, L, A_HEADS, A_NOPE + A_VDIM)
    q_nope, q_rope = qa[..., :A_NOPE], qa[..., A_NOPE:]
    k_nope, v_att = kva[..., :A_NOPE], kva[..., A_NOPE:]
    q_rope_l = apply_rope(q_rope[:, Lc:], cos[:, None, :], sin[:, None, :])
    k_rope = jnp.concatenate([akr[:, :Lc], apply_rope(akr[:, Lc:], cos, sin)], axis=1)
    o_att = latent_attention_blocks(q_nope[:, Lc:], q_rope_l, k_nope, k_rope, v_att)
    if ctx_out:
        o_c = mla_attend(q_nope[:, :Lc], q_rope[:, :Lc], k_nope[:, :Lc], k_rope[:, :Lc], v_att[:, :Lc])
        o_att = jnp.concatenate([o_c, o_att], axis=1)
    y_b = o_att.reshape(B, -1, A_WIDTH)

    gu = jax.nn.gelu(gu, approximate=False)
    gv = rmsnorm(jax.nn.gelu(gv, approximate=False).reshape(B, L, G_GROUPS, G_DG),
                 g_vnorm.reshape(G_GROUPS, G_DG)).reshape(B, L, G_WIDTH)
    s_gate = chunk_spatial_gating(gv[:, Lc:], g_ws, g_bs)
    if ctx_out:
        s_gate = jnp.concatenate([chunk_spatial_gating(gv[:, :Lc], g_ws, g_bs), s_gate], axis=1)
    y_c = gu[:, sl] * s_gate

    y = (jax.nn.sigmoid(br_a[:, sl]) * (y_a @ w_pa) + jax.nn.sigmoid(br_b[:, sl]) * (y_b @ w_pb)
         + jax.nn.sigmoid(br_c[:, sl]) * (y_c @ w_pc))
    out = y @ w_out
    if ctx_out:
        return out[:, :Lc], out[:, Lc:]
    return None, out


def hier_moe(h, r_group, r_group_b, r_expert, r_expert_b, w1, w3, w2):
    B, L, _ = h.shape
    g_logits = (h @ r_group).astype(jnp.float32) + r_group_b.astype(jnp.float32)
    g_sel = jnp.argmax(g_logits, axis=-1)
    g_prob = jnp.max(jax.nn.softmax(g_logits, axis=-1), axis=-1, keepdims=True)
    e_logits = ((h @ r_expert).astype(jnp.float32) + r_expert_b.astype(jnp.float32)).reshape(
        B, L, N_GROUPS, EXP_PER_GROUP)
    e_in_group = jnp.einsum('blg,blge->ble', jax.nn.one_hot(g_sel, N_GROUPS, dtype=jnp.float32), e_logits)
    top_v, top_i = lax.top_k(e_in_group, TOP_K)
    top_w = jax.nn.softmax(top_v, axis=-1) * g_prob
    expert_id = g_sel[..., None] * EXP_PER_GROUP + top_i
    combine = jnp.einsum('blk,blke->ble', top_w,
                         jax.nn.one_hot(expert_id, N_EXPERTS, dtype=jnp.float32)).astype(h.dtype)
    out = jnp.zeros_like(h)
    for e in range(N_EXPERTS):
        hidden = jax.nn.silu(h @ w1[e]) * (h @ w3[e])
        out = out + combine[..., e:e + 1] * (hidden @ w2[e])
    return out


def setup_inputs(seed: int = 0) -> dict:
    key = jax.random.key(seed)
    keys = jax.random.split(key, 48)
    count = [0]

    def nrm(shape, scale):
        k = keys[count[0]]
        count[0] += 1
        return jax.random.normal(k, shape, jnp.float32) * scale

    def gain(shape):
        return 1.0 + nrm(shape, 0.02)

    Ld = DEPTH
    D = D_MODEL
    f_bias = jnp.linspace(M_FGATE_BIAS_LO, M_FGATE_BIAS_HI, M_HEADS, dtype=jnp.float32)
    m_gate_b = jnp.stack([nrm((Ld, M_HEADS), 0.1), f_bias + nrm((Ld, M_HEADS), 0.1),
                          nrm((Ld, M_HEADS), 0.1), f_bias + nrm((Ld, M_HEADS), 0.1)], axis=1).reshape(Ld, 4 * M_HEADS)
    return {
        'x': nrm((BATCH, SEQ, D), 1.0),
        'c': nrm((BATCH, D), 1.0),
        'ctx': nrm((BATCH, CTX_LEN, D), 1.0),
        'c_ctx': nrm((D,), 1.0),
        'w_ada': nrm((Ld, D, 6 * D), 0.5 * D ** -0.5),
        'b_ada': nrm((Ld, 6 * D), 0.02),
        'norm1': gain((Ld, D)),
        'norm2': gain((Ld, D)),
        'final_norm': gain((D,)),
        'w_in': nrm((Ld, D, D_IN), D ** -0.5),
        'm_conv': nrm((Ld, M_CONV, 2 * M_WIDTH), M_CONV ** -0.5),
        'm_gate_b': m_gate_b,
        'm_norm': gain((Ld, M_WIDTH)),
        'a_qnorm': gain((Ld, A_QRANK)),
        'a_wuq': nrm((Ld, A_QRANK, A_HEADS * (A_NOPE + A_ROPE)), A_QRANK ** -0.5),
        'a_kvnorm': gain((Ld, A_KVRANK)),
        'a_wukv': nrm((Ld, A_KVRANK, A_HEADS * (A_NOPE + A_VDIM)), A_KVRANK ** -0.5),
        'g_ws': nrm((Ld, G_GROUPS, G_CHUNK, G_CHUNK), G_CHUNK ** -0.5),
        'g_bs': gain((Ld, G_GROUPS, G_CHUNK)),
        'g_vnorm': gain((Ld, G_WIDTH)),
        'w_pa': nrm((Ld, M_WIDTH, D), M_WIDTH ** -0.5),
        'w_pb': nrm((Ld, A_WIDTH, D), A_WIDTH ** -0.5),
        'w_pc': nrm((Ld, G_WIDTH, D), G_WIDTH ** -0.5),
        'w_out': nrm((Ld, D, D), D ** -0.5),
        'r_group': nrm((Ld, D, N_GROUPS), D ** -0.5),
        'r_group_b': nrm((Ld, N_GROUPS), 0.01),
        'r_expert': nrm((Ld, D, N_EXPERTS), D ** -0.5),
        'r_expert_b': nrm((Ld, N_EXPERTS), 0.01),
        'e_w1': nrm((Ld, N_EXPERTS, D, D_EXPERT), D ** -0.5),
        'e_w3': nrm((Ld, N_EXPERTS, D, D_EXPERT), D ** -0.5),
        'e_w2': nrm((Ld, N_EXPERTS, D_EXPERT, D), D_EXPERT ** -0.5),
    }


def reference(x, c, ctx, c_ctx, w_ada, b_ada, norm1, norm2, final_norm, w_in, m_conv, m_gate_b, m_norm,
              a_qnorm, a_wuq, a_kvnorm, a_wukv, g_ws, g_bs, g_vnorm, w_pa, w_pb, w_pc, w_out,
              r_group, r_group_b, r_expert, r_expert_b, e_w1, e_w3, e_w2):
    T = x.shape[1]
    ROWS = T // GRID_W
    cos, sin = axial_rope_tables(ROWS)
    Lc = ctx.shape[1]
    silu_c = jax.nn.silu(c)[:, None, :]
    silu_cc = jax.nn.silu(c_ctx)
    xl, xc = x, ctx
    for l in range(DEPTH):
        last = l == DEPTH - 1
        sh1, sc1, g1, sh2, sc2, g2 = jnp.split(silu_c @ w_ada[l] + b_ada[l], 6, axis=-1)
        sh1c, sc1c, g1c, sh2c, sc2c, g2c = jnp.split(silu_cc @ w_ada[l] + b_ada[l], 6, axis=-1)
        hl = modulate(rmsnorm(xl, norm1[l]), sh1, sc1)
        hc = modulate(rmsnorm(xc, norm1[l]), sh1c, sc1c)
        yc, yl = token_mixer(hc, hl, cos, sin, w_in[l], m_conv[l], m_gate_b[l], m_norm[l],
                             a_qnorm[l], a_wuq[l], a_kvnorm[l], a_wukv[l], g_ws[l], g_bs[l], g_vnorm[l],
                             w_pa[l], w_pb[l], w_pc[l], w_out[l], not last)
        xl = xl + g1 * yl
        if last:
            h2 = modulate(rmsnorm(xl, norm2[l]), sh2, sc2)
            xl = xl + g2 * hier_moe(h2, r_group[l], r_group_b[l], r_expert[l], r_expert_b[l],
                                    e_w1[l], e_w3[l], e_w2[l])
        else:
            xc = xc + g1c * yc
            h2 = jnp.concatenate([modulate(rmsnorm(xc, norm2[l]), sh2c, sc2c),
                                  modulate(rmsnorm(xl, norm2[l]), sh2, sc2)], axis=1)
            f = hier_moe(h2, r_group[l], r_group_b[l], r_expert[l], r_expert_b[l], e_w1[l], e_w3[l], e_w2[l])
            xc = xc + g2c * f[:, :Lc]
            xl = xl + g2 * f[:, Lc:]
    return rmsnorm(xl, final_norm)
```

```python
import numpy as np
from contextlib import ExitStack
import concourse.bass as bass
import concourse.mybir as mybir
from concourse.bass_utils import run_bass_kernel_spmd

F32 = mybir.dt.float32
BF16 = mybir.dt.bfloat16
AF = mybir.ActivationFunctionType
ALU = mybir.AluOpType
AX = mybir.AxisListType

NCORES = 8
NB = 2
LC = 256
T = 2048
L = LC + T
S = NB * L
D = 1024
DEPTH = 4
EPS = 1e-6
NFM = 47
NTMC = 1040
NV = 112
NR = 16 + 512 + 20 + 512
ATT_SCALE = 96 ** -0.5
QS = 128 ** -0.5
NDS = 12
SAME_ENGINE_SYNC = True

C_MQ, C_MK, C_MO, C_AQ, C_AKV, C_KRA, C_KRB, C_GU, C_BRA, C_BRB, C_BRC = 0, 4, 8, 12, 15, 17, 18, 19, 23, 31, 39
V_N1, V_N2, V_BADA, V_CONV, V_MNORM, V_QN, V_KVN, V_FN = 0, 8, 16, 64, 88, 92, 95, 97
R_GB, R_VN, R_RB, R_BS = 0, 16, 528, 548


def token_tiles():
    tl = []
    for b in range(NB):
        tl.append((b * L, LC, 2, b))
        for i in range(T // 512):
            tl.append((b * L + LC + i * 512, 512, b, b))
    return tl


class MK:
    def __init__(self, nc, st):
        self.nc = nc
        self.E = {"pe": nc.tensor, "act": nc.scalar, "dve": nc.vector, "pool": nc.gpsimd, "sp": nc.sync}
        self.sem = {e: st.enter_context(nc.semaphore("s_" + e)) for e in self.E}
        self.cnt = {e: 0 for e in self.E}
        self.dq = ("sp", "act", "pool")
        self.dsem = {q: [st.enter_context(nc.semaphore(f"d_{q}{i}")) for i in range(NDS)] for q in self.dq}
        self.dcnt = {q: [0] * NDS for q in self.dq}
        self.dnext = {q: 0 for q in self.dq}
        self.waited = {}
        self.W = {}
        self.R = {}
        self.pending = {e: {} for e in self.E}
        self.ninst = 0

    def _semh(self, sk):
        return self.sem[sk[1]] if sk[0] == "e" else self.dsem[sk[1]][sk[2]]

    def _wait(self, eng, sk, val):
        if val <= 0:
            return
        if sk == ("e", eng) and (eng == "pe" or not SAME_ENGINE_SYNC):
            return
        k = (eng, sk)
        if self.waited.get(k, 0) >= val:
            return
        self.waited[k] = val
        self.E[eng].wait_ge(self._semh(sk), val)
        self.ninst += 1

    def _pre(self, eng, r, w):
        pend = self.pending[eng]
        if pend:
            for sk, v in pend.items():
                self._wait(eng, sk, v)
            self.pending[eng] = {}
        for k in r:
            for sk, v in self.W.get(k, {}).items():
                self._wait(eng, sk, v)
        for k in w:
            for sk, v in self.W.get(k, {}).items():
                self._wait(eng, sk, v)
            for sk, v in self.R.get(k, {}).items():
                self._wait(eng, sk, v)

    def _post(self, sk, val, r, w):
        for k in r:
            d = self.R.setdefault(k, {})
            d[sk] = max(d.get(sk, 0), val)
        for k in w:
            self.W[k] = {sk: val}
            self.R[k] = {}

    def op(self, eng, fn, r=(), w=()):
        self._pre(eng, r, w)
        ins = fn(self.E[eng])
        self.cnt[eng] += 1
        ins.then_inc(self.sem[eng], 1)
        self.ninst += 1
        self._post(("e", eng), self.cnt[eng], r, w)

    def dma(self, q, out, in_, r=(), w=(), **kw):
        i = self.dnext[q]
        self.dnext[q] = (i + 1) % NDS
        sk = ("d", q, i)
        self._wait(q, sk, self.dcnt[q][i])
        self._pre(q, r, w)
        ins = self.E[q].dma_start(out=out, in_=in_, **kw)
        self.dcnt[q][i] += 16
        ins.then_inc(self.dsem[q][i], 16)
        self.ninst += 1
        self._post(sk, self.dcnt[q][i], r, w)

    def barrier(self):
        snap = {}
        for e in self.E:
            snap[("e", e)] = self.cnt[e]
        for q in self.dq:
            for i in range(NDS):
                snap[("d", q, i)] = self.dcnt[q][i]
        for e in self.E:
            self.pending[e] = dict(snap)
        self.W = {}
        self.R = {}

    def finish(self):
        self.barrier()
        for e in self.E:
            for sk, v in self.pending[e].items():
                if sk == ("e", e):
                    continue
                k = (e, sk)
                if self.waited.get(k, 0) >= v or v <= 0:
                    continue
                self.waited[k] = v
                self.E[e].wait_ge(self._semh(sk), v)
            self.pending[e] = {}

    def mm(self, out, lhsT, rhs, start, stop, r=(), w=()):
        self.op("pe", lambda e: e.matmul(out, lhsT=lhsT, rhs=rhs, start=start, stop=stop), r=r, w=w)

    def tr(self, out, in_, ident, r=(), w=()):
        self.op("pe", lambda e: e.transpose(out, in_, ident), r=r, w=w)

    def act(self, out, in_, func, r=(), w=(), bias=None, scale=None, eng="act"):
        kw = {}
        if bias is not None:
            kw["bias"] = bias
        if scale is not None:
            kw["scale"] = scale
        self.op(eng, lambda e: e.activation(out=out, in_=in_, func=func, **kw), r=r, w=w)

    def tt(self, eng, out, in0, in1, op, r=(), w=()):
        self.op(eng, lambda e: e.tensor_tensor(out=out, in0=in0, in1=in1, op=op), r=r, w=w)

    def ts(self, eng, out, in0, s1, op0, s2=None, op1=None, r=(), w=()):
        kw = dict(out=out, in0=in0, scalar1=s1, scalar2=s2, op0=op0)
        if op1 is not None:
            kw["op1"] = op1
        self.op(eng, lambda e: e.tensor_scalar(**kw), r=r, w=w)

    def stt(self, out, in0, scalar, in1, op0, op1, r=(), w=(), eng="dve"):
        self.op(eng, lambda e: e.scalar_tensor_tensor(out=out, in0=in0, scalar=scalar, in1=in1, op0=op0, op1=op1),
                r=r, w=w)

    def cp(self, eng, out, in_, r=(), w=()):
        if eng == "act":
            self.op(eng, lambda e: e.copy(out=out, in_=in_), r=r, w=w)
        else:
            self.op(eng, lambda e: e.tensor_copy(out=out, in_=in_), r=r, w=w)

    def memset(self, eng, ap, val, w=()):
        self.op(eng, lambda e: e.memset(ap, val), w=w)

    def recip(self, out, in_, r=(), w=()):
        self.op("dve", lambda e: e.reciprocal(out=out, in_=in_), r=r, w=w)


class Stage:
    def __init__(self, mk, name):
        self.mk = mk
        self.name = name
        self.es = ExitStack()

    def sb(self, nm, shape, dt):
        return self.es.enter_context(self.mk.nc.sbuf_tensor(f"{self.name}_{nm}", list(shape), dt))

    def ps(self, nm, shape, dt):
        return self.es.enter_context(self.mk.nc.psum_tensor(f"{self.name}_{nm}", list(shape), dt))

    def close(self):
        self.mk.barrier()
        self.es.close()


def build(nlayers=DEPTH, dbg=(), stop=None):
    nc = bass.Bass("TRN2", target_bir_lowering=False)
    dbg = set(dbg)

    def din(name, shape, dt=F32):
        return nc.dram_tensor(name, list(shape), dt, kind="ExternalInput").ap()

    def dscr(name, shape, dt):
        kind = "ExternalOutput" if name in dbg else "Internal"
        return nc.dram_tensor(name, list(shape), dt, kind=kind).ap()

    xin = din("xin", [S, D])
    cv = din("cv", [128, 8, 4])
    vecs = din("vecs", [DEPTH, 128, NV])
    rows = din("rows", [DEPTH, 1, NR])
    w_ada = din("w_ada", [DEPTH, D, 6 * D])
    w_in_r = din("w_in_r", [DEPTH, D, NFM * 128 + NTMC])
    wuq_r = din("wuq_r", [DEPTH, 384, 1536])
    wukv_r = din("wukv_r", [DEPTH, 256, 1024])
    gws_r = din("gws_r", [DEPTH, 128, 512])
    w_pa = din("w_pa", [DEPTH, 512, D])
    w_pb = din("w_pb", [DEPTH, 512, D])
    w_pc = din("w_pc", [DEPTH, 512, D])
    w_out = din("w_out", [DEPTH, D, D])
    rw = din("rw", [DEPTH, D, 20])
    e_w1 = din("e_w1", [DEPTH, 16, D, 512])
    e_w3 = din("e_w3", [DEPTH, 16, D, 512])
    e_w2 = din("e_w2", [DEPTH, 16, 512, D])
    c_ident = din("c_ident", [128, 128])
    c_trif = din("c_trif", [128, 128])
    c_trib = din("c_trib", [128, 128])
    c_maskf = din("c_maskf", [128, 128])
    c_maskb = din("c_maskb", [128, 128])
    c_cc = din("c_cc", [128, L])
    c_ss = din("c_ss", [128, L])
    out = nc.dram_tensor("out", [NB, T, D], F32, kind="ExternalOutput").ap()

    XT = dscr("XT", [8, 128, S], F32)
    HT = dscr("HT", [8, 128, S], BF16)
    ZF = dscr("ZF", [NFM, 128, S], BF16)
    ZTV = dscr("ZTV", [S, 512], BF16)
    ZTG = dscr("ZTG", [S, 16], F32)
    ZTGV = dscr("ZTGV", [S, 512], BF16)
    YA = dscr("YA", [4, 128, S], BF16)
    YB = dscr("YB", [4, 128, S], BF16)
    YC = dscr("YC", [4, 128, S], BF16)
    COMB = dscr("COMB", [S, 16], F32)
    MODD = dscr("MODD", [128, DEPTH * 48 * 4], F32)

    TILES = token_tiles()

    with ExitStack() as st:
        mk = MK(nc, st)
        gsb = lambda nm, shape, dt: st.enter_context(nc.sbuf_tensor(nm, list(shape), dt))
        ident_f = gsb("ident_f", [128, 128], F32)
        ident_b = gsb("ident_b", [128, 128], BF16)
        ones_b = gsb("ones_b", [128, 128], BF16)
        ones_f = gsb("ones_f", [128, 128], F32)
        VEC = gsb("VEC", [128, DEPTH, NV], F32)
        MOD = gsb("MOD", [128, DEPTH, 48, 4], F32)
        AB = gsb("AB", [128, DEPTH, 2, 4, 8], F32)

        mk.dma("sp", out=ident_f[:], in_=c_ident[:, :], w=["ident_f"])
        mk.dma("pool", out=ident_b[:], in_=c_ident[:, :], w=["ident_b"])
        mk.memset("dve", ones_b[:], 1.0, w=["ones_b"])
        mk.memset("dve", ones_f[:], 1.0, w=["ones_f"])
        mk.dma("sp", out=VEC[:], in_=vecs.rearrange("l p v -> p l v"), w=["VEC"])
        mk.barrier()

        def check_stop(l, name):
            return stop is not None and stop == (l, name)

        def stage_load_x():
            sg = Stage(mk, "ldx")
            xi = [sg.sb(f"xi{i}", [128, D], F32) for i in range(2)]
            xo = [sg.sb(f"xo{i}", [128, 8, 128], F32) for i in range(2)]
            pt = [[sg.ps(f"pt{i}{h}", [128, 512], F32) for h in range(2)] for i in range(2)]
            mk.dma("sp", out=xi[0][:], in_=xin[0:128, :], w=["xi0"])
            for blk in range(S // 128):
                p = blk % 2
                if blk + 1 < S // 128:
                    mk.dma("sp", out=xi[(blk + 1) % 2][:], in_=xin[(blk + 1) * 128:(blk + 2) * 128, :],
                           w=[f"xi{(blk + 1) % 2}"])
                for h in range(2):
                    for jj in range(4):
                        j = h * 4 + jj
                        mk.tr(pt[p][h][:, jj * 128:(jj + 1) * 128], xi[p][:, j * 128:(j + 1) * 128], ident_f[:],
                              r=[f"xi{p}"], w=[f"pt{p}{h}"])
                    mk.cp("dve" if h == 0 else "act", xo[p][:, h * 4:(h + 1) * 4, :],
                          pt[p][h][:].rearrange("p (j t) -> p j t", j=4), r=[f"pt{p}{h}"], w=[f"xo{p}{h}"])
                mk.dma("sp", out=XT[:, :, blk * 128:(blk + 1) * 128].rearrange("j p t -> p j t"), in_=xo[p][:],
                       r=[f"xo{p}0", f"xo{p}1"])
            sg.close()

        def stage_ada():
            sg = Stage(mk, "ada")
            cvt = sg.sb("cvt", [128, 8, 4], F32)
            sct = sg.sb("sct", [128, 8, 4], F32)
            wb = [sg.sb(f"wb{i}", [128, 8, 1024], F32) for i in range(2)]
            pp = [sg.ps(f"pp{i}", [128, 512], F32) for i in range(2)]
            mk.dma("sp", out=cvt[:], in_=cv[:, :, :], w=["cvt"])
            mk.act(sct[:], cvt[:], AF.Silu, r=["cvt"], w=["sct"])
            it = 0
            for l in range(nlayers):
                for g in range(6):
                    p = it % 2
                    it += 1
                    mk.dma("sp", out=wb[p][:],
                           in_=w_ada[l, :, g * 1024:(g + 1) * 1024].rearrange("(kc p) n -> p kc n", p=128),
                           w=[f"wb{p}"])
                    for j in range(8):
                        q = j % 2
                        for kc in range(8):
                            mk.mm(pp[q][:, 0:4], wb[p][:, kc, j * 128:(j + 1) * 128], sct[:, kc, :], kc == 0, kc == 7,
                                  r=[f"wb{p}", "sct"], w=[f"pp{q}"])
                        col = V_BADA + g * 8 + j
                        mk.ts("dve", MOD[:, l, g * 8 + j, :], pp[q][:, 0:4], VEC[:, l, col:col + 1], ALU.add,
                              r=[f"pp{q}"], w=["MOD"])
                for which, scc, nv in ((0, 8, V_N1), (1, 32, V_N2)):
                    for vi in range(3):
                        mk.stt(AB[:, l, which, vi, :], MOD[:, l, scc:scc + 8, vi], 1.0, VEC[:, l, nv:nv + 8],
                               ALU.add, ALU.mult, r=["MOD"], w=["AB"])
            if "MODD" in dbg:
                mk.dma("sp", out=MODD[:, :], in_=MOD[:].rearrange("p l c v -> p (l c v)"), r=["MOD"])
            sg.close()

        def stage_norm(l, which, router):
            sg = Stage(mk, f"nm{l}{which}")
            shc = 0 if which == 0 else 24
            xt = [sg.sb(f"xt{i}", [128, 8, 512], F32) for i in range(2)]
            sq = [sg.sb(f"sq{i}", [128, 8, 512], BF16) for i in range(2)]
            rt = [sg.sb(f"rt{i}", [128, 512], F32) for i in range(2)]
            hb = [sg.sb(f"hb{i}", [128, 8, 512], BF16) for i in range(2)]
            pss = [sg.ps(f"pss{i}", [128, 512], F32) for i in range(2)]
            if router:
                hf = [sg.sb(f"hf{i}", [128, 8, 512], F32) for i in range(2)]
                rwt = sg.sb("rwt", [128, 8, 20], F32)
                rbb = sg.sb("rbb", [128, 20], F32)
                psr = [sg.ps(f"psr{i}", [128, 512], F32) for i in range(2)]
                lg = [sg.sb(f"lg{i}", [128, 4, 20], F32) for i in range(2)]
                wk = {n: [sg.sb(f"{n}{i}", shp, F32) for i in range(2)] for n, shp in (
                    ("gmx", [128, 4, 1]), ("goh", [128, 4, 4]), ("gex", [128, 4, 4]), ("gsm", [128, 4, 1]),
                    ("em", [128, 4, 16]), ("t1", [128, 4, 1]), ("m1", [128, 4, 16]), ("em2", [128, 4, 16]),
                    ("t2", [128, 4, 1]), ("m2", [128, 4, 16]), ("w1", [128, 4, 1]), ("cmb", [128, 4, 16]))}
                mk.dma("sp", out=rwt[:], in_=rw[l].rearrange("(kc p) n -> p kc n", p=128), w=["rwt"])
                mk.dma("sp", out=rbb[:], in_=rows[l, 0:1, R_RB:R_RB + 20].to_broadcast([128, 20]), w=["rbb"])
            TL = [t_ for t_ in TILES if not (which == 1 and l == DEPTH - 1 and t_[2] == 2)]

            def ld_(i):
                n0_, sz_, vi_, b_ = TL[i]
                p_ = i % 2
                mk.dma("sp", out=xt[p_][:, :, :sz_], in_=XT[:, :, n0_:n0_ + sz_].rearrange("j p t -> p j t"),
                       w=[f"xt{p_}"] + [f"xs{p_}{j}" for j in range(8)])
            ld_(0)
            for i, (n0, sz, vi, b) in enumerate(TL):
                p = i % 2
                if i + 1 < len(TL):
                    ld_(i + 1)
                mk.act(sq[p][:, :, :sz], xt[p][:, :, :sz], AF.Square, r=[f"xt{p}"], w=[f"sq{p}"])
                for j in range(8):
                    mk.mm(pss[p][:, :sz], ones_b[:], sq[p][:, j, :sz], j == 0, j == 7, r=[f"sq{p}"], w=[f"pss{p}"])
                mk.act(rt[p][:, :sz], pss[p][:, :sz], AF.Sqrt, bias=EPS, scale=1.0 / D, r=[f"pss{p}"], w=[f"rt{p}"])
                mk.recip(rt[p][:, :sz], rt[p][:, :sz], r=[f"rt{p}"], w=[f"rt{p}"])
                dst = hf[p] if router else hb[p]
                dk = f"hf{p}" if router else f"hb{p}"
                for j in range(8):
                    mk.tt("pool" if j % 2 else "dve", xt[p][:, j, :sz], xt[p][:, j, :sz], rt[p][:, :sz], ALU.mult,
                          r=[f"xt{p}", f"rt{p}"], w=[f"xs{p}{j}"])
                    mk.act(dst[:, j, :sz], xt[p][:, j, :sz], AF.Identity, scale=AB[:, l, which, vi, j:j + 1],
                           bias=MOD[:, l, shc + j, vi:vi + 1], r=[f"xs{p}{j}"], w=[f"{dk}{j}"])
                if router:
                    for hh in range(2):
                        mk.cp("dve" if hh == 0 else "pool", hb[p][:, hh * 4:(hh + 1) * 4, :sz],
                              hf[p][:, hh * 4:(hh + 1) * 4, :sz], r=[f"hf{p}{j}" for j in range(hh * 4, hh * 4 + 4)],
                              w=[f"hb{p}_{hh}"])
                    hbk = [f"hb{p}_0", f"hb{p}_1"]
                else:
                    hbk = [f"hb{p}{j}" for j in range(8)]
                mk.dma("sp", out=HT[:, :, n0:n0 + sz].rearrange("j p t -> p j t"), in_=hb[p][:, :, :sz], r=hbk)
                if router:
                    nb = sz // 128
                    K = lambda n: f"{n}{p}"
                    for bb in range(nb):
                        for kc in range(8):
                            mk.mm(psr[p][:, bb * 20:(bb + 1) * 20], hf[p][:, kc, bb * 128:(bb + 1) * 128],
                                  rwt[:, kc, :], kc == 0, kc == 7, r=[f"hf{p}{kc}", "rwt"], w=[K("psr")])
                    LG = lg[p][:, :nb, :]
                    mk.tt("dve", LG, psr[p][:, 0:nb * 20].rearrange("p (b n) -> p b n", n=20),
                          rbb[:].unsqueeze(1).to_broadcast([128, nb, 20]), ALU.add, r=[K("psr"), "rbb"], w=[K("lg")])
                    G = lg[p][:, :nb, 0:4]
                    EX = lg[p][:, :nb, 4:20]
                    W_ = {n: wk[n][p][:, :nb, :] for n in wk}
                    mk.op("dve", lambda e: e.tensor_reduce(out=W_["gmx"], in_=G, axis=AX.X, op=ALU.max),
                          r=[K("lg")], w=[K("gmx")])
                    mk.tt("dve", W_["goh"], G, W_["gmx"].to_broadcast([128, nb, 4]), ALU.is_equal,
                          r=[K("lg"), K("gmx")], w=[K("goh")])
                    mk.tt("dve", W_["gex"], G, W_["gmx"].to_broadcast([128, nb, 4]), ALU.subtract,
                          r=[K("lg"), K("gmx")], w=[K("gex")])
                    mk.act(W_["gex"], W_["gex"], AF.Exp, r=[K("gex")], w=[K("gex")])
                    mk.op("dve", lambda e: e.tensor_reduce(out=W_["gsm"], in_=W_["gex"], axis=AX.X, op=ALU.add),
                          r=[K("gex")], w=[K("gsm")])
                    mk.recip(W_["gsm"], W_["gsm"], r=[K("gsm")], w=[K("gsm")])
                    mk.ts("dve", W_["goh"], W_["goh"], 1e4, ALU.mult, -1e4, ALU.add, r=[K("goh")], w=[K("goh")])
                    mk.tt("dve", wk["em"][p][:, :nb, :].rearrange("p b (g e) -> p b g e", g=4),
                          EX.rearrange("p b (g e) -> p b g e", g=4),
                          W_["goh"].unsqueeze(3).to_broadcast([128, nb, 4, 4]), ALU.add,
                          r=[K("lg"), K("goh")], w=[K("em")])
                    mk.op("dve", lambda e: e.tensor_reduce(out=W_["t1"], in_=W_["em"], axis=AX.X, op=ALU.max),
                          r=[K("em")], w=[K("t1")])
                    mk.tt("dve", W_["m1"], W_["em"], W_["t1"].to_broadcast([128, nb, 16]), ALU.is_equal,
                          r=[K("em"), K("t1")], w=[K("m1")])
                    mk.stt(W_["em2"], W_["m1"], -1e4, W_["em"], ALU.mult, ALU.add, r=[K("m1"), K("em")], w=[K("em2")])
                    mk.op("dve", lambda e: e.tensor_reduce(out=W_["t2"], in_=W_["em2"], axis=AX.X, op=ALU.max),
                          r=[K("em2")], w=[K("t2")])
                    mk.tt("dve", W_["m2"], W_["em2"], W_["t2"].to_broadcast([128, nb, 16]), ALU.is_equal,
                          r=[K("em2"), K("t2")], w=[K("m2")])
                    mk.tt("dve", W_["w1"], W_["t1"], W_["t2"], ALU.subtract, r=[K("t1"), K("t2")], w=[K("w1")])
                    mk.act(W_["w1"], W_["w1"], AF.Sigmoid, r=[K("w1")], w=[K("w1")])
                    mk.tt("dve", W_["m1"], W_["m1"], W_["m2"], ALU.subtract, r=[K("m1"), K("m2")], w=[K("m1")])
                    mk.tt("dve", W_["m1"], W_["m1"], W_["w1"].to_broadcast([128, nb, 16]), ALU.mult,
                          r=[K("m1"), K("w1")], w=[K("m1")])
                    mk.tt("dve", W_["m1"], W_["m1"], W_["m2"], ALU.add, r=[K("m1"), K("m2")], w=[K("m1")])
                    mk.tt("dve", W_["cmb"], W_["m1"], W_["gsm"].to_broadcast([128, nb, 16]), ALU.mult,
                          r=[K("m1"), K("gsm")], w=[K("cmb")])
                    mk.dma("sp", out=COMB[n0:n0 + sz, :].rearrange("(b p) e -> p b e", p=128), in_=W_["cmb"],
                           r=[K("cmb")])
            sg.close()

        def stage_z(l):
            sg = Stage(mk, f"z{l}")
            ht = sg.sb("ht", [128, 8, S], BF16)
            wf = [sg.sb(f"wf{i}", [128, 8, 512], BF16) for i in range(2)]
            wt = sg.sb("wt", [128, 8, NTMC], BF16)
            zo = [sg.sb(f"zo{i}", [128, 4, 512], BF16) for i in range(2)]
            zv = [sg.sb(f"zv{i}", [128, 4, 512], BF16) for i in range(2)]
            zgv = [sg.sb(f"zgv{i}", [128, 4, 512], BF16) for i in range(2)]
            zg = [sg.sb(f"zg{i}", [128, 4, 16], F32) for i in range(2)]
            pz = [sg.ps(f"pz{i}", [128, 512], F32) for i in range(4)]
            pv = [sg.ps(f"pv{i}", [128, 512], F32) for i in range(2)]
            pg = sg.ps("pg", [128, 512], F32)
            for i, (n0, sz, vi, b) in enumerate(TILES):
                mk.dma("sp", out=ht[:, :, n0:n0 + sz], in_=HT[:, :, n0:n0 + sz].rearrange("j p t -> p j t"),
                       w=[f"ht{i}"])
            mk.dma("pool", out=wt[:], in_=w_in_r[l, :, NFM * 128:NFM * 128 + NTMC].rearrange("(kc p) n -> p kc n", p=128),
                   w=["wt"])
            funcs = {}
            for c in range(NFM):
                funcs[c] = AF.Identity
            for c in range(C_MO, C_MO + 4):
                funcs[c] = AF.Sigmoid
            for c in range(C_GU, C_GU + 4):
                funcs[c] = AF.Gelu
            for c in range(C_BRA, NFM):
                funcs[c] = AF.Sigmoid
            groups = [(c0, min(4, NFM - c0)) for c0 in range(0, NFM, 4)]
            pzi = 0
            oi = 0
            def load_wf(gi):
                c0_, ncg_ = groups[gi]
                mk.dma("pool", out=wf[gi % 2][:, :, :ncg_ * 128],
                       in_=w_in_r[l, :, c0_ * 128:(c0_ + ncg_) * 128].rearrange("(kc p) n -> p kc n", p=128),
                       w=[f"wf{gi % 2}"])
            load_wf(0)
            for gi, (c0, ncg) in enumerate(groups):
                p = gi % 2
                if gi >= 1 and gi + 1 < len(groups):
                    load_wf(gi + 1)
                if gi == 0:
                    load_wf(1)
                for i, (n0, sz, vi, b) in enumerate(TILES):
                    o = oi % 2
                    oi += 1
                    for ci in range(ncg):
                        q = pzi % 4
                        pzi += 1
                        for kc in range(8):
                            mk.mm(pz[q][:, :sz], wf[p][:, kc, ci * 128:(ci + 1) * 128], ht[:, kc, n0:n0 + sz],
                                  kc == 0, kc == 7, r=[f"wf{p}", f"ht{i}"], w=[f"pz{q}"])
                        mk.act(zo[o][:, ci, :sz], pz[q][:, :sz], funcs[c0 + ci], r=[f"pz{q}"], w=[f"zo{o}_{ci}"])
                    mk.dma("sp", out=ZF[c0:c0 + ncg, :, n0:n0 + sz].rearrange("j p t -> p j t"),
                           in_=zo[o][:, :ncg, :sz], r=[f"zo{o}_{ci}" for ci in range(ncg)])
            for i, (n0, sz, vi, b) in enumerate(TILES):
                o = i % 2
                nb = sz // 128
                for bb in range(nb):
                    t0 = n0 + bb * 128
                    q = bb % 2
                    for kc in range(8):
                        mk.mm(pv[q][:, :], ht[:, kc, t0:t0 + 128], wt[:, kc, 0:512], kc == 0, kc == 7,
                              r=[f"ht{i}", "wt"], w=[f"pv{q}"])
                    mk.cp("dve", zv[o][:, bb, :], pv[q][:, :], r=[f"pv{q}"], w=[f"zv{o}_{bb}"])
                    for kc in range(8):
                        mk.mm(pz[q][:, :], ht[:, kc, t0:t0 + 128], wt[:, kc, 528:1040], kc == 0, kc == 7,
                              r=[f"ht{i}", "wt"], w=[f"pz{q}"])
                    mk.act(zgv[o][:, bb, :], pz[q][:, :], AF.Gelu, r=[f"pz{q}"], w=[f"zgv{o}_{bb}"])
                    for kc in range(8):
                        mk.mm(pg[:, bb * 16:(bb + 1) * 16], ht[:, kc, t0:t0 + 128], wt[:, kc, 512:528], kc == 0, kc == 7,
                              r=[f"ht{i}", "wt"], w=["pg"])
                mk.cp("dve", zg[o][:, :nb, :], pg[:, 0:nb * 16].rearrange("p (b n) -> p b n", n=16), r=["pg"],
                      w=[f"zg{o}"])
                mk.dma("sp", out=ZTV[n0:n0 + sz, :].rearrange("(b p) f -> p b f", p=128), in_=zv[o][:, :nb, :],
                       r=[f"zv{o}_{bb}" for bb in range(nb)])
                mk.dma("sp", out=ZTGV[n0:n0 + sz, :].rearrange("(b p) f -> p b f", p=128), in_=zgv[o][:, :nb, :],
                       r=[f"zgv{o}_{bb}" for bb in range(nb)])
                mk.dma("sp", out=ZTG[n0:n0 + sz, :].rearrange("(b p) f -> p b f", p=128), in_=zg[o][:, :nb, :],
                       r=[f"zg{o}"])
            sg.close()

        def stage_mlstm(l, b):
            sg = Stage(mk, f"ml{l}{b}")
            NC_ = L // 128
            s0 = b * L
            raw = [sg.sb(f"raw{i}", [128, L], BF16) for i in range(2)]
            qk = sg.sb("qk", [128, 8, L], BF16)
            ktm = sg.sb("ktm", [128, NC_, 4, 128], BF16)
            vaug = sg.sb("vaug", [128, NC_, 4, 128], BF16)
            dg = sg.sb("dg", [128, 8, 3, 128], BF16)
            gt = sg.sb("gt", [128, NC_, 16], F32)
            gbb = sg.sb("gbb", [128, 16], F32)
            lt = sg.sb("lt", [128, NC_, 8], F32)
            cl = sg.sb("cl", [128, NC_, 8], F32)
            wsx = sg.sb("wsx", [128, NC_, 8], F32)
            flo = sg.sb("flo", [128, NC_, 8], F32)
            ebe = sg.sb("ebe", [128, NC_, 8], F32)
            ebs = sg.sb("ebs", [128, NC_, 8], F32)
            trif = sg.sb("trif", [128, 128], F32)
            trib = sg.sb("trib", [128, 128], F32)
            maskf = sg.sb("maskf", [128, 128], F32)
            maskb = sg.sb("maskb", [128, 128], F32)
            hX = [sg.sb("hX0", [128, NC_, 4, 128], F32), sg.sb("hX1", [128, NC_, 4, 128], BF16)]
            smo = sg.sb("smo", [128, 4, L], BF16)
            U = [sg.sb(f"U{i}", [128, 4, 130], F32) for i in range(2)]
            CTb = [[sg.sb(f"CTb{d}{i}", [128, 4, 130], BF16) for i in range(2)] for d in range(2)]
            qkt = [[sg.sb(f"qkt{d}{i}", [128, 4, 128], BF16) for i in range(2)] for d in range(2)]
            vt = [[sg.sb(f"vt{d}{i}", [128, 4, 130], BF16) for i in range(2)] for d in range(2)]
            adn = [sg.sb(f"adn{i}", [128, 4, 2], F32) for i in range(2)]
            rin = [sg.sb(f"rin{i}", [128, 4], F32) for i in range(2)]
            pst = [sg.ps(f"pst{i}", [128, 512], F32) for i in range(2)]
            pnm = [sg.ps(f"pnm{i}", [128, 512], F32) for i in range(2)]
            ppp = [sg.ps(f"ppp{i}", [128, 512], F32) for i in range(2)]
            psm = [sg.ps(f"psm{i}", [128, 512], F32) for i in range(2)]
            ptrv = [ppp[i][:].bitcast(BF16) for i in range(2)]

            mk.dma("sp", out=smo[:], in_=ZF[C_MO:C_MO + 4, :, s0:s0 + L].rearrange("j p t -> p j t"), w=["smo"])
            mk.dma("sp", out=vaug[:].rearrange("p c h e -> p c (h e)"),
                   in_=ZTV[s0:s0 + L, :].rearrange("(c p) f -> p c f", p=128), w=["vaug"])
            mk.dma("sp", out=gt[:], in_=ZTG[s0:s0 + L, :].rearrange("(c p) n -> p c n", p=128), w=["gt"])
            mk.dma("sp", out=gbb[:], in_=rows[l, 0:1, R_GB:R_GB + 16].to_broadcast([128, 16]), w=["gbb"])
            for tdst, tsrc, nm in ((trif, c_trif, "trif"), (trib, c_trib, "trib"), (maskf, c_maskf, "maskf"),
                                   (maskb, c_maskb, "maskb")):
                mk.dma("sp", out=tdst[:], in_=tsrc[:, :], w=[nm])
            for d in range(2):
                mk.memset("pool", U[d][:], 0.0, w=[f"U{d}"])
                mk.memset("pool", CTb[d][0][:], 0.0, w=[f"CTb{d}0"])
                mk.memset("pool", CTb[d][1][:], 0.0, w=[f"CTb{d}1"])
            seq_tiles_ = [(0, LC)] + [(LC + i * 512, 512) for i in range(4)]
            for j in range(8):
                for tap in range(3):
                    mk.ts("pool" if tap == 1 else "dve", dg[:, j, tap, :], ident_f[:],
                          VEC[:, l, V_CONV + j * 3 + tap:V_CONV + j * 3 + tap + 1], ALU.mult, r=["ident_f"], w=[f"dg{j}"])
            ci = 0
            for j in range(8):
                rw_ = raw[j % 2]
                rk = f"raw{j % 2}"
                mk.dma("sp", out=rw_[:], in_=ZF[C_MQ + j, :, s0:s0 + L], w=[rk])
                for (t0, sz) in seq_tiles_:
                    t1_ = t0 + sz
                    sa, sb_ = (0, LC) if t0 < LC else (LC, L)
                    u = ci % 2
                    ci += 1
                    pc_ = pnm[u]
                    mk.mm(pc_[:, 0:sz], dg[:, j, 1, :], rw_[:, t0:t1_], True, False, r=[f"dg{j}", rk], w=[f"pnm{u}"])
                    lo = max(t0, sa + 1)
                    mk.mm(pc_[:, lo - t0:sz], dg[:, j, 0, :], rw_[:, lo - 1:t1_ - 1], False, False, r=[f"dg{j}", rk],
                          w=[f"pnm{u}"])
                    hi = min(t1_, sb_ - 1)
                    mk.mm(pc_[:, 0:hi - t0], dg[:, j, 2, :], rw_[:, t0 + 1:hi + 1], False, True, r=[f"dg{j}", rk],
                          w=[f"pnm{u}"])
                    mk.act(qk[:, j, t0:t1_], pc_[:, 0:sz], AF.Silu, r=[f"pnm{u}"], w=[f"qk{j}"])
            for c in range(NC_):
                p = c % 2
                for h in range(4):
                    mk.tr(ptrv[p][:, h * 128:(h + 1) * 128], qk[:, 4 + h, c * 128:(c + 1) * 128],
                          ident_b[:], r=[f"qk{4 + h}", "ident_b"], w=[f"ppp{p}"])
                mk.cp("dve" if c % 2 else "act", ktm[:, c, :, :],
                      ptrv[p][:, 0:512].rearrange("p (h d) -> p h d", h=4), r=[f"ppp{p}"], w=[f"ktm{c}"])
            mk.tt("dve", gt[:], gt[:], gbb[:].unsqueeze(1).to_broadcast([128, NC_, 16]), ALU.add, r=["gt", "gbb"],
                  w=["gt"])
            mk.act(lt[:, :, 0:4], gt[:, :, 4:8], AF.Exp, scale=-1.0, r=["gt"], w=["lt"])
            mk.act(lt[:, :, 4:8], gt[:, :, 12:16], AF.Exp, scale=-1.0, r=["gt"], w=["lt"])
            mk.act(lt[:], lt[:], AF.Ln, bias=1.0, r=["lt"], w=["lt"])
            pcs = pst[0]
            ptot = pst[1]
            for c in range(NC_):
                mk.mm(pcs[:, c * 8:c * 8 + 4], trif[:], lt[:, c, 0:4], True, True, r=["trif", "lt"], w=["pst0"])
                mk.mm(pcs[:, c * 8 + 4:c * 8 + 8], trib[:], lt[:, c, 4:8], True, True, r=["trib", "lt"], w=["pst0"])
                mk.mm(ptot[:, c * 8:c * 8 + 8], ones_f[:], lt[:, c, :], True, True, r=["ones_f", "lt"], w=["pst1"])
            mk.cp("dve", cl[:], pcs[:, 0:NC_ * 8].rearrange("p (c n) -> p c n", n=8), r=["pst0"], w=["cl"])
            mk.act(flo[:], cl[:], AF.Exp, r=["cl"], w=["flo"])
            mk.act(ebe[:], ptot[:, 0:NC_ * 8].rearrange("p (c n) -> p c n", n=8), AF.Exp, scale=-1.0, r=["pst1"],
                   w=["ebe"])
            mk.ts("dve", ebs[:], ebe[:], QS, ALU.mult, r=["ebe"], w=["ebs"])
            mk.tt("dve", wsx[:, :, 0:4], gt[:, :, 0:4], cl[:, :, 0:4], ALU.add, r=["gt", "cl"], w=["wsx"])
            mk.tt("dve", wsx[:, :, 4:8], gt[:, :, 8:12], cl[:, :, 4:8], ALU.add, r=["gt", "cl"], w=["wsx"])
            mk.act(wsx[:], wsx[:], AF.Exp, r=["wsx"], w=["wsx"])
            order = [list(range(NC_)), [1, 0] + list(range(NC_ - 1, 1, -1))]

            def ctx_(step, d):
                c = order[d][step]
                return c, slice(c * 128, (c + 1) * 128), slice(d * 4, d * 4 + 4), f"qkt{d}{step % 2}", f"vt{d}{step % 2}"

            def a1(step):
                sp_ = step % 2
                for d in range(2):
                    c, cs, lns, qkk, vtk = ctx_(step, d)
                    mask = maskf if d == 0 else maskb
                    mkey = "maskf" if d == 0 else "maskb"
                    for h in range(4):
                        mk.mm(pst[d][:, h * 128:(h + 1) * 128], qk[:, 4 + h, cs], qk[:, h, cs], True, True,
                              r=[f"qk{h}", f"qk{4 + h}"], w=[f"pst{d}"])
                    mk.tt("dve", qkt[d][sp_][:], pst[d][:].rearrange("p (h t) -> p h t", h=4),
                          mask[:].unsqueeze(1).to_broadcast([128, 4, 128]), ALU.mult, r=[f"pst{d}", mkey], w=[qkk])
                    mk.tt("pool", vt[d][sp_][:, :, 0:128], vaug[:, c, :, :],
                          wsx[:, c, lns].unsqueeze(2).to_broadcast([128, 4, 128]), ALU.mult, r=["vaug", "wsx"], w=[vtk])
                    mk.cp("pool", vt[d][sp_][:, :, 128:130], wsx[:, c, lns].unsqueeze(2).to_broadcast([128, 4, 2]),
                          r=["wsx", vtk], w=[vtk])

            def a2(step):
                sp_ = step % 2
                for d in range(2):
                    c, cs, lns, qkk, vtk = ctx_(step, d)
                    for h in range(4):
                        mk.mm(ppp[d][:, h * 128:(h + 1) * 128], ktm[:, c, h, :], vt[d][sp_][:, h, 0:128], True, True,
                              r=[f"ktm{c}", vtk], w=[f"ppp{d}"])
                        o2 = d * 8 + h * 2
                        mk.mm(psm[0][:, o2:o2 + 2], ktm[:, c, h, :], vt[d][sp_][:, h, 128:130], True, True,
                              r=[f"ktm{c}", vtk], w=["psm0"])

            def c1(step):
                sp_ = step % 2
                cb_ = (step + 1) % 2
                for d in range(2):
                    c, cs, lns, qkk, vtk = ctx_(step, d)
                    for h in range(4):
                        mk.mm(pnm[d][:, h * 128:(h + 1) * 128], qkt[d][sp_][:, h, :], vt[d][sp_][:, h, 0:128], True, False,
                              r=[qkk, vtk], w=[f"pnm{d}"])
                        mk.mm(pnm[d][:, h * 128:(h + 1) * 128], qk[:, h, cs], CTb[d][cb_][:, h, 0:128], False, True,
                              r=[f"qk{h}", f"CTb{d}{cb_}"], w=[f"pnm{d}"])
                        o1 = d * 8 + h * 2
                        mk.mm(psm[1][:, o1:o1 + 2], qkt[d][sp_][:, h, :], vt[d][sp_][:, h, 128:130], True, False,
                              r=[qkk, vtk], w=["psm1"])
                        mk.mm(psm[1][:, o1:o1 + 2], qk[:, h, cs], CTb[d][cb_][:, h, 128:130], False, True,
                              r=[f"qk{h}", f"CTb{d}{cb_}"], w=["psm1"])

            def c2(step):
                for d in range(2):
                    c, cs, lns, qkk, vtk = ctx_(step, d)
                    denv = psm[1][:, d * 8:d * 8 + 8].rearrange("p (h n) -> p h n", n=2)
                    mk.ts("dve", adn[d][:], denv, -1.0, ALU.mult, r=["psm1"], w=[f"adn{d}"])
                    mk.tt("dve", rin[d][:], denv[:, :, 0], adn[d][:, :, 0], ALU.max, r=["psm1", f"adn{d}"],
                          w=[f"rin{d}"])
                    mk.tt("dve", rin[d][:], rin[d][:], flo[:, c, lns], ALU.max, r=[f"rin{d}", "flo"],
                          w=[f"rin{d}"])
                    mk.recip(rin[d][:], rin[d][:], r=[f"rin{d}"], w=[f"rin{d}"])
                    mk.tt("dve", hX[d][:, c, :, :], pnm[d][:].rearrange("p (h e) -> p h e", h=4),
                          rin[d][:].unsqueeze(2).to_broadcast([128, 4, 128]), ALU.mult, r=[f"pnm{d}", f"rin{d}"],
                          w=[f"hX{d}_{c}"])

            def bb_(step):
                cb_ = step % 2
                for d in range(2):
                    c, cs, lns, qkk, vtk = ctx_(step, d)
                    cprev = order[d][step - 1] if step > 0 else None
                    if cprev is not None:
                        mk.tt("pool", U[d][:], U[d][:], ebe[:, cprev, lns].unsqueeze(2).to_broadcast([128, 4, 130]),
                              ALU.mult, r=[f"U{d}", "ebe"], w=[f"U{d}"])
                    mk.tt("dve", U[d][:, :, 0:128], ppp[d][:].rearrange("p (h e) -> p h e", h=4), U[d][:, :, 0:128],
                          ALU.add, r=[f"ppp{d}", f"U{d}"], w=[f"U{d}"])
                    mk.tt("dve", U[d][:, :, 128:130],
                          psm[0][:, d * 8:d * 8 + 8].rearrange("p (h n) -> p h n", n=2), U[d][:, :, 128:130],
                          ALU.add, r=["psm0", f"U{d}"], w=[f"U{d}"])
                    mk.tt("pool", CTb[d][cb_][:], U[d][:], ebs[:, c, lns].unsqueeze(2).to_broadcast([128, 4, 130]),
                          ALU.mult, r=[f"U{d}", "ebs"], w=[f"CTb{d}{cb_}"])

            import os as _os
            _skip = _os.environ.get("MKSKIP", "")
            for i in range(NC_ + 1 if "scan" not in _skip else 0):
                if i < NC_:
                    a1(i)
                if i >= 1:
                    c1(i - 1)
                if i < NC_:
                    a2(i)
                if i >= 1:
                    c2(i - 1)
                if i < NC_:
                    bb_(i)
            GC = 6
            hsq = sg.sb("hsq", [128, GC, 4, 128], F32)
            ssq = sg.sb("ssq", [128, NC_, 4], F32)
            hn = [sg.sb(f"hn{i}", [128, 4, 128], BF16) for i in range(2)]
            ya = smo
            for g0 in range(0, NC_ if "post" not in _skip else 0, GC):
                gk = f"hsg{g0}"
                xk = [f"hX0_{c}" for c in range(g0, g0 + GC)] + [f"hX1_{c}" for c in range(g0, g0 + GC)]
                mk.tt("pool", hX[0][:, g0:g0 + GC, :, :], hX[0][:, g0:g0 + GC, :, :], hX[1][:, g0:g0 + GC, :, :], ALU.add,
                      r=xk, w=[gk])
                mk.tt("dve", hsq[:], hX[0][:, g0:g0 + GC, :, :], hX[0][:, g0:g0 + GC, :, :], ALU.mult, r=[gk], w=["hsq"])
                mk.op("dve", lambda e: e.tensor_reduce(out=ssq[:, g0:g0 + GC, :], in_=hsq[:], axis=AX.X, op=ALU.add),
                      r=["hsq"], w=[f"ssq{g0}"])
                mk.act(ssq[:, g0:g0 + GC, :], ssq[:, g0:g0 + GC, :], AF.Sqrt, bias=EPS, scale=1.0 / 128, r=[f"ssq{g0}"],
                       w=[f"ssq{g0}"])
                mk.recip(ssq[:, g0:g0 + GC, :], ssq[:, g0:g0 + GC, :], r=[f"ssq{g0}"], w=[f"ssq{g0}"])
                for c in range(g0, g0 + GC):
                    p = c % 2
                    mk.tt("pool", hn[p][:], hX[0][:, c, :, :], ssq[:, c, :].unsqueeze(2).to_broadcast([128, 4, 128]),
                          ALU.mult, r=[gk, f"ssq{g0}"], w=[f"hn{p}"])
                    for h in range(4):
                        mk.tr(ptrv[p][:, h * 128:(h + 1) * 128], hn[p][:, h, :], ident_b[:], r=[f"hn{p}"],
                              w=[f"ppp{p}"])
                    for h in range(4):
                        mk.stt(ya[:, h, c * 128:(c + 1) * 128], ptrv[p][:, h * 128:(h + 1) * 128],
                               VEC[:, l, V_MNORM + h:V_MNORM + h + 1], smo[:, h, c * 128:(c + 1) * 128],
                               ALU.mult, ALU.mult, r=[f"ppp{p}", "smo"], w=[f"ya{c}"])
            mk.dma("sp", out=YA[:, :, s0:s0 + L].rearrange("j p t -> p j t"), in_=ya[:],
                   r=[f"ya{c}" for c in range(NC_)])
            sg.close()

        def stage_mla(l, b):
            sg = Stage(mk, f"at{l}{b}")
            NC_ = L // 128
            s0 = b * L
            seq_tiles = [(0, LC)] + [(LC + i * 512, 512) for i in range(4)]
            aq = sg.sb("aq", [128, 3, L], BF16)
            akv = sg.sb("akv", [128, 2, L], BF16)
            sqb = sg.sb("sqb", [128, 3, 512], BF16)
            rs = [sg.sb(f"rs{i}", [128, 512], F32) for i in range(2)]
            wq = sg.sb("wq", [128, 3, 1536], BF16)
            wkv = sg.sb("wkv", [128, 2, 1024], BF16)
            cc = sg.sb("cc", [128, L], F32)
            ss = sg.sb("ss", [128, L], F32)
            kra = sg.sb("kra", [128, 2, L], BF16)
            krt = [sg.sb(f"krt{i}", [128, 512], F32) for i in range(2)]
            kr = sg.sb("kr", [128, L], BF16)
            QT = [sg.sb(f"QT{i}", [128, L], BF16) for i in range(2)]
            KT = [sg.sb(f"KT{i}", [128, L], BF16) for i in range(2)]
            VA = sg.sb("VA", [128, NC_, 8, 128], BF16)
            t1 = [sg.sb(f"t1{i}", [128, 512], F32) for i in range(2)]
            t2 = [sg.sb(f"t2{i}", [128, 512], F32) for i in range(2)]
            pT = [sg.sb(f"pT{i}", [128, 512], BF16) for i in range(4)]
            rec = [sg.sb(f"rec{i}", [64, 512], F32) for i in range(2)]
            yb = sg.sb("yb", [128, 4, L], BF16)
            pa = [sg.ps("pa0", [128, 512], F32)] * 2
            pb_ = [sg.ps("pb0", [128, 512], F32)] * 2
            psc = [sg.ps(f"psc{i}", [128, 512], F32) for i in range(4)]
            pac = [sg.ps(f"pac{i}", [128, 512], F32) for i in range(2)]

            mk.dma("sp", out=aq[:], in_=ZF[C_AQ:C_AQ + 3, :, s0:s0 + L].rearrange("j p t -> p j t"), w=["aq"])
            mk.dma("sp", out=akv[:], in_=ZF[C_AKV:C_AKV + 2, :, s0:s0 + L].rearrange("j p t -> p j t"), w=["akv"])
            mk.dma("sp", out=kra[:], in_=ZF[C_KRA:C_KRA + 2, :, s0:s0 + L].rearrange("j p t -> p j t"), w=["kra"])
            mk.dma("sp", out=cc[:], in_=c_cc[:, :], w=["cc"])
            mk.dma("sp", out=ss[:], in_=c_ss[:, :], w=["ss"])
            mk.dma("pool", out=wq[:], in_=wuq_r[l].rearrange("(kc p) n -> p kc n", p=128), w=["wq"])
            mk.dma("pool", out=wkv[:], in_=wukv_r[l].rearrange("(kc p) n -> p kc n", p=128), w=["wkv"])
            mk.memset("pool", VA[:, :, :, 64:128], 1.0, w=["VA1"])

            def rms_fm(src, nch, gcol, skey, ti):
                for (a0, sz) in seq_tiles:
                    p = ti[0] % 2
                    ti[0] += 1
                    mk.act(sqb[:, :nch, :sz], src[:, :, a0:a0 + sz], AF.Square, r=[skey], w=["sqb"])
                    for j in range(nch):
                        mk.mm(pa[p][:, :sz], ones_b[:], sqb[:, j, :sz], j == 0, j == nch - 1, r=["sqb"], w=["pa0"])
                    mk.act(rs[p][:, :sz], pa[p][:, :sz], AF.Sqrt, bias=EPS, scale=1.0 / (nch * 128), r=["pa0"],
                           w=[f"rs{p}"])
                    mk.recip(rs[p][:, :sz], rs[p][:, :sz], r=[f"rs{p}"], w=[f"rs{p}"])
                    for j in range(nch):
                        mk.stt(src[:, j, a0:a0 + sz], src[:, j, a0:a0 + sz], VEC[:, l, gcol + j:gcol + j + 1],
                               rs[p][:, :sz], ALU.mult, ALU.mult, r=[skey, f"rs{p}"], w=[skey])
            ti = [0]
            rms_fm(aq, 3, V_QN, "aq", ti)
            rms_fm(akv, 2, V_KVN, "akv", ti)
            for i, (a0, sz) in enumerate(seq_tiles):
                p = i % 2
                mk.tt("dve", krt[p][64:96, :sz], kra[64:96, 0, a0:a0 + sz], cc[64:96, a0:a0 + sz], ALU.mult,
                      r=["kra", "cc"], w=[f"krt{p}"])
                mk.tt("pool", t1[p][64:96, :sz], kra[64:96, 1, a0:a0 + sz], ss[64:96, a0:a0 + sz], ALU.mult,
                      r=["kra", "ss"], w=[f"t1{p}"])
                mk.tt("dve", kr[64:96, a0:a0 + sz], krt[p][64:96, :sz], t1[p][64:96, :sz], ALU.add,
                      r=[f"krt{p}", f"t1{p}"], w=["kr"])
            for c in range(NC_):
                u = c % 2
                for kc in range(2):
                    mk.mm(pac[u][:, :], akv[:, kc, c * 128:(c + 1) * 128], wkv[:, kc, 512:1024], kc == 0, kc == 1,
                          r=["akv", "wkv"], w=[f"pac{u}"])
                mk.cp("dve" if c % 2 else "act", VA[:, c, :, 0:64], pac[u][:, :].rearrange("p (h e) -> p h e", h=8),
                      r=[f"pac{u}"], w=[f"VA{c}"])
            vak = [f"VA{c}" for c in range(NC_)] + ["VA1"]
            ui = [0]

            def proj(h):
                hp = h % 2
                for (a0, sz) in seq_tiles:
                    u = ui[0] % 2
                    ui[0] += 1
                    for kc in range(3):
                        mk.mm(pa[u][0:96, :sz], wq[:, kc, h * 96:(h + 1) * 96], aq[:, kc, a0:a0 + sz], kc == 0, kc == 2,
                              r=["wq", "aq"], w=["pa0"])
                    for kc in range(3):
                        mk.mm(pb_[u][0:96, :sz], wq[:, kc, 768 + h * 96:768 + (h + 1) * 96], aq[:, kc, a0:a0 + sz],
                              kc == 0, kc == 2, r=["wq", "aq"], w=["pb0"])
                    mk.tt("dve", t1[u][0:96, :sz], pa[u][0:96, :sz], cc[0:96, a0:a0 + sz], ALU.mult,
                          r=["pa0", "cc"], w=[f"t1{u}"])
                    mk.tt("dve", t2[u][0:96, :sz], pb_[u][0:96, :sz], ss[0:96, a0:a0 + sz], ALU.mult,
                          r=["pb0", "ss"], w=[f"t2{u}"])
                    mk.tt("pool", QT[hp][0:96, a0:a0 + sz], t1[u][0:96, :sz], t2[u][0:96, :sz], ALU.add,
                          r=[f"t1{u}", f"t2{u}"], w=[f"QT{hp}_{a0}"])
                    for kc in range(2):
                        mk.mm(pb_[u][0:64, :sz], wkv[:, kc, h * 64:(h + 1) * 64], akv[:, kc, a0:a0 + sz], kc == 0, kc == 1,
                              r=["wkv", "akv"], w=["pb0"])
                    mk.cp("dve", KT[hp][0:64, a0:a0 + sz], pb_[u][0:64, :sz], r=["pb0"], w=[f"KT{hp}_{a0}"])
                mk.cp("pool", KT[hp][64:96, :], kr[64:96, :], r=["kr"], w=[f"KTr{hp}"])

            units = []
            for h in range(8):
                for qi_, (a0, sz) in enumerate(seq_tiles):
                    nkb = 2 if a0 == 0 else NC_
                    for kb in range(nkb):
                        units.append((h, qi_, a0, sz, kb, nkb))

            def emit_qk(i):
                h, qi_, a0, sz, kb, nkb = units[i]
                hp = h % 2
                v = i % 4
                ktk = [f"KT{hp}_{a0_}" for (a0_, sz_) in seq_tiles] + [f"KTr{hp}"]
                mk.mm(psc[v][:, :sz], KT[hp][0:96, kb * 128:(kb + 1) * 128], QT[hp][0:96, a0:a0 + sz], True, True,
                      r=ktk + [f"QT{hp}_{a0}"], w=[f"psc{v}"])

            LA = 3
            proj(0)
            proj(1)
            for j in range(LA):
                emit_qk(j)
            for i, (h, qi_, a0, sz, kb, nkb) in enumerate(units):
                if qi_ == 1 and kb == 0 and 1 <= h and h + 1 < 8:
                    proj(h + 1)
                v = i % 4
                v3 = i % 4
                u = (h * len(seq_tiles) + qi_) % 2
                if i + LA < len(units):
                    emit_qk(i + LA)
                mk.act(pT[v3][:, :sz], psc[v][:, :sz], AF.Exp, scale=ATT_SCALE, r=[f"psc{v}"], w=[f"pT{v3}"])
                mk.mm(pac[u][:, :sz], VA[:, kb, h, :], pT[v3][:, :sz], kb == 0, kb == nkb - 1,
                      r=vak + [f"pT{v3}"], w=[f"pac{u}"])
                if kb == nkb - 1:
                    mk.recip(rec[u][:, :sz], pac[u][64:128, :sz], r=[f"pac{u}"], w=[f"rec{u}"])
                    po = (h % 2) * 64
                    mk.tt("dve", yb[po:po + 64, h // 2, a0:a0 + sz], pac[u][0:64, :sz], rec[u][:, :sz], ALU.mult,
                          r=[f"pac{u}", f"rec{u}"], w=[f"yb{h}_{a0}"])
            mk.dma("sp", out=YB[:, :, s0:s0 + L].rearrange("j p t -> p j t"), in_=yb[:],
                   r=[f"yb{h}_{a0}" for h in range(8) for (a0, sz) in seq_tiles])
            sg.close()

        def stage_gmlp(l):
            sg = Stage(mk, f"gm{l}")
            gv = [sg.sb(f"gv{i}", [128, 4, 512], BF16) for i in range(2)]
            gu = [sg.sb(f"gu{i}", [128, 4, 512], BF16) for i in range(2)]
            gsq = sg.sb("gsq", [128, 4, 512], F32)
            gss = [sg.sb(f"gss{i}", [128, 16], F32) for i in range(2)]
            gvn = [sg.sb(f"gvn{i}", [128, 4, 4, 128], BF16) for i in range(2)]
            gtmp = [sg.sb(f"gtmp{i}", [128, 4, 4, 128], F32) for i in range(2)]
            vnb = sg.sb("vnb", [128, 512], F32)
            bsb = sg.sb("bsb", [128, 512], F32)
            wsT = sg.sb("wsT", [128, 512], BF16)
            yc = [sg.sb(f"yc{i}", [128, 4, 512], BF16) for i in range(2)]
            pg = [sg.ps(f"pg{i}", [128, 2048], F32) for i in range(2)]
            mk.dma("sp", out=vnb[:], in_=rows[l, 0:1, R_VN:R_VN + 512].to_broadcast([128, 512]), w=["vnb"])
            mk.dma("sp", out=bsb[:], in_=rows[l, 0:1, R_BS:R_BS + 512].to_broadcast([128, 512]), w=["bsb"])
            mk.dma("pool", out=wsT[:], in_=gws_r[l], w=["wsT"])

            TL = [t_ for t_ in TILES if not (l == DEPTH - 1 and t_[2] == 2)]

            def ldg_(i):
                n0_, sz_, vi_, b_ = TL[i]
                p_ = i % 2
                mk.dma("sp", out=gv[p_][:, :sz_ // 128, :], in_=ZTGV[n0_:n0_ + sz_, :].rearrange("(b p) f -> p b f", p=128),
                       w=[f"gv{p_}"])
                mk.dma("sp", out=gu[p_][:, :, :sz_], in_=ZF[C_GU:C_GU + 4, :, n0_:n0_ + sz_].rearrange("j p t -> p j t"),
                       w=[f"gu{p_}"])
            ldg_(0)
            for i, (n0, sz, vi, b) in enumerate(TL):
                p = i % 2
                nb = sz // 128
                if i + 1 < len(TL):
                    ldg_(i + 1)
                GV = gv[p][:, :nb, :]
                mk.tt("dve", gsq[:, :nb, :], GV, GV, ALU.mult, r=[f"gv{p}"], w=["gsq"])
                mk.op("dve", lambda e: e.tensor_reduce(out=gss[p][:, :nb * 4],
                                                       in_=gsq[:, :nb, :].rearrange("p b (g c) -> p (b g) c", g=4),
                                                       axis=AX.X, op=ALU.add), r=["gsq"], w=[f"gss{p}"])
                mk.act(gss[p][:, :nb * 4], gss[p][:, :nb * 4], AF.Sqrt, bias=EPS, scale=1.0 / 128, r=[f"gss{p}"],
                       w=[f"gss{p}"])
                mk.recip(gss[p][:, :nb * 4], gss[p][:, :nb * 4], r=[f"gss{p}"], w=[f"gss{p}"])
                mk.tt("dve", gsq[:, :nb, :], GV, vnb[:].unsqueeze(1).to_broadcast([128, nb, 512]), ALU.mult,
                      r=[f"gv{p}", "vnb"], w=["gsq"])
                mk.tt("pool", gvn[p][:, :nb, :, :].rearrange("p b g c -> p (b g) c"),
                      gsq[:, :nb, :].rearrange("p b (g c) -> p (b g) c", g=4),
                      gss[p][:, :nb * 4].unsqueeze(2).to_broadcast([128, nb * 4, 128]), ALU.mult,
                      r=["gsq", f"gss{p}"], w=[f"gvn{p}"])
                for bb in range(nb):
                    for g in range(4):
                        mk.mm(pg[p][:, bb * 512 + g * 128:bb * 512 + (g + 1) * 128], gvn[p][:, bb, g, :],
                              wsT[:, g * 128:(g + 1) * 128], True, True, r=[f"gvn{p}", "wsT"], w=[f"pg{p}"])
                mk.tt("dve", gtmp[p][:, :nb, :, :].rearrange("p b g t -> p b (g t)"),
                      pg[p][:, :nb * 512].rearrange("p (b f) -> p b f", f=512),
                      bsb[:].unsqueeze(1).to_broadcast([128, nb, 512]), ALU.add, r=[f"pg{p}", "bsb"], w=[f"gtmp{p}"])
                mk.tt("pool", yc[p][:, :, :sz].rearrange("p g (b t) -> p g b t", t=128),
                      gtmp[p][:, :nb, :, :].rearrange("p b g t -> p g b t"),
                      gu[p][:, :, :sz].rearrange("p g (b t) -> p g b t", t=128), ALU.mult,
                      r=[f"gtmp{p}", f"gu{p}"], w=[f"yc{p}"])
                mk.dma("sp", out=YC[:, :, n0:n0 + sz].rearrange("j p t -> p j t"), in_=yc[p][:, :, :sz], r=[f"yc{p}"])
            sg.close()

        def stage_out(l):
            sg = Stage(mk, f"o{l}")
            wp = [sg.sb(f"wp{i}", [128, 4, D], BF16) for i in range(3)]
            wo = sg.sb("wo", [128, 8, D], BF16)
            yin = [[sg.sb(f"yin{i}{k}", [128, 4, 512], BF16) for k in range(3)] for i in range(2)]
            gts = [sg.sb(f"gts{i}", [128, 24, 512], BF16) for i in range(2)]
            xt = [sg.sb(f"xt{i}", [128, 8, 512], F32) for i in range(2)]
            ym = [sg.sb(f"ym{i}", [128, 8, 512], BF16) for i in range(2)]
            ta = [sg.sb(f"ta{i}", [128, 512], F32) for i in range(2)]
            tb = [sg.sb(f"tb{i}", [128, 512], F32) for i in range(2)]
            tc_ = [sg.sb(f"tc{i}", [128, 512], F32) for i in range(2)]
            pp = [[sg.ps(f"pp{i}{k}", [128, 512], F32) for k in range(3)] for i in range(2)]
            po = [sg.ps(f"po{i}", [128, 512], F32) for i in range(2)]
            for k, wsrc in enumerate((w_pa, w_pb, w_pc)):
                mk.dma("pool", out=wp[k][:], in_=wsrc[l].rearrange("(kc p) n -> p kc n", p=128), w=[f"wp{k}"])
            mk.dma("pool", out=wo[:], in_=w_out[l].rearrange("(kc p) n -> p kc n", p=128), w=["wo"])
            oi = [0]
            TL = [t_ for t_ in TILES if not (l == DEPTH - 1 and t_[2] == 2)]

            def loads(i):
                n0, sz, vi, b = TL[i]
                p = i % 2
                for k, src in enumerate((YA, YB, YC)):
                    mk.dma("sp", out=yin[p][k][:, :, :sz], in_=src[:, :, n0:n0 + sz].rearrange("j p t -> p j t"),
                           w=[f"yin{p}{k}"])
                mk.dma("sp", out=gts[p][:, :, :sz], in_=ZF[C_BRA:C_BRA + 24, :, n0:n0 + sz].rearrange("j p t -> p j t"),
                       w=[f"gts{p}"])
                mk.dma("sp", out=xt[p][:, :, :sz], in_=XT[:, :, n0:n0 + sz].rearrange("j p t -> p j t"), w=[f"xt{p}"])

            def merge(i):
                n0, sz, vi, b = TL[i]
                p = i % 2
                for oc in range(8):
                    u = oi[0] % 2
                    oi[0] += 1
                    for k in range(3):
                        for kc in range(4):
                            mk.mm(pp[u][k][:, :sz], wp[k][:, kc, oc * 128:(oc + 1) * 128], yin[p][k][:, kc, :sz],
                                  kc == 0, kc == 3, r=[f"wp{k}", f"yin{p}{k}"], w=[f"pp{u}{k}"])
                    mk.tt("dve", ta[u][:, :sz], pp[u][0][:, :sz], gts[p][:, oc, :sz], ALU.mult,
                          r=[f"pp{u}0", f"gts{p}"], w=[f"ta{u}"])
                    mk.tt("dve", tb[u][:, :sz], pp[u][1][:, :sz], gts[p][:, 8 + oc, :sz], ALU.mult,
                          r=[f"pp{u}1", f"gts{p}"], w=[f"tb{u}"])
                    mk.tt("dve", tc_[u][:, :sz], pp[u][2][:, :sz], gts[p][:, 16 + oc, :sz], ALU.mult,
                          r=[f"pp{u}2", f"gts{p}"], w=[f"tc{u}"])
                    mk.tt("pool", ta[u][:, :sz], ta[u][:, :sz], tb[u][:, :sz], ALU.add, r=[f"ta{u}", f"tb{u}"],
                          w=[f"ta{u}"])
                    mk.tt("pool", ym[p][:, oc, :sz], ta[u][:, :sz], tc_[u][:, :sz], ALU.add, r=[f"ta{u}", f"tc{u}"],
                          w=[f"ym{p}{oc}"])

            def outproj(i):
                n0, sz, vi, b = TL[i]
                p = i % 2
                for oc in range(8):
                    u = oc % 2
                    for kc in range(8):
                        mk.mm(po[u][:, :sz], wo[:, kc, oc * 128:(oc + 1) * 128], ym[p][:, kc, :sz], kc == 0, kc == 7,
                              r=["wo", f"ym{p}{kc}"], w=[f"po{u}"])
                    mk.stt(xt[p][:, oc, :sz], po[u][:, :sz], MOD[:, l, 16 + oc, vi:vi + 1], xt[p][:, oc, :sz],
                           ALU.mult, ALU.add, r=[f"po{u}", f"xt{p}"], w=[f"xt{p}"])
                mk.dma("sp", out=XT[:, :, n0:n0 + sz].rearrange("j p t -> p j t"), in_=xt[p][:, :, :sz], r=[f"xt{p}"])

            nT = len(TL)
            loads(0)
            merge(0)
            for i in range(nT):
                if i + 1 < nT:
                    loads(i + 1)
                    merge(i + 1)
                outproj(i)
            sg.close()

        def stage_moe(l, b):
            sg = Stage(mk, f"moe{l}{b}")
            NC_ = L // 128
            s0 = b * L
            seq_tiles = ([] if l == DEPTH - 1 else [(0, LC)]) + [(LC + i * 512, 512) for i in range(4)]
            acc = sg.sb("acc", [128, 8, L], F32)
            h2 = sg.sb("h2", [128, 8, L], BF16)
            cmf = sg.sb("cmf", [128, NC_, 16], F32)
            cmb = sg.sb("cmb", [128, NC_, 16], BF16)
            w1 = [sg.sb(f"w1{i}", [128, 8, 512], BF16) for i in range(2)]
            w3 = [sg.sb(f"w3{i}", [128, 8, 512], BF16) for i in range(2)]
            w2 = [sg.sb(f"w2{i}", [128, 4, D], BF16) for i in range(2)]
            cbs = [sg.sb(f"cbs{i}", [128, 512], F32) for i in range(2)]
            s1 = [sg.sb(f"s1{i}", [128, 512], F32) for i in range(2)]
            tm = [sg.sb(f"tm{i}", [128, 512], F32) for i in range(2)]
            hid = [sg.sb(f"hid{i}", [128, 4, 512], BF16) for i in range(2)]
            pcb = sg.ps("pcb", [128, 512], F32)
            p1 = [sg.ps(f"p1{i}", [128, 512], F32) for i in range(2)]
            p3 = [sg.ps(f"p3{i}", [128, 512], F32) for i in range(2)]
            po = [sg.ps(f"po{i}", [128, 512], F32) for i in range(3)]
            for (a0, sz) in seq_tiles:
                mk.dma("sp", out=h2[:, :, a0:a0 + sz], in_=HT[:, :, s0 + a0:s0 + a0 + sz].rearrange("j p t -> p j t"),
                       w=[f"h2_{a0}"])
            mk.dma("sp", out=cmf[:], in_=COMB[s0:s0 + L, :].rearrange("(c p) e -> p c e", p=128), w=["cmf"])
            mk.cp("dve", cmb[:], cmf[:], r=["cmf"], w=["cmb"])
            items = [(e, ti_, a0, sz) for e in range(16) for ti_, (a0, sz) in enumerate(seq_tiles)]

            def load_w(e):
                p = e % 2
                mk.dma("pool", out=w1[p][:], in_=e_w1[l, e].rearrange("(kc p) n -> p kc n", p=128), w=[f"w1{p}"])
                mk.dma("pool", out=w3[p][:], in_=e_w3[l, e].rearrange("(kc p) n -> p kc n", p=128), w=[f"w3{p}"])
                mk.dma("pool", out=w2[p][:], in_=e_w2[l, e].rearrange("(kc p) n -> p kc n", p=128), w=[f"w2{p}"])

            def phase_a(i):
                e, ti_, a0, sz = items[i]
                p = e % 2
                t = i % 2
                nb = sz // 128
                for bb in range(nb):
                    cblk = a0 // 128 + bb
                    mk.mm(pcb[:, bb * 128:(bb + 1) * 128], cmb[:, cblk, e:e + 1].to_broadcast([128, 128]),
                          ident_b[:], True, True, r=["cmb", "ident_b"], w=["pcb"])
                mk.cp("act", cbs[t][:, :sz], pcb[:, :sz], r=["pcb"], w=[f"cbs{t}"])
                for jc in range(4):
                    u = jc % 2
                    for kc in range(8):
                        mk.mm(p1[u][:, :sz], w1[p][:, kc, jc * 128:(jc + 1) * 128], h2[:, kc, a0:a0 + sz],
                              kc == 0, kc == 7, r=[f"w1{p}", f"h2_{a0}"], w=[f"p1{u}"])
                    for kc in range(8):
                        mk.mm(p3[u][:, :sz], w3[p][:, kc, jc * 128:(jc + 1) * 128], h2[:, kc, a0:a0 + sz],
                              kc == 0, kc == 7, r=[f"w3{p}", f"h2_{a0}"], w=[f"p3{u}"])
                    mk.act(s1[u][:, :sz], p1[u][:, :sz], AF.Silu, r=[f"p1{u}"], w=[f"s1{u}"])
                    mk.tt("dve", tm[u][:, :sz], p3[u][:, :sz], s1[u][:, :sz], ALU.mult, r=[f"p3{u}", f"s1{u}"],
                          w=[f"tm{u}"])
                    mk.tt("pool", hid[t][:, jc, :sz], tm[u][:, :sz], cbs[t][:, :sz], ALU.mult,
                          r=[f"tm{u}", f"cbs{t}"], w=[f"hid{t}_{jc}"])

            def phase_b(i):
                e, ti_, a0, sz = items[i]
                p = e % 2
                t = i % 2
                for oc in range(8):
                    u = (i * 8 + oc) % 3
                    for jc in range(4):
                        mk.mm(po[u][:, :sz], w2[p][:, jc, oc * 128:(oc + 1) * 128], hid[t][:, jc, :sz],
                              jc == 0, jc == 3, r=[f"w2{p}", f"hid{t}_{jc}"], w=[f"po{u}"])
                    if e == 0:
                        mk.cp("dve", acc[:, oc, a0:a0 + sz], po[u][:, :sz], r=[f"po{u}"], w=[f"acc{ti_}_{oc}"])
                    else:
                        mk.tt("dve", acc[:, oc, a0:a0 + sz], po[u][:, :sz], acc[:, oc, a0:a0 + sz], ALU.add,
                              r=[f"po{u}", f"acc{ti_}_{oc}"], w=[f"acc{ti_}_{oc}"])

            load_w(0)
            load_w(1)
            nt_ = len(seq_tiles)
            for i in range(len(items) + 1):
                if i < len(items):
                    phase_a(i)
                if i >= 1:
                    phase_b(i - 1)
                    e_prev, ti_prev = items[i - 1][0], items[i - 1][1]
                    if ti_prev == nt_ - 1 and e_prev + 2 < 16:
                        load_w(e_prev + 2)
            xtv = h2[:].rearrange("p j t -> p (j t)").bitcast(F32)
            h2keys = [f"h2_{a0_}" for (a0_, sz_) in seq_tiles]

            def xt_(i):
                return xtv[:, (i % 2) * 4096:(i % 2 + 1) * 4096].rearrange("p (j t) -> p j t", j=8)

            def ldx_(i):
                a0_, sz_ = seq_tiles[i]
                mk.dma("sp", out=xt_(i)[:, :, :sz_], in_=XT[:, :, s0 + a0_:s0 + a0_ + sz_].rearrange("j p t -> p j t"),
                       w=[f"xr{i % 2}"] + (h2keys if i < 2 else []))
            ldx_(0)
            for ti_, (a0, sz) in enumerate(seq_tiles):
                vi = 2 if a0 == 0 else b
                if ti_ + 1 < len(seq_tiles):
                    ldx_(ti_ + 1)
                xk = f"xr{ti_ % 2}"
                for oc in range(8):
                    mk.stt(xt_(ti_)[:, oc, :sz], acc[:, oc, a0:a0 + sz], MOD[:, l, 40 + oc, vi:vi + 1], xt_(ti_)[:, oc, :sz],
                           ALU.mult, ALU.add, r=[f"acc{ti_}_{oc}", xk], w=[xk])
                mk.dma("sp", out=XT[:, :, s0 + a0:s0 + a0 + sz].rearrange("j p t -> p j t"), in_=xt_(ti_)[:, :, :sz],
                       r=[xk])
            sg.close()

        def stage_final():
            sg = Stage(mk, "fin")
            xt = [sg.sb(f"xt{i}", [128, 8, 512], F32) for i in range(2)]
            sq = [sg.sb(f"sq{i}", [128, 8, 512], BF16) for i in range(2)]
            rt = [sg.sb(f"rt{i}", [128, 512], F32) for i in range(2)]
            ot = [sg.sb(f"ot{i}", [128, D], F32) for i in range(2)]
            pss = [sg.ps(f"pss{i}", [128, 512], F32) for i in range(2)]
            pt = [[sg.ps(f"pt{i}{h}", [128, 512], F32) for h in range(2)] for i in range(2)]
            bi = 0
            lat = [i for i, tl_ in enumerate(TILES) if tl_[2] != 2]

            def ldf_(li_):
                n0_, sz_, vi_, b_ = TILES[lat[li_]]
                mk.dma("sp", out=xt[li_ % 2][:, :, :sz_], in_=XT[:, :, n0_:n0_ + sz_].rearrange("j p t -> p j t"),
                       w=[f"xt{li_ % 2}"])
            ldf_(0)
            for li, i in enumerate(lat):
                n0, sz, vi, b = TILES[i]
                p = li % 2
                if li + 1 < len(lat):
                    ldf_(li + 1)
                mk.act(sq[p][:, :, :sz], xt[p][:, :, :sz], AF.Square, r=[f"xt{p}"], w=[f"sq{p}"])
                for j in range(8):
                    mk.mm(pss[p][:, :sz], ones_b[:], sq[p][:, j, :sz], j == 0, j == 7, r=[f"sq{p}"], w=[f"pss{p}"])
                mk.act(rt[p][:, :sz], pss[p][:, :sz], AF.Sqrt, bias=EPS, scale=1.0 / D, r=[f"pss{p}"], w=[f"rt{p}"])
                mk.recip(rt[p][:, :sz], rt[p][:, :sz], r=[f"rt{p}"], w=[f"rt{p}"])
                for j in range(8):
                    mk.stt(xt[p][:, j, :sz], xt[p][:, j, :sz], VEC[:, 0, V_FN + j:V_FN + j + 1], rt[p][:, :sz],
                           ALU.mult, ALU.mult, r=[f"xt{p}", f"rt{p}"], w=[f"xt{p}"])
                for bb in range(sz // 128):
                    q = bi % 2
                    bi += 1
                    for h in range(2):
                        for jj in range(4):
                            j = h * 4 + jj
                            mk.tr(pt[q][h][:, jj * 128:(jj + 1) * 128], xt[p][:, j, bb * 128:(bb + 1) * 128], ident_f[:],
                                  r=[f"xt{p}"], w=[f"pt{q}{h}"])
                        mk.cp("dve" if h == 0 else "act", ot[q][:, h * 512:(h + 1) * 512], pt[q][h][:],
                              r=[f"pt{q}{h}"], w=[f"ot{q}{h}"])
                    tpos = n0 - b * L - LC + bb * 128
                    mk.dma("sp", out=out[b, tpos:tpos + 128, :], in_=ot[q][:], r=[f"ot{q}0", f"ot{q}1"])
            sg.close()

        def program():
            stage_load_x()
            stage_ada()
            if check_stop(-1, "ada"):
                return
            for l in range(nlayers):
                stage_norm(l, 0, False)
                if check_stop(l, "norm1"):
                    return
                stage_z(l)
                if check_stop(l, "z"):
                    return
                for b in range(NB):
                    stage_mlstm(l, b)
                if check_stop(l, "mlstm"):
                    return
                for b in range(NB):
                    stage_mla(l, b)
                if check_stop(l, "mla"):
                    return
                stage_gmlp(l)
                if check_stop(l, "gmlp"):
                    return
                stage_out(l)
                if check_stop(l, "out"):
                    return
                stage_norm(l, 1, True)
                if check_stop(l, "norm2"):
                    return
                for b in range(NB):
                    stage_moe(l, b)
                if check_stop(l, "moe"):
                    return
            stage_final()

        program()
        mk.finish()
        build.last_ninst = mk.ninst
    return nc


def _fm(v, nchunks):
    return np.ascontiguousarray(v.reshape(nchunks, 128).T)


def prep_shared(inp):
    f32 = np.float32
    vecs = np.zeros((DEPTH, 128, NV), f32)
    rows = np.zeros((DEPTH, 1, NR), f32)
    for l in range(DEPTH):
        vecs[l, :, V_N1:V_N1 + 8] = _fm(inp["norm1"][l], 8)
        vecs[l, :, V_N2:V_N2 + 8] = _fm(inp["norm2"][l], 8)
        vecs[l, :, V_BADA:V_BADA + 48] = _fm(inp["b_ada"][l], 48)
        cw = inp["m_conv"][l]
        for tap in range(3):
            vecs[l, :, V_CONV + tap:V_CONV + 24:3] = _fm(cw[tap], 8)
        vecs[l, :, V_MNORM:V_MNORM + 4] = _fm(inp["m_norm"][l], 4)
        vecs[l, :, V_QN:V_QN + 3] = _fm(inp["a_qnorm"][l], 3)
        vecs[l, :, V_KVN:V_KVN + 2] = _fm(inp["a_kvnorm"][l], 2)
        vecs[l, :, V_FN:V_FN + 8] = _fm(inp["final_norm"], 8)
        rows[l, 0, R_GB:R_GB + 16] = inp["m_gate_b"][l]
        rows[l, 0, R_VN:R_VN + 512] = inp["g_vnorm"][l]
        rows[l, 0, R_RB:R_RB + 4] = inp["r_group_b"][l]
        rows[l, 0, R_RB + 4:R_RB + 20] = inp["r_expert_b"][l]
        rows[l, 0, R_BS:R_BS + 512] = inp["g_bs"][l].reshape(512)
    off = np.cumsum([0, 512, 512, 512, 512, 16, 384, 256, 32, 512, 512, 1024, 1024, 1024])
    o_mq, o_mk, o_mv, o_mo, o_mg, o_aq, o_akv, o_akr, o_gu, o_gv, o_bra, o_brb, o_brc = off[:13]
    ar = np.arange
    akr = o_akr + ar(32)
    akr_sw = o_akr + np.concatenate([ar(16, 32), ar(0, 16)])
    padA = np.concatenate([np.tile(akr, 2), akr, akr])
    padB = np.concatenate([np.tile(akr, 2), akr_sw, akr])
    idx = np.concatenate([o_mq + ar(512), o_mk + ar(512), o_mo + ar(512), o_aq + ar(384), o_akv + ar(256), padA, padB,
                          o_gu + ar(512), o_bra + ar(1024), o_brb + ar(1024), o_brc + ar(1024),
                          o_mv + ar(512), o_mg + ar(16), o_gv + ar(512)])
    assert idx.shape[0] == NFM * 128 + NTMC
    w_in_r = np.ascontiguousarray(inp["w_in"][:, :, idx])
    sw = np.concatenate([h * 96 + np.concatenate([ar(64), 64 + ar(16, 32), 64 + ar(0, 16)]) for h in range(8)])
    wuq_r = np.ascontiguousarray(np.concatenate([inp["a_wuq"], inp["a_wuq"][:, :, sw]], axis=2))
    kidx = np.concatenate([h * 128 + ar(64) for h in range(8)])
    vidx = np.concatenate([h * 128 + 64 + ar(64) for h in range(8)])
    wukv_r = np.ascontiguousarray(inp["a_wukv"][:, :, np.concatenate([kidx, vidx])])
    gws_r = np.ascontiguousarray(inp["g_ws"].transpose(0, 3, 1, 2).reshape(DEPTH, 128, 512))
    rwc = np.ascontiguousarray(np.concatenate([inp["r_group"], inp["r_expert"]], axis=2))
    s_ = np.arange(128)
    ident = np.eye(128, dtype=f32)
    trif = (s_[:, None] <= s_[None, :]).astype(f32)
    trib = (s_[:, None] >= s_[None, :]).astype(f32)
    half = 16
    r_ = np.repeat(np.arange(T // 64, dtype=f32), 64)
    col = np.tile(np.arange(64, dtype=f32), T // 64)
    inv = (np.float32(10000.0) ** (-np.arange(0, half, 2, dtype=f32) / np.float32(half))).astype(f32)
    ang = np.concatenate([r_[:, None] * inv, col[:, None] * inv], axis=-1).astype(f32)
    cos, sin = np.cos(ang).astype(f32), np.sin(ang).astype(f32)
    cc = np.ones((128, L), f32)
    ss = np.zeros((128, L), f32)
    cc[64:80, LC:] = cos.T
    cc[80:96, LC:] = cos.T
    ss[64:80, LC:] = -sin.T
    ss[80:96, LC:] = sin.T
    sh = dict(vecs=vecs, rows=rows, w_ada=np.ascontiguousarray(inp["w_ada"]), w_in_r=w_in_r, wuq_r=wuq_r, wukv_r=wukv_r,
              gws_r=gws_r, w_pa=np.ascontiguousarray(inp["w_pa"]), w_pb=np.ascontiguousarray(inp["w_pb"]),
              w_pc=np.ascontiguousarray(inp["w_pc"]), w_out=np.ascontiguousarray(inp["w_out"]), rw=rwc,
              e_w1=np.ascontiguousarray(inp["e_w1"]), e_w3=np.ascontiguousarray(inp["e_w3"]),
              e_w2=np.ascontiguousarray(inp["e_w2"]), c_ident=ident, c_trif=trif, c_trib=trib,
              c_maskf=(trif * np.float32(QS)).astype(f32), c_maskb=(trib * np.float32(QS)).astype(f32), c_cc=cc, c_ss=ss)
    return sh


def prep_core(inp, core):
    f32 = np.float32
    bs = [core * NB + i for i in range(NB)]
    xin = np.concatenate([np.concatenate([inp["ctx"][b], inp["x"][b]], axis=0) for b in bs], axis=0).astype(f32)
    vs = [inp["c"][bs[0]], inp["c"][bs[1]], inp["c_ctx"], inp["c_ctx"]]
    cv = np.stack([_fm(v, 8) for v in vs], axis=-1).astype(f32)
    return dict(xin=np.ascontiguousarray(xin), cv=np.ascontiguousarray(cv))


def kernel(**inputs):
    inp = {k: np.asarray(v) for k, v in inputs.items()}
    sh = prep_shared(inp)
    nc = build()
    in_maps = []
    for core in range(NCORES):
        m = dict(sh)
        m.update(prep_core(inp, core))
        in_maps.append(m)
    res = run_bass_kernel_spmd(nc, in_maps, core_ids=list(range(NCORES)))
    outs = [np.asarray(r["out"]) for r in res.results]
    return np.concatenate(outs, axis=0).astype(np.float32)
```

```python
import numpy as np
from contextlib import ExitStack
import concourse.bass as bass
import concourse.mybir as mybir
from concourse.bass_utils import run_bass_kernel_spmd

F32 = mybir.dt.float32
BF16 = mybir.dt.bfloat16
AF = mybir.ActivationFunctionType
ALU = mybir.AluOpType
AX = mybir.AxisListType

NCORES = 8
NB = 2
LC = 256
T = 2048
L = LC + T
S = NB * L
D = 1024
DEPTH = 4
EPS = 1e-6
NFM = 47
NTMC = 1040
NV = 112
NR = 16 + 512 + 20 + 512
ATT_SCALE = 96 ** -0.5
QS = 128 ** -0.5
NDS = 12
SAME_ENGINE_SYNC = True

C_MQ, C_MK, C_MO, C_AQ, C_AKV, C_KRA, C_KRB, C_GU, C_BRA, C_BRB, C_BRC = 0, 4, 8, 12, 15, 17, 18, 19, 23, 31, 39
V_N1, V_N2, V_BADA, V_CONV, V_MNORM, V_QN, V_KVN, V_FN = 0, 8, 16, 64, 88, 92, 95, 97
R_GB, R_VN, R_RB, R_BS = 0, 16, 528, 548


def token_tiles():
    tl = []
    for b in range(NB):
        tl.append((b * L, LC, 2, b))
        for i in range(T // 512):
            tl.append((b * L + LC + i * 512, 512, b, b))
    return tl


class MK:
    def __init__(self, nc, st):
        self.nc = nc
        self.E = {"pe": nc.tensor, "act": nc.scalar, "dve": nc.vector, "pool": nc.gpsimd, "sp": nc.sync}
        self.sem = {e: st.enter_context(nc.semaphore("s_" + e)) for e in self.E}
        self.cnt = {e: 0 for e in self.E}
        self.dq = ("sp", "act", "pool")
        self.dsem = {q: [st.enter_context(nc.semaphore(f"d_{q}{i}")) for i in range(NDS)] for q in self.dq}
        self.dcnt = {q: [0] * NDS for q in self.dq}
        self.dnext = {q: 0 for q in self.dq}
        self.waited = {}
        self.W = {}
        self.R = {}
        self.pending = {e: {} for e in self.E}
        self.ninst = 0

    def _semh(self, sk):
        return self.sem[sk[1]] if sk[0] == "e" else self.dsem[sk[1]][sk[2]]

    def _wait(self, eng, sk, val):
        if val <= 0:
            return
        if sk == ("e", eng) and (eng == "pe" or not SAME_ENGINE_SYNC):
            return
        k = (eng, sk)
        if self.waited.get(k, 0) >= val:
            return
        self.waited[k] = val
        self.E[eng].wait_ge(self._semh(sk), val)
        self.ninst += 1

    def _pre(self, eng, r, w):
        pend = self.pending[eng]
        if pend:
            for sk, v in pend.items():
                self._wait(eng, sk, v)
            self.pending[eng] = {}
        for k in r:
            for sk, v in self.W.get(k, {}).items():
                self._wait(eng, sk, v)
        for k in w:
            for sk, v in self.W.get(k, {}).items():
                self._wait(eng, sk, v)
            for sk, v in self.R.get(k, {}).items():
                self._wait(eng, sk, v)

    def _post(self, sk, val, r, w):
        for k in r:
            d = self.R.setdefault(k, {})
            d[sk] = max(d.get(sk, 0), val)
        for k in w:
            self.W[k] = {sk: val}
            self.R[k] = {}

    def op(self, eng, fn, r=(), w=()):
        self._pre(eng, r, w)
        ins = fn(self.E[eng])
        self.cnt[eng] += 1
        ins.then_inc(self.sem[eng], 1)
        self.ninst += 1
        self._post(("e", eng), self.cnt[eng], r, w)

    def dma(self, q, out, in_, r=(), w=(), **kw):
        i = self.dnext[q]
        self.dnext[q] = (i + 1) % NDS
        sk = ("d", q, i)
        self._wait(q, sk, self.dcnt[q][i])
        self._pre(q, r, w)
        ins = self.E[q].dma_start(out=out, in_=in_, **kw)
        self.dcnt[q][i] += 16
        ins.then_inc(self.dsem[q][i], 16)
        self.ninst += 1
        self._post(sk, self.dcnt[q][i], r, w)

    def barrier(self):
        snap = {}
        for e in self.E:
            snap[("e", e)] = self.cnt[e]
        for q in self.dq:
            for i in range(NDS):
                snap[("d", q, i)] = self.dcnt[q][i]
        for e in self.E:
            self.pending[e] = dict(snap)
        self.W = {}
        self.R = {}

    def finish(self):
        self.barrier()
        for e in self.E:
            for sk, v in self.pending[e].items():
                if sk == ("e", e):
                    continue
                k = (e, sk)
                if self.waited.get(k, 0) >= v or v <= 0:
                    continue
                self.waited[k] = v
                self.E[e].wait_ge(self._semh(sk), v)
            self.pending[e] = {}

    def mm(self, out, lhsT, rhs, start, stop, r=(), w=()):
        self.op("pe", lambda e: e.matmul(out, lhsT=lhsT, rhs=rhs, start=start, stop=stop), r=r, w=w)

    def tr(self, out, in_, ident, r=(), w=()):
        self.op("pe", lambda e: e.transpose(out, in_, ident), r=r, w=w)

    def act(self, out, in_, func, r=(), w=(), bias=None, scale=None, eng="act"):
        kw = {}
        if bias is not None:
            kw["bias"] = bias
        if scale is not None:
            kw["scale"] = scale
        self.op(eng, lambda e: e.activation(out=out, in_=in_, func=func, **kw), r=r, w=w)

    def tt(self, eng, out, in0, in1, op, r=(), w=()):
        self.op(eng, lambda e: e.tensor_tensor(out=out, in0=in0, in1=in1, op=op), r=r, w=w)

    def ts(self, eng, out, in0, s1, op0, s2=None, op1=None, r=(), w=()):
        kw = dict(out=out, in0=in0, scalar1=s1, scalar2=s2, op0=op0)
        if op1 is not None:
            kw["op1"] = op1
        self.op(eng, lambda e: e.tensor_scalar(**kw), r=r, w=w)

    def stt(self, out, in0, scalar, in1, op0, op1, r=(), w=(), eng="dve"):
        self.op(eng, lambda e: e.scalar_tensor_tensor(out=out, in0=in0, scalar=scalar, in1=in1, op0=op0, op1=op1),
                r=r, w=w)

    def cp(self, eng, out, in_, r=(), w=()):
        if eng == "act":
            self.op(eng, lambda e: e.copy(out=out, in_=in_), r=r, w=w)
        else:
            self.op(eng, lambda e: e.tensor_copy(out=out, in_=in_), r=r, w=w)

    def memset(self, eng, ap, val, w=()):
        self.op(eng, lambda e: e.memset(ap, val), w=w)

    def recip(self, out, in_, r=(), w=()):
        self.op("dve", lambda e: e.reciprocal(out=out, in_=in_), r=r, w=w)


class Stage:
    def __init__(self, mk, name):
        self.mk = mk
        self.name = name
        self.es = ExitStack()

    def sb(self, nm, shape, dt):
        return self.es.enter_context(self.mk.nc.sbuf_tensor(f"{self.name}_{nm}", list(shape), dt))

    def ps(self, nm, shape, dt):
        return self.es.enter_context(self.mk.nc.psum_tensor(f"{self.name}_{nm}", list(shape), dt))

    def close(self):
        self.mk.barrier()
        self.es.close()


def build(nlayers=DEPTH, dbg=(), stop=None):
    nc = bass.Bass("TRN2", target_bir_lowering=False)
    dbg = set(dbg)

    def din(name, shape, dt=F32):
        return nc.dram_tensor(name, list(shape), dt, kind="ExternalInput").ap()

    def dscr(name, shape, dt):
        kind = "ExternalOutput" if name in dbg else "Internal"
        return nc.dram_tensor(name, list(shape), dt, kind=kind).ap()

    xin = din("xin", [S, D])
    cv = din("cv", [128, 8, 4])
    vecs = din("vecs", [DEPTH, 128, NV])
    rows = din("rows", [DEPTH, 1, NR])
    w_ada = din("w_ada", [DEPTH, D, 6 * D])
    w_in_r = din("w_in_r", [DEPTH, D, NFM * 128 + NTMC])
    wuq_r = din("wuq_r", [DEPTH, 384, 1536])
    wukv_r = din("wukv_r", [DEPTH, 256, 1024])
    gws_r = din("gws_r", [DEPTH, 128, 512])
    w_pa = din("w_pa", [DEPTH, 512, D])
    w_pb = din("w_pb", [DEPTH, 512, D])
    w_pc = din("w_pc", [DEPTH, 512, D])
    w_out = din("w_out", [DEPTH, D, D])
    rw = din("rw", [DEPTH, D, 20])
    e_w1 = din("e_w1", [DEPTH, 16, D, 512])
    e_w3 = din("e_w3", [DEPTH, 16, D, 512])
    e_w2 = din("e_w2", [DEPTH, 16, 512, D])
    c_ident = din("c_ident", [128, 128])
    c_trif = din("c_trif", [128, 128])
    c_trib = din("c_trib", [128, 128])
    c_maskf = din("c_maskf", [128, 128])
    c_maskb = din("c_maskb", [128, 128])
    c_cc = din("c_cc", [128, L])
    c_ss = din("c_ss", [128, L])
    out = nc.dram_tensor("out", [NB, T, D], F32, kind="ExternalOutput").ap()

    XT = dscr("XT", [8, 128, S], F32)
    HT = dscr("HT", [8, 128, S], BF16)
    ZF = dscr("ZF", [NFM, 128, S], BF16)
    ZTV = dscr("ZTV", [S, 512], BF16)
    ZTG = dscr("ZTG", [S, 16], F32)
    ZTGV = dscr("ZTGV", [S, 512], BF16)
    YA = dscr("YA", [4, 128, S], BF16)
    YB = dscr("YB", [4, 128, S], BF16)
    YC = dscr("YC", [4, 128, S], BF16)
    COMB = dscr("COMB", [S, 16], F32)
    MODD = dscr("MODD", [128, DEPTH * 48 * 4], F32)

    TILES = token_tiles()

    with ExitStack() as st:
        mk = MK(nc, st)
        gsb = lambda nm, shape, dt: st.enter_context(nc.sbuf_tensor(nm, list(shape), dt))
        ident_f = gsb("ident_f", [128, 128], F32)
        ident_b = gsb("ident_b", [128, 128], BF16)
        ones_b = gsb("ones_b", [128, 128], BF16)
        ones_f = gsb("ones_f", [128, 128], F32)
        VEC = gsb("VEC", [128, DEPTH, NV], F32)
        MOD = gsb("MOD", [128, DEPTH, 48, 4], F32)
        AB = gsb("AB", [128, DEPTH, 2, 4, 8], F32)

        mk.dma("sp", out=ident_f[:], in_=c_ident[:, :], w=["ident_f"])
        mk.dma("pool", out=ident_b[:], in_=c_ident[:, :], w=["ident_b"])
        mk.memset("dve", ones_b[:], 1.0, w=["ones_b"])
        mk.memset("dve", ones_f[:], 1.0, w=["ones_f"])
        mk.dma("sp", out=VEC[:], in_=vecs.rearrange("l p v -> p l v"), w=["VEC"])
        mk.barrier()

        def check_stop(l, name):
            return stop is not None and stop == (l, name)

        def stage_load_x():
            sg = Stage(mk, "ldx")
            xi = [sg.sb(f"xi{i}", [128, D], F32) for i in range(2)]
            xo = [sg.sb(f"xo{i}", [128, 8, 128], F32) for i in range(2)]
            pt = [[sg.ps(f"pt{i}{h}", [128, 512], F32) for h in range(2)] for i in range(2)]
            mk.dma("sp", out=xi[0][:], in_=xin[0:128, :], w=["xi0"])
            for blk in range(S // 128):
                p = blk % 2
                if blk + 1 < S // 128:
                    mk.dma("sp", out=xi[(blk + 1) % 2][:], in_=xin[(blk + 1) * 128:(blk + 2) * 128, :],
                           w=[f"xi{(blk + 1) % 2}"])
                for h in range(2):
                    for jj in range(4):
                        j = h * 4 + jj
                        mk.tr(pt[p][h][:, jj * 128:(jj + 1) * 128], xi[p][:, j * 128:(j + 1) * 128], ident_f[:],
                              r=[f"xi{p}"], w=[f"pt{p}{h}"])
                    mk.cp("dve" if h == 0 else "act", xo[p][:, h * 4:(h + 1) * 4, :],
                          pt[p][h][:].rearrange("p (j t) -> p j t", j=4), r=[f"pt{p}{h}"], w=[f"xo{p}{h}"])
                mk.dma("sp", out=XT[:, :, blk * 128:(blk + 1) * 128].rearrange("j p t -> p j t"), in_=xo[p][:],
                       r=[f"xo{p}0", f"xo{p}1"])
            sg.close()

        def stage_ada():
            sg = Stage(mk, "ada")
            cvt = sg.sb("cvt", [128, 8, 4], F32)
            sct = sg.sb("sct", [128, 8, 4], F32)
            wb = [sg.sb(f"wb{i}", [128, 8, 1024], F32) for i in range(2)]
            pp = [sg.ps(f"pp{i}", [128, 512], F32) for i in range(2)]
            mk.dma("sp", out=cvt[:], in_=cv[:, :, :], w=["cvt"])
            mk.act(sct[:], cvt[:], AF.Silu, r=["cvt"], w=["sct"])
            it = 0
            for l in range(nlayers):
                for g in range(6):
                    p = it % 2
                    it += 1
                    mk.dma("sp", out=wb[p][:],
                           in_=w_ada[l, :, g * 1024:(g + 1) * 1024].rearrange("(kc p) n -> p kc n", p=128),
                           w=[f"wb{p}"])
                    for j in range(8):
                        q = j % 2
                        for kc in range(8):
                            mk.mm(pp[q][:, 0:4], wb[p][:, kc, j * 128:(j + 1) * 128], sct[:, kc, :], kc == 0, kc == 7,
                                  r=[f"wb{p}", "sct"], w=[f"pp{q}"])
                        col = V_BADA + g * 8 + j
                        mk.ts("dve", MOD[:, l, g * 8 + j, :], pp[q][:, 0:4], VEC[:, l, col:col + 1], ALU.add,
                              r=[f"pp{q}"], w=["MOD"])
                for which, scc, nv in ((0, 8, V_N1), (1, 32, V_N2)):
                    for vi in range(3):
                        mk.stt(AB[:, l, which, vi, :], MOD[:, l, scc:scc + 8, vi], 1.0, VEC[:, l, nv:nv + 8],
                               ALU.add, ALU.mult, r=["MOD"], w=["AB"])
            if "MODD" in dbg:
                mk.dma("sp", out=MODD[:, :], in_=MOD[:].rearrange("p l c v -> p (l c v)"), r=["MOD"])
            sg.close()

        def stage_norm(l, which, router):
            sg = Stage(mk, f"nm{l}{which}")
            shc = 0 if which == 0 else 24
            xt = [sg.sb(f"xt{i}", [128, 8, 512], F32) for i in range(2)]
            sq = [sg.sb(f"sq{i}", [128, 8, 512], BF16) for i in range(2)]
            rt = [sg.sb(f"rt{i}", [128, 512], F32) for i in range(2)]
            hb = [sg.sb(f"hb{i}", [128, 8, 512], BF16) for i in range(2)]
            pss = [sg.ps(f"pss{i}", [128, 512], F32) for i in range(2)]
            if router:
                hf = [sg.sb(f"hf{i}", [128, 8, 512], F32) for i in range(2)]
                rwt = sg.sb("rwt", [128, 8, 20], F32)
                rbb = sg.sb("rbb", [128, 20], F32)
                psr = [sg.ps(f"psr{i}", [128, 512], F32) for i in range(2)]
                lg = [sg.sb(f"lg{i}", [128, 4, 20], F32) for i in range(2)]
                wk = {n: [sg.sb(f"{n}{i}", shp, F32) for i in range(2)] for n, shp in (
                    ("gmx", [128, 4, 1]), ("goh", [128, 4, 4]), ("gex", [128, 4, 4]), ("gsm", [128, 4, 1]),
                    ("em", [128, 4, 16]), ("t1", [128, 4, 1]), ("m1", [128, 4, 16]), ("em2", [128, 4, 16]),
                    ("t2", [128, 4, 1]), ("m2", [128, 4, 16]), ("w1", [128, 4, 1]), ("cmb", [128, 4, 16]))}
                mk.dma("sp", out=rwt[:], in_=rw[l].rearrange("(kc p) n -> p kc n", p=128), w=["rwt"])
                mk.dma("sp", out=rbb[:], in_=rows[l, 0:1, R_RB:R_RB + 20].to_broadcast([128, 20]), w=["rbb"])
            TL = [t_ for t_ in TILES if not (which == 1 and l == DEPTH - 1 and t_[2] == 2)]

            def ld_(i):
                n0_, sz_, vi_, b_ = TL[i]
                p_ = i % 2
                mk.dma("sp", out=xt[p_][:, :, :sz_], in_=XT[:, :, n0_:n0_ + sz_].rearrange("j p t -> p j t"),
                       w=[f"xt{p_}"] + [f"xs{p_}{j}" for j in range(8)])
            ld_(0)
            for i, (n0, sz, vi, b) in enumerate(TL):
                p = i % 2
                if i + 1 < len(TL):
                    ld_(i + 1)
                mk.act(sq[p][:, :, :sz], xt[p][:, :, :sz], AF.Square, r=[f"xt{p}"], w=[f"sq{p}"])
                for j in range(8):
                    mk.mm(pss[p][:, :sz], ones_b[:], sq[p][:, j, :sz], j == 0, j == 7, r=[f"sq{p}"], w=[f"pss{p}"])
                mk.act(rt[p][:, :sz], pss[p][:, :sz], AF.Sqrt, bias=EPS, scale=1.0 / D, r=[f"pss{p}"], w=[f"rt{p}"])
                mk.recip(rt[p][:, :sz], rt[p][:, :sz], r=[f"rt{p}"], w=[f"rt{p}"])
                dst = hf[p] if router else hb[p]
                dk = f"hf{p}" if router else f"hb{p}"
                for j in range(8):
                    mk.tt("pool" if j % 2 else "dve", xt[p][:, j, :sz], xt[p][:, j, :sz], rt[p][:, :sz], ALU.mult,
                          r=[f"xt{p}", f"rt{p}"], w=[f"xs{p}{j}"])
                    mk.act(dst[:, j, :sz], xt[p][:, j, :sz], AF.Identity, scale=AB[:, l, which, vi, j:j + 1],
                           bias=MOD[:, l, shc + j, vi:vi + 1], r=[f"xs{p}{j}"], w=[f"{dk}{j}"])
                if router:
                    for hh in range(2):
                        mk.cp("dve" if hh == 0 else "pool", hb[p][:, hh * 4:(hh + 1) * 4, :sz],
                              hf[p][:, hh * 4:(hh + 1) * 4, :sz], r=[f"hf{p}{j}" for j in range(hh * 4, hh * 4 + 4)],
                              w=[f"hb{p}_{hh}"])
                    hbk = [f"hb{p}_0", f"hb{p}_1"]
                else:
                    hbk = [f"hb{p}{j}" for j in range(8)]
                mk.dma("sp", out=HT[:, :, n0:n0 + sz].rearrange("j p t -> p j t"), in_=hb[p][:, :, :sz], r=hbk)
                if router:
                    nb = sz // 128
                    K = lambda n: f"{n}{p}"
                    for bb in range(nb):
                        for kc in range(8):
                            mk.mm(psr[p][:, bb * 20:(bb + 1) * 20], hf[p][:, kc, bb * 128:(bb + 1) * 128],
                                  rwt[:, kc, :], kc == 0, kc == 7, r=[f"hf{p}{kc}", "rwt"], w=[K("psr")])
                    LG = lg[p][:, :nb, :]
                    mk.tt("dve", LG, psr[p][:, 0:nb * 20].rearrange("p (b n) -> p b n", n=20),
                          rbb[:].unsqueeze(1).to_broadcast([128, nb, 20]), ALU.add, r=[K("psr"), "rbb"], w=[K("lg")])
                    G = lg[p][:, :nb, 0:4]
                    EX = lg[p][:, :nb, 4:20]
                    W_ = {n: wk[n][p][:, :nb, :] for n in wk}
                    mk.op("dve", lambda e: e.tensor_reduce(out=W_["gmx"], in_=G, axis=AX.X, op=ALU.max),
                          r=[K("lg")], w=[K("gmx")])
                    mk.tt("dve", W_["goh"], G, W_["gmx"].to_broadcast([128, nb, 4]), ALU.is_equal,
                          r=[K("lg"), K("gmx")], w=[K("goh")])
                    mk.tt("dve", W_["gex"], G, W_["gmx"].to_broadcast([128, nb, 4]), ALU.subtract,
                          r=[K("lg"), K("gmx")], w=[K("gex")])
                    mk.act(W_["gex"], W_["gex"], AF.Exp, r=[K("gex")], w=[K("gex")])
                    mk.op("dve", lambda e: e.tensor_reduce(out=W_["gsm"], in_=W_["gex"], axis=AX.X, op=ALU.add),
                          r=[K("gex")], w=[K("gsm")])
                    mk.recip(W_["gsm"], W_["gsm"], r=[K("gsm")], w=[K("gsm")])
                    mk.ts("dve", W_["goh"], W_["goh"], 1e4, ALU.mult, -1e4, ALU.add, r=[K("goh")], w=[K("goh")])
                    mk.tt("dve", wk["em"][p][:, :nb, :].rearrange("p b (g e) -> p b g e", g=4),
                          EX.rearrange("p b (g e) -> p b g e", g=4),
                          W_["goh"].unsqueeze(3).to_broadcast([128, nb, 4, 4]), ALU.add,
                          r=[K("lg"), K("goh")], w=[K("em")])
                    mk.op("dve", lambda e: e.tensor_reduce(out=W_["t1"], in_=W_["em"], axis=AX.X, op=ALU.max),
                          r=[K("em")], w=[K("t1")])
                    mk.tt("dve", W_["m1"], W_["em"], W_["t1"].to_broadcast([128, nb, 16]), ALU.is_equal,
                          r=[K("em"), K("t1")], w=[K("m1")])
                    mk.stt(W_["em2"], W_["m1"], -1e4, W_["em"], ALU.mult, ALU.add, r=[K("m1"), K("em")], w=[K("em2")])
                    mk.op("dve", lambda e: e.tensor_reduce(out=W_["t2"], in_=W_["em2"], axis=AX.X, op=ALU.max),
                          r=[K("em2")], w=[K("t2")])
                    mk.tt("dve", W_["m2"], W_["em2"], W_["t2"].to_broadcast([128, nb, 16]), ALU.is_equal,
                          r=[K("em2"), K("t2")], w=[K("m2")])
                    mk.tt("dve", W_["w1"], W_["t1"], W_["t2"], ALU.subtract, r=[K("t1"), K("t2")], w=[K("w1")])
                    mk.act(W_["w1"], W_["w1"], AF.Sigmoid, r=[K("w1")], w=[K("w1")])
                    mk.tt("dve", W_["m1"], W_["m1"], W_["m2"], ALU.subtract, r=[K("m1"), K("m2")], w=[K("m1")])
                    mk.tt("dve", W_["m1"], W_["m1"], W_["w1"].to_broadcast([128, nb, 16]), ALU.mult,
                          r=[K("m1"), K("w1")], w=[K("m1")])
                    mk.tt("dve", W_["m1"], W_["m1"], W_["m2"], ALU.add, r=[K("m1"), K("m2")], w=[K("m1")])
                    mk.tt("dve", W_["cmb"], W_["m1"], W_["gsm"].to_broadcast([128, nb, 16]), ALU.mult,
                          r=[K("m1"), K("gsm")], w=[K("cmb")])
                    mk.dma("sp", out=COMB[n0:n0 + sz, :].rearrange("(b p) e -> p b e", p=128), in_=W_["cmb"],
                           r=[K("cmb")])
            sg.close()

        def stage_z(l):
            sg = Stage(mk, f"z{l}")
            ht = sg.sb("ht", [128, 8, S], BF16)
            wf = [sg.sb(f"wf{i}", [128, 8, 512], BF16) for i in range(2)]
            wt = sg.sb("wt", [128, 8, NTMC], BF16)
            zo = [sg.sb(f"zo{i}", [128, 4, 512], BF16) for i in range(2)]
            zv = [sg.sb(f"zv{i}", [128, 4, 512], BF16) for i in range(2)]
            zgv = [sg.sb(f"zgv{i}", [128, 4, 512], BF16) for i in range(2)]
            zg = [sg.sb(f"zg{i}", [128, 4, 16], F32) for i in range(2)]
            pz = [sg.ps(f"pz{i}", [128, 512], F32) for i in range(4)]
            pv = [sg.ps(f"pv{i}", [128, 512], F32) for i in range(2)]
            pg = sg.ps("pg", [128, 512], F32)
            for i, (n0, sz, vi, b) in enumerate(TILES):
                mk.dma("sp", out=ht[:, :, n0:n0 + sz], in_=HT[:, :, n0:n0 + sz].rearrange("j p t -> p j t"),
                       w=[f"ht{i}"])
            mk.dma("pool", out=wt[:], in_=w_in_r[l, :, NFM * 128:NFM * 128 + NTMC].rearrange("(kc p) n -> p kc n", p=128),
                   w=["wt"])
            funcs = {}
            for c in range(NFM):
                funcs[c] = AF.Identity
            for c in range(C_MO, C_MO + 4):
                funcs[c] = AF.Sigmoid
            for c in range(C_GU, C_GU + 4):
                funcs[c] = AF.Gelu
            for c in range(C_BRA, NFM):
                funcs[c] = AF.Sigmoid
            groups = [(c0, min(4, NFM - c0)) for c0 in range(0, NFM, 4)]
            pzi = 0
            oi = 0
            def load_wf(gi):
                c0_, ncg_ = groups[gi]
                mk.dma("pool", out=wf[gi % 2][:, :, :ncg_ * 128],
                       in_=w_in_r[l, :, c0_ * 128:(c0_ + ncg_) * 128].rearrange("(kc p) n -> p kc n", p=128),
                       w=[f"wf{gi % 2}"])
            load_wf(0)
            for gi, (c0, ncg) in enumerate(groups):
                p = gi % 2
                if gi >= 1 and gi + 1 < len(groups):
                    load_wf(gi + 1)
                if gi == 0:
                    load_wf(1)
                dead_ctx = l == DEPTH - 1 and (C_MO <= c0 < C_MO + 4 or c0 >= C_GU + 1)
                for i, (n0, sz, vi, b) in enumerate(TILES):
                    if dead_ctx and vi == 2:
                        continue
                    o = oi % 2
                    oi += 1
                    for ci in range(ncg):
                        q = pzi % 4
                        pzi += 1
                        for kc in range(8):
                            mk.mm(pz[q][:, :sz], wf[p][:, kc, ci * 128:(ci + 1) * 128], ht[:, kc, n0:n0 + sz],
                                  kc == 0, kc == 7, r=[f"wf{p}", f"ht{i}"], w=[f"pz{q}"])
                        mk.act(zo[o][:, ci, :sz], pz[q][:, :sz], funcs[c0 + ci], r=[f"pz{q}"], w=[f"zo{o}_{ci}"])
                    mk.dma("sp", out=ZF[c0:c0 + ncg, :, n0:n0 + sz].rearrange("j p t -> p j t"),
                           in_=zo[o][:, :ncg, :sz], r=[f"zo{o}_{ci}" for ci in range(ncg)])
            for i, (n0, sz, vi, b) in enumerate(TILES):
                o = i % 2
                nb = sz // 128
                for bb in range(nb):
                    t0 = n0 + bb * 128
                    q = bb % 2
                    for kc in range(8):
                        mk.mm(pv[q][:, :], ht[:, kc, t0:t0 + 128], wt[:, kc, 0:512], kc == 0, kc == 7,
                              r=[f"ht{i}", "wt"], w=[f"pv{q}"])
                    mk.cp("dve", zv[o][:, bb, :], pv[q][:, :], r=[f"pv{q}"], w=[f"zv{o}_{bb}"])
                    for kc in range(8):
                        mk.mm(pz[q][:, :], ht[:, kc, t0:t0 + 128], wt[:, kc, 528:1040], kc == 0, kc == 7,
                              r=[f"ht{i}", "wt"], w=[f"pz{q}"])
                    mk.act(zgv[o][:, bb, :], pz[q][:, :], AF.Gelu, r=[f"pz{q}"], w=[f"zgv{o}_{bb}"])
                    for kc in range(8):
                        mk.mm(pg[:, bb * 16:(bb + 1) * 16], ht[:, kc, t0:t0 + 128], wt[:, kc, 512:528], kc == 0, kc == 7,
                              r=[f"ht{i}", "wt"], w=["pg"])
                mk.cp("dve", zg[o][:, :nb, :], pg[:, 0:nb * 16].rearrange("p (b n) -> p b n", n=16), r=["pg"],
                      w=[f"zg{o}"])
                mk.dma("sp", out=ZTV[n0:n0 + sz, :].rearrange("(b p) f -> p b f", p=128), in_=zv[o][:, :nb, :],
                       r=[f"zv{o}_{bb}" for bb in range(nb)])
                mk.dma("sp", out=ZTGV[n0:n0 + sz, :].rearrange("(b p) f -> p b f", p=128), in_=zgv[o][:, :nb, :],
                       r=[f"zgv{o}_{bb}" for bb in range(nb)])
                mk.dma("sp", out=ZTG[n0:n0 + sz, :].rearrange("(b p) f -> p b f", p=128), in_=zg[o][:, :nb, :],
                       r=[f"zg{o}"])
            sg.close()

        def stage_mlstm(l, b):
            sg = Stage(mk, f"ml{l}{b}")
            NC_ = L // 128
            s0 = b * L
            raw = [sg.sb(f"raw{i}", [128, L], BF16) for i in range(2)]
            qk = sg.sb("qk", [128, 8, L], BF16)
            ktm = sg.sb("ktm", [128, NC_, 4, 128], BF16)
            vaug = sg.sb("vaug", [128, NC_, 4, 128], BF16)
            dg = sg.sb("dg", [128, 8, 3, 128], BF16)
            gt = sg.sb("gt", [128, NC_, 16], F32)
            gbb = sg.sb("gbb", [128, 16], F32)
            lt = sg.sb("lt", [128, NC_, 8], F32)
            cl = sg.sb("cl", [128, NC_, 8], F32)
            wsx = sg.sb("wsx", [128, NC_, 8], F32)
            flo = sg.sb("flo", [128, NC_, 8], F32)
            ebe = sg.sb("ebe", [128, NC_, 8], F32)
            ebs = sg.sb("ebs", [128, NC_, 8], F32)
            trif = sg.sb("trif", [128, 128], F32)
            trib = sg.sb("trib", [128, 128], F32)
            maskf = sg.sb("maskf", [128, 128], F32)
            maskb = sg.sb("maskb", [128, 128], F32)
            hX = [sg.sb("hX0", [128, NC_, 4, 128], F32), sg.sb("hX1", [128, NC_, 4, 128], BF16)]
            smo = sg.sb("smo", [128, 4, L], BF16)
            U = [sg.sb(f"U{i}", [128, 4, 130], F32) for i in range(2)]
            CTb = [[sg.sb(f"CTb{d}{i}", [128, 4, 130], BF16) for i in range(2)] for d in range(2)]
            qkt = [[sg.sb(f"qkt{d}{i}", [128, 4, 128], BF16) for i in range(2)] for d in range(2)]
            vt = [[sg.sb(f"vt{d}{i}", [128, 4, 130], BF16) for i in range(2)] for d in range(2)]
            adn = [sg.sb(f"adn{i}", [128, 4, 2], F32) for i in range(2)]
            rin = [sg.sb(f"rin{i}", [128, 4], F32) for i in range(2)]
            pst = [sg.ps(f"pst{i}", [128, 512], F32) for i in range(2)]
            pnm = [sg.ps(f"pnm{i}", [128, 512], F32) for i in range(2)]
            ppp = [sg.ps(f"ppp{i}", [128, 512], F32) for i in range(2)]
            psm = [sg.ps(f"psm{i}", [128, 512], F32) for i in range(2)]
            ptrv = [ppp[i][:].bitcast(BF16) for i in range(2)]

            mk.dma("sp", out=smo[:], in_=ZF[C_MO:C_MO + 4, :, s0:s0 + L].rearrange("j p t -> p j t"), w=["smo"])
            mk.dma("sp", out=vaug[:].rearrange("p c h e -> p c (h e)"),
                   in_=ZTV[s0:s0 + L, :].rearrange("(c p) f -> p c f", p=128), w=["vaug"])
            mk.dma("sp", out=gt[:], in_=ZTG[s0:s0 + L, :].rearrange("(c p) n -> p c n", p=128), w=["gt"])
            mk.dma("sp", out=gbb[:], in_=rows[l, 0:1, R_GB:R_GB + 16].to_broadcast([128, 16]), w=["gbb"])
            for tdst, tsrc, nm in ((trif, c_trif, "trif"), (trib, c_trib, "trib"), (maskf, c_maskf, "maskf"),
                                   (maskb, c_maskb, "maskb")):
                mk.dma("sp", out=tdst[:], in_=tsrc[:, :], w=[nm])
            for d in range(2):
                mk.memset("pool", U[d][:], 0.0, w=[f"U{d}"])
                mk.memset("pool", CTb[d][0][:], 0.0, w=[f"CTb{d}0"])
                mk.memset("pool", CTb[d][1][:], 0.0, w=[f"CTb{d}1"])
            seq_tiles_ = [(0, LC)] + [(LC + i * 512, 512) for i in range(4)]
            for j in range(8):
                for tap in range(3):
                    mk.ts("pool" if tap == 1 else "dve", dg[:, j, tap, :], ident_f[:],
                          VEC[:, l, V_CONV + j * 3 + tap:V_CONV + j * 3 + tap + 1], ALU.mult, r=["ident_f"], w=[f"dg{j}"])
            ci = 0
            for j in range(8):
                rw_ = raw[j % 2]
                rk = f"raw{j % 2}"
                mk.dma("sp", out=rw_[:], in_=ZF[C_MQ + j, :, s0:s0 + L], w=[rk])
                for (t0, sz) in seq_tiles_:
                    t1_ = t0 + sz
                    sa, sb_ = (0, LC) if t0 < LC else (LC, L)
                    u = ci % 2
                    ci += 1
                    pc_ = pnm[u]
                    mk.mm(pc_[:, 0:sz], dg[:, j, 1, :], rw_[:, t0:t1_], True, False, r=[f"dg{j}", rk], w=[f"pnm{u}"])
                    lo = max(t0, sa + 1)
                    mk.mm(pc_[:, lo - t0:sz], dg[:, j, 0, :], rw_[:, lo - 1:t1_ - 1], False, False, r=[f"dg{j}", rk],
                          w=[f"pnm{u}"])
                    hi = min(t1_, sb_ - 1)
                    mk.mm(pc_[:, 0:hi - t0], dg[:, j, 2, :], rw_[:, t0 + 1:hi + 1], False, True, r=[f"dg{j}", rk],
                          w=[f"pnm{u}"])
                    mk.act(qk[:, j, t0:t1_], pc_[:, 0:sz], AF.Silu, r=[f"pnm{u}"], w=[f"qk{j}"])
            for c in range(NC_):
                p = c % 2
                for h in range(4):
                    mk.tr(ptrv[p][:, h * 128:(h + 1) * 128], qk[:, 4 + h, c * 128:(c + 1) * 128],
                          ident_b[:], r=[f"qk{4 + h}", "ident_b"], w=[f"ppp{p}"])
                mk.cp("dve" if c % 2 else "act", ktm[:, c, :, :],
                      ptrv[p][:, 0:512].rearrange("p (h d) -> p h d", h=4), r=[f"ppp{p}"], w=[f"ktm{c}"])
            mk.tt("dve", gt[:], gt[:], gbb[:].unsqueeze(1).to_broadcast([128, NC_, 16]), ALU.add, r=["gt", "gbb"],
                  w=["gt"])
            mk.act(lt[:, :, 0:4], gt[:, :, 4:8], AF.Exp, scale=-1.0, r=["gt"], w=["lt"])
            mk.act(lt[:, :, 4:8], gt[:, :, 12:16], AF.Exp, scale=-1.0, r=["gt"], w=["lt"])
            mk.act(lt[:], lt[:], AF.Ln, bias=1.0, r=["lt"], w=["lt"])
            pcs = pst[0]
            ptot = pst[1]
            for c in range(NC_):
                mk.mm(pcs[:, c * 8:c * 8 + 4], trif[:], lt[:, c, 0:4], True, True, r=["trif", "lt"], w=["pst0"])
                mk.mm(pcs[:, c * 8 + 4:c * 8 + 8], trib[:], lt[:, c, 4:8], True, True, r=["trib", "lt"], w=["pst0"])
                mk.mm(ptot[:, c * 8:c * 8 + 8], ones_f[:], lt[:, c, :], True, True, r=["ones_f", "lt"], w=["pst1"])
            mk.cp("dve", cl[:], pcs[:, 0:NC_ * 8].rearrange("p (c n) -> p c n", n=8), r=["pst0"], w=["cl"])
            mk.act(flo[:], cl[:], AF.Exp, r=["cl"], w=["flo"])
            mk.act(ebe[:], ptot[:, 0:NC_ * 8].rearrange("p (c n) -> p c n", n=8), AF.Exp, scale=-1.0, r=["pst1"],
                   w=["ebe"])
            mk.ts("dve", ebs[:], ebe[:], QS, ALU.mult, r=["ebe"], w=["ebs"])
            mk.tt("dve", wsx[:, :, 0:4], gt[:, :, 0:4], cl[:, :, 0:4], ALU.add, r=["gt", "cl"], w=["wsx"])
            mk.tt("dve", wsx[:, :, 4:8], gt[:, :, 8:12], cl[:, :, 4:8], ALU.add, r=["gt", "cl"], w=["wsx"])
            mk.act(wsx[:], wsx[:], AF.Exp, r=["wsx"], w=["wsx"])
            order = [list(range(NC_)), [1, 0] + list(range(NC_ - 1, 1, -1))]

            def ctx_(step, d):
                c = order[d][step]
                return c, slice(c * 128, (c + 1) * 128), slice(d * 4, d * 4 + 4), f"qkt{d}{step % 2}", f"vt{d}{step % 2}"

            def a1(step):
                sp_ = step % 2
                for d in range(2):
                    c, cs, lns, qkk, vtk = ctx_(step, d)
                    mask = maskf if d == 0 else maskb
                    mkey = "maskf" if d == 0 else "maskb"
                    for h in range(4):
                        mk.mm(pst[d][:, h * 128:(h + 1) * 128], qk[:, 4 + h, cs], qk[:, h, cs], True, True,
                              r=[f"qk{h}", f"qk{4 + h}"], w=[f"pst{d}"])
                    mk.tt("dve", qkt[d][sp_][:], pst[d][:].rearrange("p (h t) -> p h t", h=4),
                          mask[:].unsqueeze(1).to_broadcast([128, 4, 128]), ALU.mult, r=[f"pst{d}", mkey], w=[qkk])
                    mk.tt("pool", vt[d][sp_][:, :, 0:128], vaug[:, c, :, :],
                          wsx[:, c, lns].unsqueeze(2).to_broadcast([128, 4, 128]), ALU.mult, r=["vaug", "wsx"], w=[vtk])
                    mk.cp("pool", vt[d][sp_][:, :, 128:130], wsx[:, c, lns].unsqueeze(2).to_broadcast([128, 4, 2]),
                          r=["wsx", vtk], w=[vtk])

            def a2(step):
                sp_ = step % 2
                for d in range(2):
                    c, cs, lns, qkk, vtk = ctx_(step, d)
                    for h in range(4):
                        mk.mm(ppp[d][:, h * 128:(h + 1) * 128], ktm[:, c, h, :], vt[d][sp_][:, h, 0:128], True, True,
                              r=[f"ktm{c}", vtk], w=[f"ppp{d}"])
                        o2 = d * 8 + h * 2
                        mk.mm(psm[0][:, o2:o2 + 2], ktm[:, c, h, :], vt[d][sp_][:, h, 128:130], True, True,
                              r=[f"ktm{c}", vtk], w=["psm0"])

            def c1(step):
                sp_ = step % 2
                cb_ = (step + 1) % 2
                for d in range(2):
                    c, cs, lns, qkk, vtk = ctx_(step, d)
                    for h in range(4):
                        mk.mm(pnm[d][:, h * 128:(h + 1) * 128], qkt[d][sp_][:, h, :], vt[d][sp_][:, h, 0:128], True, False,
                              r=[qkk, vtk], w=[f"pnm{d}"])
                        mk.mm(pnm[d][:, h * 128:(h + 1) * 128], qk[:, h, cs], CTb[d][cb_][:, h, 0:128], False, True,
                              r=[f"qk{h}", f"CTb{d}{cb_}"], w=[f"pnm{d}"])
                        o1 = d * 8 + h * 2
                        mk.mm(psm[1][:, o1:o1 + 2], qkt[d][sp_][:, h, :], vt[d][sp_][:, h, 128:130], True, False,
                              r=[qkk, vtk], w=["psm1"])
                        mk.mm(psm[1][:, o1:o1 + 2], qk[:, h, cs], CTb[d][cb_][:, h, 128:130], False, True,
                              r=[f"qk{h}", f"CTb{d}{cb_}"], w=["psm1"])

            def c2(step):
                for d in range(2):
                    c, cs, lns, qkk, vtk = ctx_(step, d)
                    denv = psm[1][:, d * 8:d * 8 + 8].rearrange("p (h n) -> p h n", n=2)
                    mk.ts("dve", adn[d][:], denv, -1.0, ALU.mult, r=["psm1"], w=[f"adn{d}"])
                    mk.tt("dve", rin[d][:], denv[:, :, 0], adn[d][:, :, 0], ALU.max, r=["psm1", f"adn{d}"],
                          w=[f"rin{d}"])
                    mk.tt("dve", rin[d][:], rin[d][:], flo[:, c, lns], ALU.max, r=[f"rin{d}", "flo"],
                          w=[f"rin{d}"])
                    mk.recip(rin[d][:], rin[d][:], r=[f"rin{d}"], w=[f"rin{d}"])
                    mk.tt("dve", hX[d][:, c, :, :], pnm[d][:].rearrange("p (h e) -> p h e", h=4),
                          rin[d][:].unsqueeze(2).to_broadcast([128, 4, 128]), ALU.mult, r=[f"pnm{d}", f"rin{d}"],
                          w=[f"hX{d}_{c}"])

            def bb_(step):
                cb_ = step % 2
                for d in range(2):
                    c, cs, lns, qkk, vtk = ctx_(step, d)
                    cprev = order[d][step - 1] if step > 0 else None
                    if cprev is not None:
                        mk.tt("pool", U[d][:], U[d][:], ebe[:, cprev, lns].unsqueeze(2).to_broadcast([128, 4, 130]),
                              ALU.mult, r=[f"U{d}", "ebe"], w=[f"U{d}"])
                    mk.tt("dve", U[d][:, :, 0:128], ppp[d][:].rearrange("p (h e) -> p h e", h=4), U[d][:, :, 0:128],
                          ALU.add, r=[f"ppp{d}", f"U{d}"], w=[f"U{d}"])
                    mk.tt("dve", U[d][:, :, 128:130],
                          psm[0][:, d * 8:d * 8 + 8].rearrange("p (h n) -> p h n", n=2), U[d][:, :, 128:130],
                          ALU.add, r=["psm0", f"U{d}"], w=[f"U{d}"])
                    mk.tt("pool", CTb[d][cb_][:], U[d][:], ebs[:, c, lns].unsqueeze(2).to_broadcast([128, 4, 130]),
                          ALU.mult, r=[f"U{d}", "ebs"], w=[f"CTb{d}{cb_}"])

            import os as _os
            _skip = _os.environ.get("MKSKIP", "")
            for i in range(NC_ + 1 if "scan" not in _skip else 0):
                if i < NC_:
                    a1(i)
                if i >= 1:
                    c1(i - 1)
                if i < NC_:
                    a2(i)
                if i >= 1:
                    c2(i - 1)
                if i < NC_:
                    bb_(i)
            GC = 6
            hsq = sg.sb("hsq", [128, GC, 4, 128], F32)
            ssq = sg.sb("ssq", [128, NC_, 4], F32)
            hn = [sg.sb(f"hn{i}", [128, 4, 128], BF16) for i in range(2)]
            ya = smo
            for g0 in range(0, NC_ if "post" not in _skip else 0, GC):
                gk = f"hsg{g0}"
                xk = [f"hX0_{c}" for c in range(g0, g0 + GC)] + [f"hX1_{c}" for c in range(g0, g0 + GC)]
                mk.tt("pool", hX[0][:, g0:g0 + GC, :, :], hX[0][:, g0:g0 + GC, :, :], hX[1][:, g0:g0 + GC, :, :], ALU.add,
                      r=xk, w=[gk])
                mk.tt("dve", hsq[:], hX[0][:, g0:g0 + GC, :, :], hX[0][:, g0:g0 + GC, :, :], ALU.mult, r=[gk], w=["hsq"])
                mk.op("dve", lambda e: e.tensor_reduce(out=ssq[:, g0:g0 + GC, :], in_=hsq[:], axis=AX.X, op=ALU.add),
                      r=["hsq"], w=[f"ssq{g0}"])
                mk.act(ssq[:, g0:g0 + GC, :], ssq[:, g0:g0 + GC, :], AF.Sqrt, bias=EPS, scale=1.0 / 128, r=[f"ssq{g0}"],
                       w=[f"ssq{g0}"])
                mk.recip(ssq[:, g0:g0 + GC, :], ssq[:, g0:g0 + GC, :], r=[f"ssq{g0}"], w=[f"ssq{g0}"])
                for c in range(g0, g0 + GC):
                    p = c % 2
                    mk.tt("pool", hn[p][:], hX[0][:, c, :, :], ssq[:, c, :].unsqueeze(2).to_broadcast([128, 4, 128]),
                          ALU.mult, r=[gk, f"ssq{g0}"], w=[f"hn{p}"])
                    for h in range(4):
                        mk.tr(ptrv[p][:, h * 128:(h + 1) * 128], hn[p][:, h, :], ident_b[:], r=[f"hn{p}"],
                              w=[f"ppp{p}"])
                    for h in range(4):
                        mk.stt(ya[:, h, c * 128:(c + 1) * 128], ptrv[p][:, h * 128:(h + 1) * 128],
                               VEC[:, l, V_MNORM + h:V_MNORM + h + 1], smo[:, h, c * 128:(c + 1) * 128],
                               ALU.mult, ALU.mult, r=[f"ppp{p}", "smo"], w=[f"ya{c}"])
            mk.dma("sp", out=YA[:, :, s0:s0 + L].rearrange("j p t -> p j t"), in_=ya[:],
                   r=[f"ya{c}" for c in range(NC_)])
            sg.close()

        def stage_mla(l, b):
            sg = Stage(mk, f"at{l}{b}")
            NC_ = L // 128
            s0 = b * L
            seq_tiles = [(0, LC)] + [(LC + i * 512, 512) for i in range(4)]
            aq = sg.sb("aq", [128, 3, L], BF16)
            akv = sg.sb("akv", [128, 2, L], BF16)
            sqb = sg.sb("sqb", [128, 3, 512], BF16)
            rs = [sg.sb(f"rs{i}", [128, 512], F32) for i in range(2)]
            wq = sg.sb("wq", [128, 3, 1536], BF16)
            wkv = sg.sb("wkv", [128, 2, 1024], BF16)
            cc = sg.sb("cc", [128, L], F32)
            ss = sg.sb("ss", [128, L], F32)
            kra = sg.sb("kra", [128, 2, L], BF16)
            krt = [sg.sb(f"krt{i}", [128, 512], F32) for i in range(2)]
            kr = sg.sb("kr", [128, L], BF16)
            QT = [sg.sb(f"QT{i}", [128, L], BF16) for i in range(2)]
            KT = [sg.sb(f"KT{i}", [128, L], BF16) for i in range(2)]
            VA = sg.sb("VA", [128, NC_, 8, 128], BF16)
            t1 = [sg.sb(f"t1{i}", [128, 512], F32) for i in range(2)]
            t2 = [sg.sb(f"t2{i}", [128, 512], F32) for i in range(2)]
            pT = [sg.sb(f"pT{i}", [128, 512], BF16) for i in range(4)]
            rec = [sg.sb(f"rec{i}", [64, 512], F32) for i in range(2)]
            yb = sg.sb("yb", [128, 4, L], BF16)
            pa = [sg.ps("pa0", [128, 512], F32)] * 2
            pb_ = [sg.ps("pb0", [128, 512], F32)] * 2
            psc = [sg.ps(f"psc{i}", [128, 512], F32) for i in range(4)]
            pac = [sg.ps(f"pac{i}", [128, 512], F32) for i in range(2)]

            mk.dma("sp", out=aq[:], in_=ZF[C_AQ:C_AQ + 3, :, s0:s0 + L].rearrange("j p t -> p j t"), w=["aq"])
            mk.dma("sp", out=akv[:], in_=ZF[C_AKV:C_AKV + 2, :, s0:s0 + L].rearrange("j p t -> p j t"), w=["akv"])
            mk.dma("sp", out=kra[:], in_=ZF[C_KRA:C_KRA + 2, :, s0:s0 + L].rearrange("j p t -> p j t"), w=["kra"])
            mk.dma("sp", out=cc[:], in_=c_cc[:, :], w=["cc"])
            mk.dma("sp", out=ss[:], in_=c_ss[:, :], w=["ss"])
            mk.dma("pool", out=wq[:], in_=wuq_r[l].rearrange("(kc p) n -> p kc n", p=128), w=["wq"])
            mk.dma("pool", out=wkv[:], in_=wukv_r[l].rearrange("(kc p) n -> p kc n", p=128), w=["wkv"])
            mk.memset("pool", VA[:, :, :, 64:128], 1.0, w=["VA1"])

            def rms_fm(src, nch, gcol, skey, ti):
                for (a0, sz) in seq_tiles:
                    p = ti[0] % 2
                    ti[0] += 1
                    mk.act(sqb[:, :nch, :sz], src[:, :, a0:a0 + sz], AF.Square, r=[skey], w=["sqb"])
                    for j in range(nch):
                        mk.mm(pa[p][:, :sz], ones_b[:], sqb[:, j, :sz], j == 0, j == nch - 1, r=["sqb"], w=["pa0"])
                    mk.act(rs[p][:, :sz], pa[p][:, :sz], AF.Sqrt, bias=EPS, scale=1.0 / (nch * 128), r=["pa0"],
                           w=[f"rs{p}"])
                    mk.recip(rs[p][:, :sz], rs[p][:, :sz], r=[f"rs{p}"], w=[f"rs{p}"])
                    for j in range(nch):
                        mk.stt(src[:, j, a0:a0 + sz], src[:, j, a0:a0 + sz], VEC[:, l, gcol + j:gcol + j + 1],
                               rs[p][:, :sz], ALU.mult, ALU.mult, r=[skey, f"rs{p}"], w=[skey])
            ti = [0]
            rms_fm(aq, 3, V_QN, "aq", ti)
            rms_fm(akv, 2, V_KVN, "akv", ti)
            for i, (a0, sz) in enumerate(seq_tiles):
                p = i % 2
                mk.tt("dve", krt[p][64:96, :sz], kra[64:96, 0, a0:a0 + sz], cc[64:96, a0:a0 + sz], ALU.mult,
                      r=["kra", "cc"], w=[f"krt{p}"])
                mk.tt("pool", t1[p][64:96, :sz], kra[64:96, 1, a0:a0 + sz], ss[64:96, a0:a0 + sz], ALU.mult,
                      r=["kra", "ss"], w=[f"t1{p}"])
                mk.tt("dve", kr[64:96, a0:a0 + sz], krt[p][64:96, :sz], t1[p][64:96, :sz], ALU.add,
                      r=[f"krt{p}", f"t1{p}"], w=["kr"])
            for c in range(NC_):
                u = c % 2
                for kc in range(2):
                    mk.mm(pac[u][:, :], akv[:, kc, c * 128:(c + 1) * 128], wkv[:, kc, 512:1024], kc == 0, kc == 1,
                          r=["akv", "wkv"], w=[f"pac{u}"])
                mk.cp("dve" if c % 2 else "act", VA[:, c, :, 0:64], pac[u][:, :].rearrange("p (h e) -> p h e", h=8),
                      r=[f"pac{u}"], w=[f"VA{c}"])
            vak = [f"VA{c}" for c in range(NC_)] + ["VA1"]
            ui = [0]

            def proj(h):
                hp = h % 2
                for (a0, sz) in seq_tiles:
                    u = ui[0] % 2
                    ui[0] += 1
                    for kc in range(3):
                        mk.mm(pa[u][0:96, :sz], wq[:, kc, h * 96:(h + 1) * 96], aq[:, kc, a0:a0 + sz], kc == 0, kc == 2,
                              r=["wq", "aq"], w=["pa0"])
                    for kc in range(3):
                        mk.mm(pb_[u][0:96, :sz], wq[:, kc, 768 + h * 96:768 + (h + 1) * 96], aq[:, kc, a0:a0 + sz],
                              kc == 0, kc == 2, r=["wq", "aq"], w=["pb0"])
                    mk.tt("dve", t1[u][0:96, :sz], pa[u][0:96, :sz], cc[0:96, a0:a0 + sz], ALU.mult,
                          r=["pa0", "cc"], w=[f"t1{u}"])
                    mk.tt("dve", t2[u][0:96, :sz], pb_[u][0:96, :sz], ss[0:96, a0:a0 + sz], ALU.mult,
                          r=["pb0", "ss"], w=[f"t2{u}"])
                    mk.tt("pool", QT[hp][0:96, a0:a0 + sz], t1[u][0:96, :sz], t2[u][0:96, :sz], ALU.add,
                          r=[f"t1{u}", f"t2{u}"], w=[f"QT{hp}_{a0}"])
                    for kc in range(2):
                        mk.mm(pb_[u][0:64, :sz], wkv[:, kc, h * 64:(h + 1) * 64], akv[:, kc, a0:a0 + sz], kc == 0, kc == 1,
                              r=["wkv", "akv"], w=["pb0"])
                    mk.cp("dve", KT[hp][0:64, a0:a0 + sz], pb_[u][0:64, :sz], r=["pb0"], w=[f"KT{hp}_{a0}"])
                mk.cp("pool", KT[hp][64:96, :], kr[64:96, :], r=["kr"], w=[f"KTr{hp}"])

            units = []
            for h in range(8):
                for qi_, (a0, sz) in enumerate(seq_tiles):
                    nkb = 2 if a0 == 0 else NC_
                    for kb in range(nkb):
                        units.append((h, qi_, a0, sz, kb, nkb))

            def emit_qk(i):
                h, qi_, a0, sz, kb, nkb = units[i]
                hp = h % 2
                v = i % 4
                ktk = [f"KT{hp}_{a0_}" for (a0_, sz_) in seq_tiles] + [f"KTr{hp}"]
                mk.mm(psc[v][:, :sz], KT[hp][0:96, kb * 128:(kb + 1) * 128], QT[hp][0:96, a0:a0 + sz], True, True,
                      r=ktk + [f"QT{hp}_{a0}"], w=[f"psc{v}"])

            LA = 3
            proj(0)
            proj(1)
            for j in range(LA):
                emit_qk(j)
            for i, (h, qi_, a0, sz, kb, nkb) in enumerate(units):
                if qi_ == 1 and kb == 0 and 1 <= h and h + 1 < 8:
                    proj(h + 1)
                v = i % 4
                v3 = i % 4
                u = (h * len(seq_tiles) + qi_) % 2
                if i + LA < len(units):
                    emit_qk(i + LA)
                mk.act(pT[v3][:, :sz], psc[v][:, :sz], AF.Exp, scale=ATT_SCALE, r=[f"psc{v}"], w=[f"pT{v3}"])
                mk.mm(pac[u][:, :sz], VA[:, kb, h, :], pT[v3][:, :sz], kb == 0, kb == nkb - 1,
                      r=vak + [f"pT{v3}"], w=[f"pac{u}"])
                if kb == nkb - 1:
                    mk.recip(rec[u][:, :sz], pac[u][64:128, :sz], r=[f"pac{u}"], w=[f"rec{u}"])
                    po = (h % 2) * 64
                    mk.tt("dve", yb[po:po + 64, h // 2, a0:a0 + sz], pac[u][0:64, :sz], rec[u][:, :sz], ALU.mult,
                          r=[f"pac{u}", f"rec{u}"], w=[f"yb{h}_{a0}"])
            mk.dma("sp", out=YB[:, :, s0:s0 + L].rearrange("j p t -> p j t"), in_=yb[:],
                   r=[f"yb{h}_{a0}" for h in range(8) for (a0, sz) in seq_tiles])
            sg.close()

        def stage_gmlp(l):
            sg = Stage(mk, f"gm{l}")
            gv = [sg.sb(f"gv{i}", [128, 4, 512], BF16) for i in range(2)]
            gu = [sg.sb(f"gu{i}", [128, 4, 512], BF16) for i in range(2)]
            gsq = sg.sb("gsq", [128, 4, 512], F32)
            gss = [sg.sb(f"gss{i}", [128, 16], F32) for i in range(2)]
            gvn = [sg.sb(f"gvn{i}", [128, 4, 4, 128], BF16) for i in range(2)]
            gtmp = [sg.sb(f"gtmp{i}", [128, 4, 4, 128], F32) for i in range(2)]
            vnb = sg.sb("vnb", [128, 512], F32)
            bsb = sg.sb("bsb", [128, 512], F32)
            wsT = sg.sb("wsT", [128, 512], BF16)
            yc = [sg.sb(f"yc{i}", [128, 4, 512], BF16) for i in range(2)]
            pg = [sg.ps(f"pg{i}", [128, 2048], F32) for i in range(2)]
            mk.dma("sp", out=vnb[:], in_=rows[l, 0:1, R_VN:R_VN + 512].to_broadcast([128, 512]), w=["vnb"])
            mk.dma("sp", out=bsb[:], in_=rows[l, 0:1, R_BS:R_BS + 512].to_broadcast([128, 512]), w=["bsb"])
            mk.dma("pool", out=wsT[:], in_=gws_r[l], w=["wsT"])

            TL = [t_ for t_ in TILES if not (l == DEPTH - 1 and t_[2] == 2)]

            def ldg_(i):
                n0_, sz_, vi_, b_ = TL[i]
                p_ = i % 2
                mk.dma("sp", out=gv[p_][:, :sz_ // 128, :], in_=ZTGV[n0_:n0_ + sz_, :].rearrange("(b p) f -> p b f", p=128),
                       w=[f"gv{p_}"])
                mk.dma("sp", out=gu[p_][:, :, :sz_], in_=ZF[C_GU:C_GU + 4, :, n0_:n0_ + sz_].rearrange("j p t -> p j t"),
                       w=[f"gu{p_}"])
            ldg_(0)
            for i, (n0, sz, vi, b) in enumerate(TL):
                p = i % 2
                nb = sz // 128
                if i + 1 < len(TL):
                    ldg_(i + 1)
                GV = gv[p][:, :nb, :]
                mk.tt("dve", gsq[:, :nb, :], GV, GV, ALU.mult, r=[f"gv{p}"], w=["gsq"])
                mk.op("dve", lambda e: e.tensor_reduce(out=gss[p][:, :nb * 4],
                                                       in_=gsq[:, :nb, :].rearrange("p b (g c) -> p (b g) c", g=4),
                                                       axis=AX.X, op=ALU.add), r=["gsq"], w=[f"gss{p}"])
                mk.act(gss[p][:, :nb * 4], gss[p][:, :nb * 4], AF.Sqrt, bias=EPS, scale=1.0 / 128, r=[f"gss{p}"],
                       w=[f"gss{p}"])
                mk.recip(gss[p][:, :nb * 4], gss[p][:, :nb * 4], r=[f"gss{p}"], w=[f"gss{p}"])
                mk.tt("dve", gsq[:, :nb, :], GV, vnb[:].unsqueeze(1).to_broadcast([128, nb, 512]), ALU.mult,
                      r=[f"gv{p}", "vnb"], w=["gsq"])
                mk.tt("pool", gvn[p][:, :nb, :, :].rearrange("p b g c -> p (b g) c"),
                      gsq[:, :nb, :].rearrange("p b (g c) -> p (b g) c", g=4),
                      gss[p][:, :nb * 4].unsqueeze(2).to_broadcast([128, nb * 4, 128]), ALU.mult,
                      r=["gsq", f"gss{p}"], w=[f"gvn{p}"])
                for bb in range(nb):
                    for g in range(4):
                        mk.mm(pg[p][:, bb * 512 + g * 128:bb * 512 + (g + 1) * 128], gvn[p][:, bb, g, :],
                              wsT[:, g * 128:(g + 1) * 128], True, True, r=[f"gvn{p}", "wsT"], w=[f"pg{p}"])
                mk.tt("dve", gtmp[p][:, :nb, :, :].rearrange("p b g t -> p b (g t)"),
                      pg[p][:, :nb * 512].rearrange("p (b f) -> p b f", f=512),
                      bsb[:].unsqueeze(1).to_broadcast([128, nb, 512]), ALU.add, r=[f"pg{p}", "bsb"], w=[f"gtmp{p}"])
                mk.tt("pool", yc[p][:, :, :sz].rearrange("p g (b t) -> p g b t", t=128),
                      gtmp[p][:, :nb, :, :].rearrange("p b g t -> p g b t"),
                      gu[p][:, :, :sz].rearrange("p g (b t) -> p g b t", t=128), ALU.mult,
                      r=[f"gtmp{p}", f"gu{p}"], w=[f"yc{p}"])
                mk.dma("sp", out=YC[:, :, n0:n0 + sz].rearrange("j p t -> p j t"), in_=yc[p][:, :, :sz], r=[f"yc{p}"])
            sg.close()

        def stage_out(l):
            sg = Stage(mk, f"o{l}")
            wp = [sg.sb(f"wp{i}", [128, 4, D], BF16) for i in range(3)]
            wo = sg.sb("wo", [128, 8, D], BF16)
            yin = [[sg.sb(f"yin{i}{k}", [128, 4, 512], BF16) for k in range(3)] for i in range(2)]
            gts = [sg.sb(f"gts{i}", [128, 24, 512], BF16) for i in range(2)]
            xt = [sg.sb(f"xt{i}", [128, 8, 512], F32) for i in range(2)]
            ym = [sg.sb(f"ym{i}", [128, 8, 512], BF16) for i in range(2)]
            ta = [sg.sb(f"ta{i}", [128, 512], F32) for i in range(2)]
            tb = [sg.sb(f"tb{i}", [128, 512], F32) for i in range(2)]
            tc_ = [sg.sb(f"tc{i}", [128, 512], F32) for i in range(2)]
            pp = [[sg.ps(f"pp{i}{k}", [128, 512], F32) for k in range(3)] for i in range(2)]
            po = [sg.ps(f"po{i}", [128, 512], F32) for i in range(2)]
            for k, wsrc in enumerate((w_pa, w_pb, w_pc)):
                mk.dma("pool", out=wp[k][:], in_=wsrc[l].rearrange("(kc p) n -> p kc n", p=128), w=[f"wp{k}"])
            mk.dma("pool", out=wo[:], in_=w_out[l].rearrange("(kc p) n -> p kc n", p=128), w=["wo"])
            oi = [0]
            TL = [t_ for t_ in TILES if not (l == DEPTH - 1 and t_[2] == 2)]

            def loads(i):
                n0, sz, vi, b = TL[i]
                p = i % 2
                for k, src in enumerate((YA, YB, YC)):
                    mk.dma("sp", out=yin[p][k][:, :, :sz], in_=src[:, :, n0:n0 + sz].rearrange("j p t -> p j t"),
                           w=[f"yin{p}{k}"])
                mk.dma("sp", out=gts[p][:, :, :sz], in_=ZF[C_BRA:C_BRA + 24, :, n0:n0 + sz].rearrange("j p t -> p j t"),
                       w=[f"gts{p}"])
                mk.dma("sp", out=xt[p][:, :, :sz], in_=XT[:, :, n0:n0 + sz].rearrange("j p t -> p j t"), w=[f"xt{p}"])

            def merge(i):
                n0, sz, vi, b = TL[i]
                p = i % 2
                for oc in range(8):
                    u = oi[0] % 2
                    oi[0] += 1
                    for k in range(3):
                        for kc in range(4):
                            mk.mm(pp[u][k][:, :sz], wp[k][:, kc, oc * 128:(oc + 1) * 128], yin[p][k][:, kc, :sz],
                                  kc == 0, kc == 3, r=[f"wp{k}", f"yin{p}{k}"], w=[f"pp{u}{k}"])
                    mk.tt("dve", ta[u][:, :sz], pp[u][0][:, :sz], gts[p][:, oc, :sz], ALU.mult,
                          r=[f"pp{u}0", f"gts{p}"], w=[f"ta{u}"])
                    mk.tt("dve", tb[u][:, :sz], pp[u][1][:, :sz], gts[p][:, 8 + oc, :sz], ALU.mult,
                          r=[f"pp{u}1", f"gts{p}"], w=[f"tb{u}"])
                    mk.tt("dve", tc_[u][:, :sz], pp[u][2][:, :sz], gts[p][:, 16 + oc, :sz], ALU.mult,
                          r=[f"pp{u}2", f"gts{p}"], w=[f"tc{u}"])
                    mk.tt("pool", ta[u][:, :sz], ta[u][:, :sz], tb[u][:, :sz], ALU.add, r=[f"ta{u}", f"tb{u}"],
                          w=[f"ta{u}"])
                    mk.tt("pool", ym[p][:, oc, :sz], ta[u][:, :sz], tc_[u][:, :sz], ALU.add, r=[f"ta{u}", f"tc{u}"],
                          w=[f"ym{p}{oc}"])

            def outproj(i):
                n0, sz, vi, b = TL[i]
                p = i % 2
                for oc in range(8):
                    u = oc % 2
                    for kc in range(8):
                        mk.mm(po[u][:, :sz], wo[:, kc, oc * 128:(oc + 1) * 128], ym[p][:, kc, :sz], kc == 0, kc == 7,
                              r=["wo", f"ym{p}{kc}"], w=[f"po{u}"])
                    mk.stt(xt[p][:, oc, :sz], po[u][:, :sz], MOD[:, l, 16 + oc, vi:vi + 1], xt[p][:, oc, :sz],
                           ALU.mult, ALU.add, r=[f"po{u}", f"xt{p}"], w=[f"xt{p}"])
                mk.dma("sp", out=XT[:, :, n0:n0 + sz].rearrange("j p t -> p j t"), in_=xt[p][:, :, :sz], r=[f"xt{p}"])

            nT = len(TL)
            loads(0)
            merge(0)
            for i in range(nT):
                if i + 1 < nT:
                    loads(i + 1)
                    merge(i + 1)
                outproj(i)
            sg.close()

        def stage_moe(l, b):
            sg = Stage(mk, f"moe{l}{b}")
            NC_ = L // 128
            s0 = b * L
            seq_tiles = ([] if l == DEPTH - 1 else [(0, LC)]) + [(LC + i * 512, 512) for i in range(4)]
            acc = sg.sb("acc", [128, 8, L], F32)
            h2 = sg.sb("h2", [128, 8, L], BF16)
            cmf = sg.sb("cmf", [128, NC_, 16], F32)
            cmb = sg.sb("cmb", [128, NC_, 16], BF16)
            w1 = [sg.sb(f"w1{i}", [128, 8, 512], BF16) for i in range(2)]
            w3 = [sg.sb(f"w3{i}", [128, 8, 512], BF16) for i in range(2)]
            w2 = [sg.sb(f"w2{i}", [128, 4, D], BF16) for i in range(2)]
            cbs = [sg.sb(f"cbs{i}", [128, 512], F32) for i in range(2)]
            s1 = [sg.sb(f"s1{i}", [128, 512], F32) for i in range(2)]
            tm = [sg.sb(f"tm{i}", [128, 512], F32) for i in range(2)]
            hid = [sg.sb(f"hid{i}", [128, 4, 512], BF16) for i in range(2)]
            pcb = sg.ps("pcb", [128, 512], F32)
            p1 = [sg.ps(f"p1{i}", [128, 512], F32) for i in range(2)]
            p3 = [sg.ps(f"p3{i}", [128, 512], F32) for i in range(2)]
            po = [sg.ps(f"po{i}", [128, 512], F32) for i in range(3)]
            for (a0, sz) in seq_tiles:
                mk.dma("sp", out=h2[:, :, a0:a0 + sz], in_=HT[:, :, s0 + a0:s0 + a0 + sz].rearrange("j p t -> p j t"),
                       w=[f"h2_{a0}"])
            mk.dma("sp", out=cmf[:], in_=COMB[s0:s0 + L, :].rearrange("(c p) e -> p c e", p=128), w=["cmf"])
            mk.cp("dve", cmb[:], cmf[:], r=["cmf"], w=["cmb"])
            items = [(e, ti_, a0, sz) for e in range(16) for ti_, (a0, sz) in enumerate(seq_tiles)]

            def load_w(e):
                p = e % 2
                mk.dma("pool", out=w1[p][:], in_=e_w1[l, e].rearrange("(kc p) n -> p kc n", p=128), w=[f"w1{p}"])
                mk.dma("pool", out=w3[p][:], in_=e_w3[l, e].rearrange("(kc p) n -> p kc n", p=128), w=[f"w3{p}"])
                mk.dma("pool", out=w2[p][:], in_=e_w2[l, e].rearrange("(kc p) n -> p kc n", p=128), w=[f"w2{p}"])

            def phase_a(i):
                e, ti_, a0, sz = items[i]
                p = e % 2
                t = i % 2
                nb = sz // 128
                for bb in range(nb):
                    cblk = a0 // 128 + bb
                    mk.mm(pcb[:, bb * 128:(bb + 1) * 128], cmb[:, cblk, e:e + 1].to_broadcast([128, 128]),
                          ident_b[:], True, True, r=["cmb", "ident_b"], w=["pcb"])
                mk.cp("act", cbs[t][:, :sz], pcb[:, :sz], r=["pcb"], w=[f"cbs{t}"])
                for jc in range(4):
                    u = jc % 2
                    for kc in range(8):
                        mk.mm(p1[u][:, :sz], w1[p][:, kc, jc * 128:(jc + 1) * 128], h2[:, kc, a0:a0 + sz],
                              kc == 0, kc == 7, r=[f"w1{p}", f"h2_{a0}"], w=[f"p1{u}"])
                    for kc in range(8):
                        mk.mm(p3[u][:, :sz], w3[p][:, kc, jc * 128:(jc + 1) * 128], h2[:, kc, a0:a0 + sz],
                              kc == 0, kc == 7, r=[f"w3{p}", f"h2_{a0}"], w=[f"p3{u}"])
                    mk.act(s1[u][:, :sz], p1[u][:, :sz], AF.Silu, r=[f"p1{u}"], w=[f"s1{u}"])
                    mk.tt("dve", tm[u][:, :sz], p3[u][:, :sz], s1[u][:, :sz], ALU.mult, r=[f"p3{u}", f"s1{u}"],
                          w=[f"tm{u}"])
                    mk.tt("pool", hid[t][:, jc, :sz], tm[u][:, :sz], cbs[t][:, :sz], ALU.mult,
                          r=[f"tm{u}", f"cbs{t}"], w=[f"hid{t}_{jc}"])

            def phase_b(i):
                e, ti_, a0, sz = items[i]
                p = e % 2
                t = i % 2
                for oc in range(8):
                    u = (i * 8 + oc) % 3
                    for jc in range(4):
                        mk.mm(po[u][:, :sz], w2[p][:, jc, oc * 128:(oc + 1) * 128], hid[t][:, jc, :sz],
                              jc == 0, jc == 3, r=[f"w2{p}", f"hid{t}_{jc}"], w=[f"po{u}"])
                    if e == 0:
                        mk.cp("dve", acc[:, oc, a0:a0 + sz], po[u][:, :sz], r=[f"po{u}"], w=[f"acc{ti_}_{oc}"])
                    else:
                        mk.tt("dve", acc[:, oc, a0:a0 + sz], po[u][:, :sz], acc[:, oc, a0:a0 + sz], ALU.add,
                              r=[f"po{u}", f"acc{ti_}_{oc}"], w=[f"acc{ti_}_{oc}"])

            load_w(0)
            load_w(1)
            nt_ = len(seq_tiles)
            for i in range(len(items) + 1):
                if i < len(items):
                    phase_a(i)
                if i >= 1:
                    phase_b(i - 1)
                    e_prev, ti_prev = items[i - 1][0], items[i - 1][1]
                    if ti_prev == nt_ - 1 and e_prev + 2 < 16:
                        load_w(e_prev + 2)
            xtv = h2[:].rearrange("p j t -> p (j t)").bitcast(F32)
            h2keys = [f"h2_{a0_}" for (a0_, sz_) in seq_tiles]

            def xt_(i):
                return xtv[:, (i % 2) * 4096:(i % 2 + 1) * 4096].rearrange("p (j t) -> p j t", j=8)

            def ldx_(i):
                a0_, sz_ = seq_tiles[i]
                mk.dma("sp", out=xt_(i)[:, :, :sz_], in_=XT[:, :, s0 + a0_:s0 + a0_ + sz_].rearrange("j p t -> p j t"),
                       w=[f"xr{i % 2}"] + (h2keys if i < 2 else []))
            ldx_(0)
            for ti_, (a0, sz) in enumerate(seq_tiles):
                vi = 2 if a0 == 0 else b
                if ti_ + 1 < len(seq_tiles):
                    ldx_(ti_ + 1)
                xk = f"xr{ti_ % 2}"
                for oc in range(8):
                    mk.stt(xt_(ti_)[:, oc, :sz], acc[:, oc, a0:a0 + sz], MOD[:, l, 40 + oc, vi:vi + 1], xt_(ti_)[:, oc, :sz],
                           ALU.mult, ALU.add, r=[f"acc{ti_}_{oc}", xk], w=[xk])
                mk.dma("sp", out=XT[:, :, s0 + a0:s0 + a0 + sz].rearrange("j p t -> p j t"), in_=xt_(ti_)[:, :, :sz],
                       r=[xk])
            sg.close()

        def stage_final():
            sg = Stage(mk, "fin")
            xt = [sg.sb(f"xt{i}", [128, 8, 512], F32) for i in range(2)]
            sq = [sg.sb(f"sq{i}", [128, 8, 512], BF16) for i in range(2)]
            rt = [sg.sb(f"rt{i}", [128, 512], F32) for i in range(2)]
            ot = [sg.sb(f"ot{i}", [128, D], F32) for i in range(2)]
            pss = [sg.ps(f"pss{i}", [128, 512], F32) for i in range(2)]
            pt = [[sg.ps(f"pt{i}{h}", [128, 512], F32) for h in range(2)] for i in range(2)]
            bi = 0
            lat = [i for i, tl_ in enumerate(TILES) if tl_[2] != 2]

            def ldf_(li_):
                n0_, sz_, vi_, b_ = TILES[lat[li_]]
                mk.dma("sp", out=xt[li_ % 2][:, :, :sz_], in_=XT[:, :, n0_:n0_ + sz_].rearrange("j p t -> p j t"),
                       w=[f"xt{li_ % 2}"])
            ldf_(0)
            for li, i in enumerate(lat):
                n0, sz, vi, b = TILES[i]
                p = li % 2
                if li + 1 < len(lat):
                    ldf_(li + 1)
                mk.act(sq[p][:, :, :sz], xt[p][:, :, :sz], AF.Square, r=[f"xt{p}"], w=[f"sq{p}"])
                for j in range(8):
                    mk.mm(pss[p][:, :sz], ones_b[:], sq[p][:, j, :sz], j == 0, j == 7, r=[f"sq{p}"], w=[f"pss{p}"])
                mk.act(rt[p][:, :sz], pss[p][:, :sz], AF.Sqrt, bias=EPS, scale=1.0 / D, r=[f"pss{p}"], w=[f"rt{p}"])
                mk.recip(rt[p][:, :sz], rt[p][:, :sz], r=[f"rt{p}"], w=[f"rt{p}"])
                for j in range(8):
                    mk.stt(xt[p][:, j, :sz], xt[p][:, j, :sz], VEC[:, 0, V_FN + j:V_FN + j + 1], rt[p][:, :sz],
                           ALU.mult, ALU.mult, r=[f"xt{p}", f"rt{p}"], w=[f"xt{p}"])
                for bb in range(sz // 128):
                    q = bi % 2
                    bi += 1
                    for h in range(2):
                        for jj in range(4):
                            j = h * 4 + jj
                            mk.tr(pt[q][h][:, jj * 128:(jj + 1) * 128], xt[p][:, j, bb * 128:(bb + 1) * 128], ident_f[:],
                                  r=[f"xt{p}"], w=[f"pt{q}{h}"])
                        mk.cp("dve" if h == 0 else "act", ot[q][:, h * 512:(h + 1) * 512], pt[q][h][:],
                              r=[f"pt{q}{h}"], w=[f"ot{q}{h}"])
                    tpos = n0 - b * L - LC + bb * 128
                    mk.dma("sp", out=out[b, tpos:tpos + 128, :], in_=ot[q][:], r=[f"ot{q}0", f"ot{q}1"])
            sg.close()

        def program():
            stage_load_x()
            stage_ada()
            if check_stop(-1, "ada"):
                return
            for l in range(nlayers):
                stage_norm(l, 0, False)
                if check_stop(l, "norm1"):
                    return
                stage_z(l)
                if check_stop(l, "z"):
                    return
                for b in range(NB):
                    stage_mlstm(l, b)
                if check_stop(l, "mlstm"):
                    return
                for b in range(NB):
                    stage_mla(l, b)
                if check_stop(l, "mla"):
                    return
                stage_gmlp(l)
                if check_stop(l, "gmlp"):
                    return
                stage_out(l)
                if check_stop(l, "out"):
                    return
                stage_norm(l, 1, True)
                if check_stop(l, "norm2"):
                    return
                for b in range(NB):
                    stage_moe(l, b)
                if check_stop(l, "moe"):
                    return
            stage_final()

        program()
        mk.finish()
        build.last_ninst = mk.ninst
    return nc


def _fm(v, nchunks):
    return np.ascontiguousarray(v.reshape(nchunks, 128).T)


def prep_shared(inp):
    f32 = np.float32
    vecs = np.zeros((DEPTH, 128, NV), f32)
    rows = np.zeros((DEPTH, 1, NR), f32)
    for l in range(DEPTH):
        vecs[l, :, V_N1:V_N1 + 8] = _fm(inp["norm1"][l], 8)
        vecs[l, :, V_N2:V_N2 + 8] = _fm(inp["norm2"][l], 8)
        vecs[l, :, V_BADA:V_BADA + 48] = _fm(inp["b_ada"][l], 48)
        cw = inp["m_conv"][l]
        for tap in range(3):
            vecs[l, :, V_CONV + tap:V_CONV + 24:3] = _fm(cw[tap], 8)
        vecs[l, :, V_MNORM:V_MNORM + 4] = _fm(inp["m_norm"][l], 4)
        vecs[l, :, V_QN:V_QN + 3] = _fm(inp["a_qnorm"][l], 3)
        vecs[l, :, V_KVN:V_KVN + 2] = _fm(inp["a_kvnorm"][l], 2)
        vecs[l, :, V_FN:V_FN + 8] = _fm(inp["final_norm"], 8)
        rows[l, 0, R_GB:R_GB + 16] = inp["m_gate_b"][l]
        rows[l, 0, R_VN:R_VN + 512] = inp["g_vnorm"][l]
        rows[l, 0, R_RB:R_RB + 4] = inp["r_group_b"][l]
        rows[l, 0, R_RB + 4:R_RB + 20] = inp["r_expert_b"][l]
        rows[l, 0, R_BS:R_BS + 512] = inp["g_bs"][l].reshape(512)
    off = np.cumsum([0, 512, 512, 512, 512, 16, 384, 256, 32, 512, 512, 1024, 1024, 1024])
    o_mq, o_mk, o_mv, o_mo, o_mg, o_aq, o_akv, o_akr, o_gu, o_gv, o_bra, o_brb, o_brc = off[:13]
    ar = np.arange
    akr = o_akr + ar(32)
    akr_sw = o_akr + np.concatenate([ar(16, 32), ar(0, 16)])
    padA = np.concatenate([np.tile(akr, 2), akr, akr])
    padB = np.concatenate([np.tile(akr, 2), akr_sw, akr])
    idx = np.concatenate([o_mq + ar(512), o_mk + ar(512), o_mo + ar(512), o_aq + ar(384), o_akv + ar(256), padA, padB,
                          o_gu + ar(512), o_bra + ar(1024), o_brb + ar(1024), o_brc + ar(1024),
                          o_mv + ar(512), o_mg + ar(16), o_gv + ar(512)])
    assert idx.shape[0] == NFM * 128 + NTMC
    w_in_r = np.ascontiguousarray(inp["w_in"][:, :, idx])
    sw = np.concatenate([h * 96 + np.concatenate([ar(64), 64 + ar(16, 32), 64 + ar(0, 16)]) for h in range(8)])
    wuq_r = np.ascontiguousarray(np.concatenate([inp["a_wuq"], inp["a_wuq"][:, :, sw]], axis=2))
    kidx = np.concatenate([h * 128 + ar(64) for h in range(8)])
    vidx = np.concatenate([h * 128 + 64 + ar(64) for h in range(8)])
    wukv_r = np.ascontiguousarray(inp["a_wukv"][:, :, np.concatenate([kidx, vidx])])
    gws_r = np.ascontiguousarray(inp["g_ws"].transpose(0, 3, 1, 2).reshape(DEPTH, 128, 512))
    rwc = np.ascontiguousarray(np.concatenate([inp["r_group"], inp["r_expert"]], axis=2))
    s_ = np.arange(128)
    ident = np.eye(128, dtype=f32)
    trif = (s_[:, None] <= s_[None, :]).astype(f32)
    trib = (s_[:, None] >= s_[None, :]).astype(f32)
    half = 16
    r_ = np.repeat(np.arange(T // 64, dtype=f32), 64)
    col = np.tile(np.arange(64, dtype=f32), T // 64)
    inv = (np.float32(10000.0) ** (-np.arange(0, half, 2, dtype=f32) / np.float32(half))).astype(f32)
    ang = np.concatenate([r_[:, None] * inv, col[:, None] * inv], axis=-1).astype(f32)
    cos, sin = np.cos(ang).astype(f32), np.sin(ang).astype(f32)
    cc = np.ones((128, L), f32)
    ss = np.zeros((128, L), f32)
    cc[64:80, LC:] = cos.T
    cc[80:96, LC:] = cos.T
    ss[64:80, LC:] = -sin.T
    ss[80:96, LC:] = sin.T
    sh = dict(vecs=vecs, rows=rows, w_ada=np.ascontiguousarray(inp["w_ada"]), w_in_r=w_in_r, wuq_r=wuq_r, wukv_r=wukv_r,
              gws_r=gws_r, w_pa=np.ascontiguousarray(inp["w_pa"]), w_pb=np.ascontiguousarray(inp["w_pb"]),
              w_pc=np.ascontiguousarray(inp["w_pc"]), w_out=np.ascontiguousarray(inp["w_out"]), rw=rwc,
              e_w1=np.ascontiguousarray(inp["e_w1"]), e_w3=np.ascontiguousarray(inp["e_w3"]),
              e_w2=np.ascontiguousarray(inp["e_w2"]), c_ident=ident, c_trif=trif, c_trib=trib,
              c_maskf=(trif * np.float32(QS)).astype(f32), c_maskb=(trib * np.float32(QS)).astype(f32), c_cc=cc, c_ss=ss)
    return sh


def prep_core(inp, core):
    f32 = np.float32
    bs = [core * NB + i for i in range(NB)]
    xin = np.concatenate([np.concatenate([inp["ctx"][b], inp["x"][b]], axis=0) for b in bs], axis=0).astype(f32)
    vs = [inp["c"][bs[0]], inp["c"][bs[1]], inp["c_ctx"], inp["c_ctx"]]
    cv = np.stack([_fm(v, 8) for v in vs], axis=-1).astype(f32)
    return dict(xin=np.ascontiguousarray(xin), cv=np.ascontiguousarray(cv))


def kernel(**inputs):
    inp = {k: np.asarray(v) for k, v in inputs.items()}
    sh = prep_shared(inp)
    nc = build()
    in_maps = []
    for core in range(NCORES):
        m = dict(sh)
        m.update(prep_core(inp, core))
        in_maps.append(m)
    res = run_bass_kernel_spmd(nc, in_maps, core_ids=list(range(NCORES)))
    outs = [np.asarray(r["out"]) for r in res.results]
    return np.concatenate(outs, axis=0).astype(np.float32)
```

```python
import numpy as np
from contextlib import ExitStack
import concourse.bass as bass
import concourse.mybir as mybir
from concourse.bass_utils import run_bass_kernel_spmd

F32 = mybir.dt.float32
BF16 = mybir.dt.bfloat16
AF = mybir.ActivationFunctionType
ALU = mybir.AluOpType
AX = mybir.AxisListType

NCORES = 8
NB = 2
LC = 256
T = 2048
L = LC + T
S = NB * L
D = 1024
DEPTH = 4
EPS = 1e-6
NFM = 47
NTMC = 1040
NV = 112
NR = 16 + 512 + 20 + 512
ATT_SCALE = 96 ** -0.5
QS = 128 ** -0.5
NDS = 12
SAME_ENGINE_SYNC = True

C_MQ, C_MK, C_MO, C_AQ, C_AKV, C_KRA, C_KRB, C_GU, C_BRA, C_BRB, C_BRC = 0, 4, 8, 12, 15, 17, 18, 19, 23, 31, 39
V_N1, V_N2, V_BADA, V_CONV, V_MNORM, V_QN, V_KVN, V_FN = 0, 8, 16, 64, 88, 92, 95, 97
R_GB, R_VN, R_RB, R_BS = 0, 16, 528, 548


def token_tiles():
    tl = []
    for b in range(NB):
        tl.append((b * L, LC, 2, b))
        for i in range(T // 512):
            tl.append((b * L + LC + i * 512, 512, b, b))
    return tl


class MK:
    def __init__(self, nc, st):
        self.nc = nc
        self.E = {"pe": nc.tensor, "act": nc.scalar, "dve": nc.vector, "pool": nc.gpsimd, "sp": nc.sync}
        self.sem = {e: st.enter_context(nc.semaphore("s_" + e)) for e in self.E}
        self.cnt = {e: 0 for e in self.E}
        self.dq = ("sp", "act", "pool")
        self.dsem = {q: [st.enter_context(nc.semaphore(f"d_{q}{i}")) for i in range(NDS)] for q in self.dq}
        self.dcnt = {q: [0] * NDS for q in self.dq}
        self.dnext = {q: 0 for q in self.dq}
        self.waited = {}
        self.W = {}
        self.R = {}
        self.pending = {e: {} for e in self.E}
        self.ninst = 0

    def _semh(self, sk):
        return self.sem[sk[1]] if sk[0] == "e" else self.dsem[sk[1]][sk[2]]

    def _wait(self, eng, sk, val):
        if val <= 0:
            return
        if sk == ("e", eng) and (eng == "pe" or not SAME_ENGINE_SYNC):
            return
        k = (eng, sk)
        if self.waited.get(k, 0) >= val:
            return
        self.waited[k] = val
        self.E[eng].wait_ge(self._semh(sk), val)
        self.ninst += 1

    def _pre(self, eng, r, w):
        pend = self.pending[eng]
        if pend:
            for sk, v in pend.items():
                self._wait(eng, sk, v)
            self.pending[eng] = {}
        for k in r:
            for sk, v in self.W.get(k, {}).items():
                self._wait(eng, sk, v)
        for k in w:
            for sk, v in self.W.get(k, {}).items():
                self._wait(eng, sk, v)
            for sk, v in self.R.get(k, {}).items():
                self._wait(eng, sk, v)

    def _post(self, sk, val, r, w):
        for k in r:
            d = self.R.setdefault(k, {})
            d[sk] = max(d.get(sk, 0), val)
        for k in w:
            self.W[k] = {sk: val}
            self.R[k] = {}

    def op(self, eng, fn, r=(), w=()):
        self._pre(eng, r, w)
        ins = fn(self.E[eng])
        self.cnt[eng] += 1
        ins.then_inc(self.sem[eng], 1)
        self.ninst += 1
        self._post(("e", eng), self.cnt[eng], r, w)

    def dma(self, q, out, in_, r=(), w=(), **kw):
        i = self.dnext[q]
        self.dnext[q] = (i + 1) % NDS
        sk = ("d", q, i)
        self._wait(q, sk, self.dcnt[q][i])
        self._pre(q, r, w)
        ins = self.E[q].dma_start(out=out, in_=in_, **kw)
        self.dcnt[q][i] += 16
        ins.then_inc(self.dsem[q][i], 16)
        self.ninst += 1
        self._post(sk, self.dcnt[q][i], r, w)

    def barrier(self):
        snap = {}
        for e in self.E:
            snap[("e", e)] = self.cnt[e]
        for q in self.dq:
            for i in range(NDS):
                snap[("d", q, i)] = self.dcnt[q][i]
        for e in self.E:
            self.pending[e] = dict(snap)
        self.W = {}
        self.R = {}

    def finish(self):
        self.barrier()
        for e in self.E:
            for sk, v in self.pending[e].items():
                if sk == ("e", e):
                    continue
                k = (e, sk)
                if self.waited.get(k, 0) >= v or v <= 0:
                    continue
                self.waited[k] = v
                self.E[e].wait_ge(self._semh(sk), v)
            self.pending[e] = {}

    def mm(self, out, lhsT, rhs, start, stop, r=(), w=()):
        self.op("pe", lambda e: e.matmul(out, lhsT=lhsT, rhs=rhs, start=start, stop=stop), r=r, w=w)

    def tr(self, out, in_, ident, r=(), w=()):
        self.op("pe", lambda e: e.transpose(out, in_, ident), r=r, w=w)

    def act(self, out, in_, func, r=(), w=(), bias=None, scale=None, eng="act"):
        kw = {}
        if bias is not None:
            kw["bias"] = bias
        if scale is not None:
            kw["scale"] = scale
        self.op(eng, lambda e: e.activation(out=out, in_=in_, func=func, **kw), r=r, w=w)

    def tt(self, eng, out, in0, in1, op, r=(), w=()):
        self.op(eng, lambda e: e.tensor_tensor(out=out, in0=in0, in1=in1, op=op), r=r, w=w)

    def ts(self, eng, out, in0, s1, op0, s2=None, op1=None, r=(), w=()):
        kw = dict(out=out, in0=in0, scalar1=s1, scalar2=s2, op0=op0)
        if op1 is not None:
            kw["op1"] = op1
        self.op(eng, lambda e: e.tensor_scalar(**kw), r=r, w=w)

    def stt(self, out, in0, scalar, in1, op0, op1, r=(), w=(), eng="dve"):
        self.op(eng, lambda e: e.scalar_tensor_tensor(out=out, in0=in0, scalar=scalar, in1=in1, op0=op0, op1=op1),
                r=r, w=w)

    def cp(self, eng, out, in_, r=(), w=()):
        if eng == "act":
            self.op(eng, lambda e: e.copy(out=out, in_=in_), r=r, w=w)
        else:
            self.op(eng, lambda e: e.tensor_copy(out=out, in_=in_), r=r, w=w)

    def memset(self, eng, ap, val, w=()):
        self.op(eng, lambda e: e.memset(ap, val), w=w)

    def recip(self, out, in_, r=(), w=()):
        self.op("dve", lambda e: e.reciprocal(out=out, in_=in_), r=r, w=w)


class Stage:
    def __init__(self, mk, name):
        self.mk = mk
        self.name = name
        self.es = ExitStack()

    def sb(self, nm, shape, dt):
        return self.es.enter_context(self.mk.nc.sbuf_tensor(f"{self.name}_{nm}", list(shape), dt))

    def ps(self, nm, shape, dt):
        return self.es.enter_context(self.mk.nc.psum_tensor(f"{self.name}_{nm}", list(shape), dt))

    def close(self):
        self.mk.barrier()
        self.es.close()


def build(nlayers=DEPTH, dbg=(), stop=None):
    nc = bass.Bass("TRN2", target_bir_lowering=False)
    dbg = set(dbg)

    def din(name, shape, dt=F32):
        return nc.dram_tensor(name, list(shape), dt, kind="ExternalInput").ap()

    def dscr(name, shape, dt):
        kind = "ExternalOutput" if name in dbg else "Internal"
        return nc.dram_tensor(name, list(shape), dt, kind=kind).ap()

    xin = din("xin", [S, D])
    cv = din("cv", [128, 8, 4])
    vecs = din("vecs", [DEPTH, 128, NV])
    rows = din("rows", [DEPTH, 1, NR])
    w_ada = din("w_ada", [DEPTH, D, 6 * D])
    w_in_r = din("w_in_r", [DEPTH, D, NFM * 128 + NTMC])
    wuq_r = din("wuq_r", [DEPTH, 384, 1536])
    wukv_r = din("wukv_r", [DEPTH, 256, 1024])
    gws_r = din("gws_r", [DEPTH, 128, 512])
    w_pa = din("w_pa", [DEPTH, 512, D])
    w_pb = din("w_pb", [DEPTH, 512, D])
    w_pc = din("w_pc", [DEPTH, 512, D])
    w_out = din("w_out", [DEPTH, D, D])
    rw = din("rw", [DEPTH, D, 20])
    e_w1 = din("e_w1", [DEPTH, 16, D, 512])
    e_w3 = din("e_w3", [DEPTH, 16, D, 512])
    e_w2 = din("e_w2", [DEPTH, 16, 512, D])
    c_ident = din("c_ident", [128, 128])
    c_trif = din("c_trif", [128, 128])
    c_trib = din("c_trib", [128, 128])
    c_maskf = din("c_maskf", [128, 128])
    c_maskb = din("c_maskb", [128, 128])
    c_cc = din("c_cc", [128, L])
    c_ss = din("c_ss", [128, L])
    out = nc.dram_tensor("out", [NB, T, D], F32, kind="ExternalOutput").ap()

    XT = dscr("XT", [8, 128, S], F32)
    HT = dscr("HT", [8, 128, S], BF16)
    ZF = dscr("ZF", [NFM, 128, S], BF16)
    ZTV = dscr("ZTV", [S, 512], BF16)
    ZTG = dscr("ZTG", [S, 16], F32)
    ZTGV = dscr("ZTGV", [S, 512], BF16)
    YA = dscr("YA", [4, 128, S], BF16)
    YB = dscr("YB", [4, 128, S], BF16)
    YC = dscr("YC", [4, 128, S], BF16)
    COMB = dscr("COMB", [S, 16], F32)
    MODD = dscr("MODD", [128, DEPTH * 48 * 4], F32)

    TILES = token_tiles()

    with ExitStack() as st:
        mk = MK(nc, st)
        gsb = lambda nm, shape, dt: st.enter_context(nc.sbuf_tensor(nm, list(shape), dt))
        ident_f = gsb("ident_f", [128, 128], F32)
        ident_b = gsb("ident_b", [128, 128], BF16)
        ones_b = gsb("ones_b", [128, 128], BF16)
        ones_f = gsb("ones_f", [128, 128], F32)
        VEC = gsb("VEC", [128, DEPTH, NV], F32)
        MOD = gsb("MOD", [128, DEPTH, 48, 4], F32)
        AB = gsb("AB", [128, DEPTH, 2, 4, 8], F32)

        mk.dma("sp", out=ident_f[:], in_=c_ident[:, :], w=["ident_f"])
        mk.dma("pool", out=ident_b[:], in_=c_ident[:, :], w=["ident_b"])
        mk.memset("dve", ones_b[:], 1.0, w=["ones_b"])
        mk.memset("dve", ones_f[:], 1.0, w=["ones_f"])
        mk.dma("sp", out=VEC[:], in_=vecs.rearrange("l p v -> p l v"), w=["VEC"])
        mk.barrier()

        def check_stop(l, name):
            return stop is not None and stop == (l, name)

        def stage_load_x():
            sg = Stage(mk, "ldx")
            xi = [sg.sb(f"xi{i}", [128, D], F32) for i in range(2)]
            xo = [sg.sb(f"xo{i}", [128, 8, 128], F32) for i in range(2)]
            pt = [[sg.ps(f"pt{i}{h}", [128, 512], F32) for h in range(2)] for i in range(2)]
            mk.dma("sp", out=xi[0][:], in_=xin[0:128, :], w=["xi0"])
            for blk in range(S // 128):
                p = blk % 2
                if blk + 1 < S // 128:
                    mk.dma("sp", out=xi[(blk + 1) % 2][:], in_=xin[(blk + 1) * 128:(blk + 2) * 128, :],
                           w=[f"xi{(blk + 1) % 2}"])
                for h in range(2):
                    for jj in range(4):
                        j = h * 4 + jj
                        mk.tr(pt[p][h][:, jj * 128:(jj + 1) * 128], xi[p][:, j * 128:(j + 1) * 128], ident_f[:],
                              r=[f"xi{p}"], w=[f"pt{p}{h}"])
                    mk.cp("dve" if h == 0 else "act", xo[p][:, h * 4:(h + 1) * 4, :],
                          pt[p][h][:].rearrange("p (j t) -> p j t", j=4), r=[f"pt{p}{h}"], w=[f"xo{p}{h}"])
                mk.dma("sp", out=XT[:, :, blk * 128:(blk + 1) * 128].rearrange("j p t -> p j t"), in_=xo[p][:],
                       r=[f"xo{p}0", f"xo{p}1"])
            sg.close()

        def stage_ada():
            sg = Stage(mk, "ada")
            cvt = sg.sb("cvt", [128, 8, 4], F32)
            sct = sg.sb("sct", [128, 8, 4], F32)
            wb = [sg.sb(f"wb{i}", [128, 8, 1024], F32) for i in range(2)]
            pp = [sg.ps(f"pp{i}", [128, 512], F32) for i in range(2)]
            mk.dma("sp", out=cvt[:], in_=cv[:, :, :], w=["cvt"])
            mk.act(sct[:], cvt[:], AF.Silu, r=["cvt"], w=["sct"])
            it = 0
            for l in range(nlayers):
                for g in range(6):
                    p = it % 2
                    it += 1
                    mk.dma("sp", out=wb[p][:],
                           in_=w_ada[l, :, g * 1024:(g + 1) * 1024].rearrange("(kc p) n -> p kc n", p=128),
                           w=[f"wb{p}"])
                    for j in range(8):
                        q = j % 2
                        for kc in range(8):
                            mk.mm(pp[q][:, 0:4], wb[p][:, kc, j * 128:(j + 1) * 128], sct[:, kc, :], kc == 0, kc == 7,
                                  r=[f"wb{p}", "sct"], w=[f"pp{q}"])
                        col = V_BADA + g * 8 + j
                        mk.ts("dve", MOD[:, l, g * 8 + j, :], pp[q][:, 0:4], VEC[:, l, col:col + 1], ALU.add,
                              r=[f"pp{q}"], w=["MOD"])
                for which, scc, nv in ((0, 8, V_N1), (1, 32, V_N2)):
                    for vi in range(3):
                        mk.stt(AB[:, l, which, vi, :], MOD[:, l, scc:scc + 8, vi], 1.0, VEC[:, l, nv:nv + 8],
                               ALU.add, ALU.mult, r=["MOD"], w=["AB"])
            if "MODD" in dbg:
                mk.dma("sp", out=MODD[:, :], in_=MOD[:].rearrange("p l c v -> p (l c v)"), r=["MOD"])
            sg.close()

        def stage_norm(l, which, router):
            sg = Stage(mk, f"nm{l}{which}")
            shc = 0 if which == 0 else 24
            xt = [sg.sb(f"xt{i}", [128, 8, 512], F32) for i in range(2)]
            sq = [sg.sb(f"sq{i}", [128, 8, 512], BF16) for i in range(2)]
            rt = [sg.sb(f"rt{i}", [128, 512], F32) for i in range(2)]
            hb = [sg.sb(f"hb{i}", [128, 8, 512], BF16) for i in range(2)]
            pss = [sg.ps(f"pss{i}", [128, 512], F32) for i in range(2)]
            if router:
                hf = [sg.sb(f"hf{i}", [128, 8, 512], F32) for i in range(2)]
                rwt = sg.sb("rwt", [128, 8, 20], F32)
                rbb = sg.sb("rbb", [128, 20], F32)
                psr = [sg.ps(f"psr{i}", [128, 512], F32) for i in range(2)]
                lg = [sg.sb(f"lg{i}", [128, 4, 20], F32) for i in range(2)]
                wk = {n: [sg.sb(f"{n}{i}", shp, F32) for i in range(2)] for n, shp in (
                    ("gmx", [128, 4, 1]), ("goh", [128, 4, 4]), ("gex", [128, 4, 4]), ("gsm", [128, 4, 1]),
                    ("em", [128, 4, 16]), ("t1", [128, 4, 1]), ("m1", [128, 4, 16]), ("em2", [128, 4, 16]),
                    ("t2", [128, 4, 1]), ("m2", [128, 4, 16]), ("w1", [128, 4, 1]), ("cmb", [128, 4, 16]))}
                mk.dma("sp", out=rwt[:], in_=rw[l].rearrange("(kc p) n -> p kc n", p=128), w=["rwt"])
                mk.dma("sp", out=rbb[:], in_=rows[l, 0:1, R_RB:R_RB + 20].to_broadcast([128, 20]), w=["rbb"])
            TL = [t_ for t_ in TILES if not (which == 1 and l == DEPTH - 1 and t_[2] == 2)]

            def ld_(i):
                n0_, sz_, vi_, b_ = TL[i]
                p_ = i % 2
                mk.dma("sp", out=xt[p_][:, :, :sz_], in_=XT[:, :, n0_:n0_ + sz_].rearrange("j p t -> p j t"),
                       w=[f"xt{p_}"] + [f"xs{p_}{j}" for j in range(8)])
            ld_(0)
            for i, (n0, sz, vi, b) in enumerate(TL):
                p = i % 2
                if i + 1 < len(TL):
                    ld_(i + 1)
                mk.act(sq[p][:, :, :sz], xt[p][:, :, :sz], AF.Square, r=[f"xt{p}"], w=[f"sq{p}"])
                for j in range(8):
                    mk.mm(pss[p][:, :sz], ones_b[:], sq[p][:, j, :sz], j == 0, j == 7, r=[f"sq{p}"], w=[f"pss{p}"])
                mk.act(rt[p][:, :sz], pss[p][:, :sz], AF.Sqrt, bias=EPS, scale=1.0 / D, r=[f"pss{p}"], w=[f"rt{p}"])
                mk.recip(rt[p][:, :sz], rt[p][:, :sz], r=[f"rt{p}"], w=[f"rt{p}"])
                dst = hf[p] if router else hb[p]
                dk = f"hf{p}" if router else f"hb{p}"
                for j in range(8):
                    mk.tt("pool" if j % 2 else "dve", xt[p][:, j, :sz], xt[p][:, j, :sz], rt[p][:, :sz], ALU.mult,
                          r=[f"xt{p}", f"rt{p}"], w=[f"xs{p}{j}"])
                    mk.act(dst[:, j, :sz], xt[p][:, j, :sz], AF.Identity, scale=AB[:, l, which, vi, j:j + 1],
                           bias=MOD[:, l, shc + j, vi:vi + 1], r=[f"xs{p}{j}"], w=[f"{dk}{j}"])
                if router:
                    for hh in range(2):
                        mk.cp("dve" if hh == 0 else "pool", hb[p][:, hh * 4:(hh + 1) * 4, :sz],
                              hf[p][:, hh * 4:(hh + 1) * 4, :sz], r=[f"hf{p}{j}" for j in range(hh * 4, hh * 4 + 4)],
                              w=[f"hb{p}_{hh}"])
                    hbk = [f"hb{p}_0", f"hb{p}_1"]
                else:
                    hbk = [f"hb{p}{j}" for j in range(8)]
                mk.dma("act", out=HT[:, :, n0:n0 + sz].rearrange("j p t -> p j t"), in_=hb[p][:, :, :sz], r=hbk)
                if router:
                    nb = sz // 128
                    K = lambda n: f"{n}{p}"
                    for bb in range(nb):
                        for kc in range(8):
                            mk.mm(psr[p][:, bb * 20:(bb + 1) * 20], hf[p][:, kc, bb * 128:(bb + 1) * 128],
                                  rwt[:, kc, :], kc == 0, kc == 7, r=[f"hf{p}{kc}", "rwt"], w=[K("psr")])
                    LG = lg[p][:, :nb, :]
                    mk.tt("dve", LG, psr[p][:, 0:nb * 20].rearrange("p (b n) -> p b n", n=20),
                          rbb[:].unsqueeze(1).to_broadcast([128, nb, 20]), ALU.add, r=[K("psr"), "rbb"], w=[K("lg")])
                    G = lg[p][:, :nb, 0:4]
                    EX = lg[p][:, :nb, 4:20]
                    W_ = {n: wk[n][p][:, :nb, :] for n in wk}
                    mk.op("dve", lambda e: e.tensor_reduce(out=W_["gmx"], in_=G, axis=AX.X, op=ALU.max),
                          r=[K("lg")], w=[K("gmx")])
                    mk.tt("dve", W_["goh"], G, W_["gmx"].to_broadcast([128, nb, 4]), ALU.is_equal,
                          r=[K("lg"), K("gmx")], w=[K("goh")])
                    mk.tt("dve", W_["gex"], G, W_["gmx"].to_broadcast([128, nb, 4]), ALU.subtract,
                          r=[K("lg"), K("gmx")], w=[K("gex")])
                    mk.act(W_["gex"], W_["gex"], AF.Exp, r=[K("gex")], w=[K("gex")])
                    mk.op("dve", lambda e: e.tensor_reduce(out=W_["gsm"], in_=W_["gex"], axis=AX.X, op=ALU.add),
                          r=[K("gex")], w=[K("gsm")])
                    mk.recip(W_["gsm"], W_["gsm"], r=[K("gsm")], w=[K("gsm")])
                    mk.ts("dve", W_["goh"], W_["goh"], 1e4, ALU.mult, -1e4, ALU.add, r=[K("goh")], w=[K("goh")])
                    mk.tt("dve", wk["em"][p][:, :nb, :].rearrange("p b (g e) -> p b g e", g=4),
                          EX.rearrange("p b (g e) -> p b g e", g=4),
                          W_["goh"].unsqueeze(3).to_broadcast([128, nb, 4, 4]), ALU.add,
                          r=[K("lg"), K("goh")], w=[K("em")])
                    mk.op("dve", lambda e: e.tensor_reduce(out=W_["t1"], in_=W_["em"], axis=AX.X, op=ALU.max),
                          r=[K("em")], w=[K("t1")])
                    mk.tt("dve", W_["m1"], W_["em"], W_["t1"].to_broadcast([128, nb, 16]), ALU.is_equal,
                          r=[K("em"), K("t1")], w=[K("m1")])
                    mk.stt(W_["em2"], W_["m1"], -1e4, W_["em"], ALU.mult, ALU.add, r=[K("m1"), K("em")], w=[K("em2")])
                    mk.op("dve", lambda e: e.tensor_reduce(out=W_["t2"], in_=W_["em2"], axis=AX.X, op=ALU.max),
                          r=[K("em2")], w=[K("t2")])
                    mk.tt("dve", W_["m2"], W_["em2"], W_["t2"].to_broadcast([128, nb, 16]), ALU.is_equal,
                          r=[K("em2"), K("t2")], w=[K("m2")])
                    mk.tt("dve", W_["w1"], W_["t1"], W_["t2"], ALU.subtract, r=[K("t1"), K("t2")], w=[K("w1")])
                    mk.act(W_["w1"], W_["w1"], AF.Sigmoid, r=[K("w1")], w=[K("w1")])
                    mk.tt("dve", W_["m1"], W_["m1"], W_["m2"], ALU.subtract, r=[K("m1"), K("m2")], w=[K("m1")])
                    mk.tt("dve", W_["m1"], W_["m1"], W_["w1"].to_broadcast([128, nb, 16]), ALU.mult,
                          r=[K("m1"), K("w1")], w=[K("m1")])
                    mk.tt("dve", W_["m1"], W_["m1"], W_["m2"], ALU.add, r=[K("m1"), K("m2")], w=[K("m1")])
                    mk.tt("dve", W_["cmb"], W_["m1"], W_["gsm"].to_broadcast([128, nb, 16]), ALU.mult,
                          r=[K("m1"), K("gsm")], w=[K("cmb")])
                    mk.dma("sp", out=COMB[n0:n0 + sz, :].rearrange("(b p) e -> p b e", p=128), in_=W_["cmb"],
                           r=[K("cmb")])
            sg.close()

        def stage_z(l):
            sg = Stage(mk, f"z{l}")
            ht = sg.sb("ht", [128, 8, S], BF16)
            wf = [sg.sb(f"wf{i}", [128, 8, 512], BF16) for i in range(2)]
            wt = sg.sb("wt", [128, 8, NTMC], BF16)
            zo = [sg.sb(f"zo{i}", [128, 4, 512], BF16) for i in range(2)]
            zv = [sg.sb(f"zv{i}", [128, 4, 512], BF16) for i in range(2)]
            zgv = [sg.sb(f"zgv{i}", [128, 4, 512], BF16) for i in range(2)]
            zg = [sg.sb(f"zg{i}", [128, 4, 16], F32) for i in range(2)]
            pz = [sg.ps(f"pz{i}", [128, 512], F32) for i in range(4)]
            pv = [sg.ps(f"pv{i}", [128, 512], F32) for i in range(2)]
            pg = sg.ps("pg", [128, 512], F32)
            for i, (n0, sz, vi, b) in enumerate(TILES):
                mk.dma("sp", out=ht[:, :, n0:n0 + sz], in_=HT[:, :, n0:n0 + sz].rearrange("j p t -> p j t"),
                       w=[f"ht{i}"])
            mk.dma("pool", out=wt[:], in_=w_in_r[l, :, NFM * 128:NFM * 128 + NTMC].rearrange("(kc p) n -> p kc n", p=128),
                   w=["wt"])
            funcs = {}
            for c in range(NFM):
                funcs[c] = AF.Identity
            for c in range(C_MO, C_MO + 4):
                funcs[c] = AF.Sigmoid
            for c in range(C_GU, C_GU + 4):
                funcs[c] = AF.Gelu
            for c in range(C_BRA, NFM):
                funcs[c] = AF.Sigmoid
            groups = [(c0, min(4, NFM - c0)) for c0 in range(0, NFM, 4)]
            pzi = 0
            oi = 0
            def load_wf(gi):
                c0_, ncg_ = groups[gi]
                mk.dma("pool", out=wf[gi % 2][:, :, :ncg_ * 128],
                       in_=w_in_r[l, :, c0_ * 128:(c0_ + ncg_) * 128].rearrange("(kc p) n -> p kc n", p=128),
                       w=[f"wf{gi % 2}"])
            load_wf(0)
            for gi, (c0, ncg) in enumerate(groups):
                p = gi % 2
                if gi >= 1 and gi + 1 < len(groups):
                    load_wf(gi + 1)
                if gi == 0:
                    load_wf(1)
                dead_ctx = l == DEPTH - 1 and (C_MO <= c0 < C_MO + 4 or c0 >= C_GU + 1)
                for i, (n0, sz, vi, b) in enumerate(TILES):
                    if dead_ctx and vi == 2:
                        continue
                    o = oi % 2
                    oi += 1
                    for ci in range(ncg):
                        q = pzi % 4
                        pzi += 1
                        for kc in range(8):
                            mk.mm(pz[q][:, :sz], wf[p][:, kc, ci * 128:(ci + 1) * 128], ht[:, kc, n0:n0 + sz],
                                  kc == 0, kc == 7, r=[f"wf{p}", f"ht{i}"], w=[f"pz{q}"])
                        mk.act(zo[o][:, ci, :sz], pz[q][:, :sz], funcs[c0 + ci], r=[f"pz{q}"], w=[f"zo{o}_{ci}"])
                    mk.dma("sp", out=ZF[c0:c0 + ncg, :, n0:n0 + sz].rearrange("j p t -> p j t"),
                           in_=zo[o][:, :ncg, :sz], r=[f"zo{o}_{ci}" for ci in range(ncg)])
            for i, (n0, sz, vi, b) in enumerate(TILES):
                o = i % 2
                nb = sz // 128
                for bb in range(nb):
                    t0 = n0 + bb * 128
                    q = bb % 2
                    for kc in range(8):
                        mk.mm(pv[q][:, :], ht[:, kc, t0:t0 + 128], wt[:, kc, 0:512], kc == 0, kc == 7,
                              r=[f"ht{i}", "wt"], w=[f"pv{q}"])
                    mk.cp("dve", zv[o][:, bb, :], pv[q][:, :], r=[f"pv{q}"], w=[f"zv{o}_{bb}"])
                    for kc in range(8):
                        mk.mm(pz[q][:, :], ht[:, kc, t0:t0 + 128], wt[:, kc, 528:1040], kc == 0, kc == 7,
                              r=[f"ht{i}", "wt"], w=[f"pz{q}"])
                    mk.act(zgv[o][:, bb, :], pz[q][:, :], AF.Gelu, r=[f"pz{q}"], w=[f"zgv{o}_{bb}"])
                    for kc in range(8):
                        mk.mm(pg[:, bb * 16:(bb + 1) * 16], ht[:, kc, t0:t0 + 128], wt[:, kc, 512:528], kc == 0, kc == 7,
                              r=[f"ht{i}", "wt"], w=["pg"])
                mk.cp("dve", zg[o][:, :nb, :], pg[:, 0:nb * 16].rearrange("p (b n) -> p b n", n=16), r=["pg"],
                      w=[f"zg{o}"])
                mk.dma("sp", out=ZTV[n0:n0 + sz, :].rearrange("(b p) f -> p b f", p=128), in_=zv[o][:, :nb, :],
                       r=[f"zv{o}_{bb}" for bb in range(nb)])
                mk.dma("sp", out=ZTGV[n0:n0 + sz, :].rearrange("(b p) f -> p b f", p=128), in_=zgv[o][:, :nb, :],
                       r=[f"zgv{o}_{bb}" for bb in range(nb)])
                mk.dma("sp", out=ZTG[n0:n0 + sz, :].rearrange("(b p) f -> p b f", p=128), in_=zg[o][:, :nb, :],
                       r=[f"zg{o}"])
            sg.close()

        def stage_mlstm(l, b):
            sg = Stage(mk, f"ml{l}{b}")
            NC_ = L // 128
            s0 = b * L
            raw = [sg.sb(f"raw{i}", [128, L], BF16) for i in range(2)]
            qk = sg.sb("qk", [128, 8, L], BF16)
            ktm = sg.sb("ktm", [128, NC_, 4, 128], BF16)
            vaug = sg.sb("vaug", [128, NC_, 4, 128], BF16)
            dg = sg.sb("dg", [128, 8, 3, 128], BF16)
            gt = sg.sb("gt", [128, NC_, 16], F32)
            gbb = sg.sb("gbb", [128, 16], F32)
            lt = sg.sb("lt", [128, NC_, 8], F32)
            cl = sg.sb("cl", [128, NC_, 8], F32)
            wsx = sg.sb("wsx", [128, NC_, 8], F32)
            flo = sg.sb("flo", [128, NC_, 8], F32)
            ebe = sg.sb("ebe", [128, NC_, 8], F32)
            ebs = sg.sb("ebs", [128, NC_, 8], F32)
            trif = sg.sb("trif", [128, 128], F32)
            trib = sg.sb("trib", [128, 128], F32)
            maskf = sg.sb("maskf", [128, 128], F32)
            maskb = sg.sb("maskb", [128, 128], F32)
            hX = [sg.sb("hX0", [128, NC_, 4, 128], F32), sg.sb("hX1", [128, NC_, 4, 128], BF16)]
            smo = sg.sb("smo", [128, 4, L], BF16)
            U = [sg.sb(f"U{i}", [128, 4, 130], F32) for i in range(2)]
            CTb = [[sg.sb(f"CTb{d}{i}", [128, 4, 130], BF16) for i in range(2)] for d in range(2)]
            qkt = [[sg.sb(f"qkt{d}{i}", [128, 4, 128], BF16) for i in range(2)] for d in range(2)]
            vt = [[sg.sb(f"vt{d}{i}", [128, 4, 130], BF16) for i in range(2)] for d in range(2)]
            adn = [sg.sb(f"adn{i}", [128, 4, 2], F32) for i in range(2)]
            rin = [sg.sb(f"rin{i}", [128, 4], F32) for i in range(2)]
            pst = [sg.ps(f"pst{i}", [128, 512], F32) for i in range(2)]
            pnm = [sg.ps(f"pnm{i}", [128, 512], F32) for i in range(2)]
            ppp = [sg.ps(f"ppp{i}", [128, 512], F32) for i in range(2)]
            psm = [sg.ps(f"psm{i}", [128, 512], F32) for i in range(2)]
            ptrv = [ppp[i][:].bitcast(BF16) for i in range(2)]

            mk.dma("sp", out=smo[:], in_=ZF[C_MO:C_MO + 4, :, s0:s0 + L].rearrange("j p t -> p j t"), w=["smo"])
            mk.dma("sp", out=vaug[:].rearrange("p c h e -> p c (h e)"),
                   in_=ZTV[s0:s0 + L, :].rearrange("(c p) f -> p c f", p=128), w=["vaug"])
            mk.dma("sp", out=gt[:], in_=ZTG[s0:s0 + L, :].rearrange("(c p) n -> p c n", p=128), w=["gt"])
            mk.dma("sp", out=gbb[:], in_=rows[l, 0:1, R_GB:R_GB + 16].to_broadcast([128, 16]), w=["gbb"])
            for tdst, tsrc, nm in ((trif, c_trif, "trif"), (trib, c_trib, "trib"), (maskf, c_maskf, "maskf"),
                                   (maskb, c_maskb, "maskb")):
                mk.dma("sp", out=tdst[:], in_=tsrc[:, :], w=[nm])
            for d in range(2):
                mk.memset("pool", U[d][:], 0.0, w=[f"U{d}"])
                mk.memset("pool", CTb[d][0][:], 0.0, w=[f"CTb{d}0"])
                mk.memset("pool", CTb[d][1][:], 0.0, w=[f"CTb{d}1"])
            seq_tiles_ = [(0, LC)] + [(LC + i * 512, 512) for i in range(4)]
            for j in range(8):
                for tap in range(3):
                    mk.ts("pool" if tap == 1 else "dve", dg[:, j, tap, :], ident_f[:],
                          VEC[:, l, V_CONV + j * 3 + tap:V_CONV + j * 3 + tap + 1], ALU.mult, r=["ident_f"], w=[f"dg{j}"])
            ci = 0
            for j in range(8):
                rw_ = raw[j % 2]
                rk = f"raw{j % 2}"
                mk.dma("sp", out=rw_[:], in_=ZF[C_MQ + j, :, s0:s0 + L], w=[rk])
                for (t0, sz) in seq_tiles_:
                    t1_ = t0 + sz
                    sa, sb_ = (0, LC) if t0 < LC else (LC, L)
                    u = ci % 2
                    ci += 1
                    pc_ = pnm[u]
                    mk.mm(pc_[:, 0:sz], dg[:, j, 1, :], rw_[:, t0:t1_], True, False, r=[f"dg{j}", rk], w=[f"pnm{u}"])
                    lo = max(t0, sa + 1)
                    mk.mm(pc_[:, lo - t0:sz], dg[:, j, 0, :], rw_[:, lo - 1:t1_ - 1], False, False, r=[f"dg{j}", rk],
                          w=[f"pnm{u}"])
                    hi = min(t1_, sb_ - 1)
                    mk.mm(pc_[:, 0:hi - t0], dg[:, j, 2, :], rw_[:, t0 + 1:hi + 1], False, True, r=[f"dg{j}", rk],
                          w=[f"pnm{u}"])
                    mk.act(qk[:, j, t0:t1_], pc_[:, 0:sz], AF.Silu, r=[f"pnm{u}"], w=[f"qk{j}"])
            for c in range(NC_):
                p = c % 2
                for h in range(4):
                    mk.tr(ptrv[p][:, h * 128:(h + 1) * 128], qk[:, 4 + h, c * 128:(c + 1) * 128],
                          ident_b[:], r=[f"qk{4 + h}", "ident_b"], w=[f"ppp{p}"])
                mk.cp("dve" if c % 2 else "act", ktm[:, c, :, :],
                      ptrv[p][:, 0:512].rearrange("p (h d) -> p h d", h=4), r=[f"ppp{p}"], w=[f"ktm{c}"])
            mk.tt("dve", gt[:], gt[:], gbb[:].unsqueeze(1).to_broadcast([128, NC_, 16]), ALU.add, r=["gt", "gbb"],
                  w=["gt"])
            mk.act(lt[:, :, 0:4], gt[:, :, 4:8], AF.Exp, scale=-1.0, r=["gt"], w=["lt"])
            mk.act(lt[:, :, 4:8], gt[:, :, 12:16], AF.Exp, scale=-1.0, r=["gt"], w=["lt"])
            mk.act(lt[:], lt[:], AF.Ln, bias=1.0, r=["lt"], w=["lt"])
            pcs = pst[0]
            ptot = pst[1]
            for c in range(NC_):
                mk.mm(pcs[:, c * 8:c * 8 + 4], trif[:], lt[:, c, 0:4], True, True, r=["trif", "lt"], w=["pst0"])
                mk.mm(pcs[:, c * 8 + 4:c * 8 + 8], trib[:], lt[:, c, 4:8], True, True, r=["trib", "lt"], w=["pst0"])
                mk.mm(ptot[:, c * 8:c * 8 + 8], ones_f[:], lt[:, c, :], True, True, r=["ones_f", "lt"], w=["pst1"])
            mk.cp("dve", cl[:], pcs[:, 0:NC_ * 8].rearrange("p (c n) -> p c n", n=8), r=["pst0"], w=["cl"])
            mk.act(flo[:], cl[:], AF.Exp, r=["cl"], w=["flo"])
            mk.act(ebe[:], ptot[:, 0:NC_ * 8].rearrange("p (c n) -> p c n", n=8), AF.Exp, scale=-1.0, r=["pst1"],
                   w=["ebe"])
            mk.ts("dve", ebs[:], ebe[:], QS, ALU.mult, r=["ebe"], w=["ebs"])
            mk.tt("dve", wsx[:, :, 0:4], gt[:, :, 0:4], cl[:, :, 0:4], ALU.add, r=["gt", "cl"], w=["wsx"])
            mk.tt("dve", wsx[:, :, 4:8], gt[:, :, 8:12], cl[:, :, 4:8], ALU.add, r=["gt", "cl"], w=["wsx"])
            mk.act(wsx[:], wsx[:], AF.Exp, r=["wsx"], w=["wsx"])
            order = [list(range(NC_)), [1, 0] + list(range(NC_ - 1, 1, -1))]

            def ctx_(step, d):
                c = order[d][step]
                return c, slice(c * 128, (c + 1) * 128), slice(d * 4, d * 4 + 4), f"qkt{d}{step % 2}", f"vt{d}{step % 2}"

            def a1(step):
                sp_ = step % 2
                for d in range(2):
                    c, cs, lns, qkk, vtk = ctx_(step, d)
                    mask = maskf if d == 0 else maskb
                    mkey = "maskf" if d == 0 else "maskb"
                    for h in range(4):
                        mk.mm(pst[d][:, h * 128:(h + 1) * 128], qk[:, 4 + h, cs], qk[:, h, cs], True, True,
                              r=[f"qk{h}", f"qk{4 + h}"], w=[f"pst{d}"])
                    mk.tt("dve", qkt[d][sp_][:], pst[d][:].rearrange("p (h t) -> p h t", h=4),
                          mask[:].unsqueeze(1).to_broadcast([128, 4, 128]), ALU.mult, r=[f"pst{d}", mkey], w=[qkk])
                    mk.tt("pool", vt[d][sp_][:, :, 0:128], vaug[:, c, :, :],
                          wsx[:, c, lns].unsqueeze(2).to_broadcast([128, 4, 128]), ALU.mult, r=["vaug", "wsx"], w=[vtk])
                    mk.cp("pool", vt[d][sp_][:, :, 128:130], wsx[:, c, lns].unsqueeze(2).to_broadcast([128, 4, 2]),
                          r=["wsx", vtk], w=[vtk])

            def a2(step):
                sp_ = step % 2
                for d in range(2):
                    c, cs, lns, qkk, vtk = ctx_(step, d)
                    for h in range(4):
                        mk.mm(ppp[d][:, h * 128:(h + 1) * 128], ktm[:, c, h, :], vt[d][sp_][:, h, 0:128], True, True,
                              r=[f"ktm{c}", vtk], w=[f"ppp{d}"])
                        o2 = d * 8 + h * 2
                        mk.mm(psm[0][:, o2:o2 + 2], ktm[:, c, h, :], vt[d][sp_][:, h, 128:130], True, True,
                              r=[f"ktm{c}", vtk], w=["psm0"])

            def c1(step):
                sp_ = step % 2
                cb_ = (step + 1) % 2
                for d in range(2):
                    c, cs, lns, qkk, vtk = ctx_(step, d)
                    for h in range(4):
                        mk.mm(pnm[d][:, h * 128:(h + 1) * 128], qkt[d][sp_][:, h, :], vt[d][sp_][:, h, 0:128], True, False,
                              r=[qkk, vtk], w=[f"pnm{d}"])
                        mk.mm(pnm[d][:, h * 128:(h + 1) * 128], qk[:, h, cs], CTb[d][cb_][:, h, 0:128], False, True,
                              r=[f"qk{h}", f"CTb{d}{cb_}"], w=[f"pnm{d}"])
                        o1 = d * 8 + h * 2
                        mk.mm(psm[1][:, o1:o1 + 2], qkt[d][sp_][:, h, :], vt[d][sp_][:, h, 128:130], True, False,
                              r=[qkk, vtk], w=["psm1"])
                        mk.mm(psm[1][:, o1:o1 + 2], qk[:, h, cs], CTb[d][cb_][:, h, 128:130], False, True,
                              r=[f"qk{h}", f"CTb{d}{cb_}"], w=["psm1"])

            def c2(step):
                for d in range(2):
                    c, cs, lns, qkk, vtk = ctx_(step, d)
                    denv = psm[1][:, d * 8:d * 8 + 8].rearrange("p (h n) -> p h n", n=2)
                    mk.ts("dve", adn[d][:], denv, -1.0, ALU.mult, r=["psm1"], w=[f"adn{d}"])
                    mk.tt("dve", rin[d][:], denv[:, :, 0], adn[d][:, :, 0], ALU.max, r=["psm1", f"adn{d}"],
                          w=[f"rin{d}"])
                    mk.tt("dve", rin[d][:], rin[d][:], flo[:, c, lns], ALU.max, r=[f"rin{d}", "flo"],
                          w=[f"rin{d}"])
                    mk.recip(rin[d][:], rin[d][:], r=[f"rin{d}"], w=[f"rin{d}"])
                    mk.tt("dve", hX[d][:, c, :, :], pnm[d][:].rearrange("p (h e) -> p h e", h=4),
                          rin[d][:].unsqueeze(2).to_broadcast([128, 4, 128]), ALU.mult, r=[f"pnm{d}", f"rin{d}"],
                          w=[f"hX{d}_{c}"])

            def bb_(step):
                cb_ = step % 2
                for d in range(2):
                    c, cs, lns, qkk, vtk = ctx_(step, d)
                    cprev = order[d][step - 1] if step > 0 else None
                    if cprev is not None:
                        mk.tt("pool", U[d][:], U[d][:], ebe[:, cprev, lns].unsqueeze(2).to_broadcast([128, 4, 130]),
                              ALU.mult, r=[f"U{d}", "ebe"], w=[f"U{d}"])
                    mk.tt("dve", U[d][:, :, 0:128], ppp[d][:].rearrange("p (h e) -> p h e", h=4), U[d][:, :, 0:128],
                          ALU.add, r=[f"ppp{d}", f"U{d}"], w=[f"U{d}"])
                    mk.tt("dve", U[d][:, :, 128:130],
                          psm[0][:, d * 8:d * 8 + 8].rearrange("p (h n) -> p h n", n=2), U[d][:, :, 128:130],
                          ALU.add, r=["psm0", f"U{d}"], w=[f"U{d}"])
                    mk.tt("pool", CTb[d][cb_][:], U[d][:], ebs[:, c, lns].unsqueeze(2).to_broadcast([128, 4, 130]),
                          ALU.mult, r=[f"U{d}", "ebs"], w=[f"CTb{d}{cb_}"])

            import os as _os
            _skip = _os.environ.get("MKSKIP", "")
            for i in range(NC_ + 1 if "scan" not in _skip else 0):
                if i < NC_:
                    a1(i)
                if i >= 1:
                    c1(i - 1)
                if i < NC_:
                    a2(i)
                if i >= 1:
                    c2(i - 1)
                if i < NC_:
                    bb_(i)
            GC = 6
            hsq = sg.sb("hsq", [128, GC, 4, 128], F32)
            ssq = sg.sb("ssq", [128, NC_, 4], F32)
            hn = [sg.sb(f"hn{i}", [128, 4, 128], BF16) for i in range(2)]
            ya = smo
            for g0 in range(0, NC_ if "post" not in _skip else 0, GC):
                gk = f"hsg{g0}"
                xk = [f"hX0_{c}" for c in range(g0, g0 + GC)] + [f"hX1_{c}" for c in range(g0, g0 + GC)]
                mk.tt("pool", hX[0][:, g0:g0 + GC, :, :], hX[0][:, g0:g0 + GC, :, :], hX[1][:, g0:g0 + GC, :, :], ALU.add,
                      r=xk, w=[gk])
                mk.tt("dve", hsq[:], hX[0][:, g0:g0 + GC, :, :], hX[0][:, g0:g0 + GC, :, :], ALU.mult, r=[gk], w=["hsq"])
                mk.op("dve", lambda e: e.tensor_reduce(out=ssq[:, g0:g0 + GC, :], in_=hsq[:], axis=AX.X, op=ALU.add),
                      r=["hsq"], w=[f"ssq{g0}"])
                mk.act(ssq[:, g0:g0 + GC, :], ssq[:, g0:g0 + GC, :], AF.Sqrt, bias=EPS, scale=1.0 / 128, r=[f"ssq{g0}"],
                       w=[f"ssq{g0}"])
                mk.recip(ssq[:, g0:g0 + GC, :], ssq[:, g0:g0 + GC, :], r=[f"ssq{g0}"], w=[f"ssq{g0}"])
                for c in range(g0, g0 + GC):
                    p = c % 2
                    mk.tt("pool", hn[p][:], hX[0][:, c, :, :], ssq[:, c, :].unsqueeze(2).to_broadcast([128, 4, 128]),
                          ALU.mult, r=[gk, f"ssq{g0}"], w=[f"hn{p}"])
                    for h in range(4):
                        mk.tr(ptrv[p][:, h * 128:(h + 1) * 128], hn[p][:, h, :], ident_b[:], r=[f"hn{p}"],
                              w=[f"ppp{p}"])
                    for h in range(4):
                        mk.stt(ya[:, h, c * 128:(c + 1) * 128], ptrv[p][:, h * 128:(h + 1) * 128],
                               VEC[:, l, V_MNORM + h:V_MNORM + h + 1], smo[:, h, c * 128:(c + 1) * 128],
                               ALU.mult, ALU.mult, r=[f"ppp{p}", "smo"], w=[f"ya{c}"])
            mk.dma("sp", out=YA[:, :, s0:s0 + L].rearrange("j p t -> p j t"), in_=ya[:],
                   r=[f"ya{c}" for c in range(NC_)])
            sg.close()

        def stage_mla(l, b):
            sg = Stage(mk, f"at{l}{b}")
            NC_ = L // 128
            s0 = b * L
            seq_tiles = [(0, LC)] + [(LC + i * 512, 512) for i in range(4)]
            aq = sg.sb("aq", [128, 3, L], BF16)
            akv = sg.sb("akv", [128, 2, L], BF16)
            sqb = sg.sb("sqb", [128, 3, 512], BF16)
            rs = [sg.sb(f"rs{i}", [128, 512], F32) for i in range(2)]
            wq = sg.sb("wq", [128, 3, 1536], BF16)
            wkv = sg.sb("wkv", [128, 2, 1024], BF16)
            cc = sg.sb("cc", [128, L], F32)
            ss = sg.sb("ss", [128, L], F32)
            kra = sg.sb("kra", [128, 2, L], BF16)
            krt = [sg.sb(f"krt{i}", [128, 512], F32) for i in range(2)]
            kr = sg.sb("kr", [128, L], BF16)
            QT = [sg.sb(f"QT{i}", [128, L], BF16) for i in range(2)]
            KT = [sg.sb(f"KT{i}", [128, L], BF16) for i in range(2)]
            VA = sg.sb("VA", [128, NC_, 8, 128], BF16)
            t1 = [sg.sb(f"t1{i}", [128, 512], F32) for i in range(2)]
            t2 = [sg.sb(f"t2{i}", [128, 512], F32) for i in range(2)]
            pT = [sg.sb(f"pT{i}", [128, 512], BF16) for i in range(4)]
            rec = [sg.sb(f"rec{i}", [64, 512], F32) for i in range(2)]
            yb = sg.sb("yb", [128, 4, L], BF16)
            pa = [sg.ps("pa0", [128, 512], F32)] * 2
            pb_ = [sg.ps("pb0", [128, 512], F32)] * 2
            psc = [sg.ps(f"psc{i}", [128, 512], F32) for i in range(4)]
            pac = [sg.ps(f"pac{i}", [128, 512], F32) for i in range(2)]

            mk.dma("sp", out=aq[:], in_=ZF[C_AQ:C_AQ + 3, :, s0:s0 + L].rearrange("j p t -> p j t"), w=["aq"])
            mk.dma("sp", out=akv[:], in_=ZF[C_AKV:C_AKV + 2, :, s0:s0 + L].rearrange("j p t -> p j t"), w=["akv"])
            mk.dma("sp", out=kra[:], in_=ZF[C_KRA:C_KRA + 2, :, s0:s0 + L].rearrange("j p t -> p j t"), w=["kra"])
            mk.dma("sp", out=cc[:], in_=c_cc[:, :], w=["cc"])
            mk.dma("sp", out=ss[:], in_=c_ss[:, :], w=["ss"])
            mk.dma("pool", out=wq[:], in_=wuq_r[l].rearrange("(kc p) n -> p kc n", p=128), w=["wq"])
            mk.dma("pool", out=wkv[:], in_=wukv_r[l].rearrange("(kc p) n -> p kc n", p=128), w=["wkv"])
            mk.memset("pool", VA[:, :, :, 64:128], 1.0, w=["VA1"])

            def rms_fm(src, nch, gcol, skey, ti):
                for (a0, sz) in seq_tiles:
                    p = ti[0] % 2
                    ti[0] += 1
                    mk.act(sqb[:, :nch, :sz], src[:, :, a0:a0 + sz], AF.Square, r=[skey], w=["sqb"])
                    for j in range(nch):
                        mk.mm(pa[p][:, :sz], ones_b[:], sqb[:, j, :sz], j == 0, j == nch - 1, r=["sqb"], w=["pa0"])
                    mk.act(rs[p][:, :sz], pa[p][:, :sz], AF.Sqrt, bias=EPS, scale=1.0 / (nch * 128), r=["pa0"],
                           w=[f"rs{p}"])
                    mk.recip(rs[p][:, :sz], rs[p][:, :sz], r=[f"rs{p}"], w=[f"rs{p}"])
                    for j in range(nch):
                        mk.stt(src[:, j, a0:a0 + sz], src[:, j, a0:a0 + sz], VEC[:, l, gcol + j:gcol + j + 1],
                               rs[p][:, :sz], ALU.mult, ALU.mult, r=[skey, f"rs{p}"], w=[skey])
            ti = [0]
            rms_fm(aq, 3, V_QN, "aq", ti)
            rms_fm(akv, 2, V_KVN, "akv", ti)
            for i, (a0, sz) in enumerate(seq_tiles):
                p = i % 2
                mk.tt("dve", krt[p][64:96, :sz], kra[64:96, 0, a0:a0 + sz], cc[64:96, a0:a0 + sz], ALU.mult,
                      r=["kra", "cc"], w=[f"krt{p}"])
                mk.tt("pool", t1[p][64:96, :sz], kra[64:96, 1, a0:a0 + sz], ss[64:96, a0:a0 + sz], ALU.mult,
                      r=["kra", "ss"], w=[f"t1{p}"])
                mk.tt("dve", kr[64:96, a0:a0 + sz], krt[p][64:96, :sz], t1[p][64:96, :sz], ALU.add,
                      r=[f"krt{p}", f"t1{p}"], w=["kr"])
            for c in range(NC_):
                u = c % 2
                for kc in range(2):
                    mk.mm(pac[u][:, :], akv[:, kc, c * 128:(c + 1) * 128], wkv[:, kc, 512:1024], kc == 0, kc == 1,
                          r=["akv", "wkv"], w=[f"pac{u}"])
                mk.cp("dve" if c % 2 else "act", VA[:, c, :, 0:64], pac[u][:, :].rearrange("p (h e) -> p h e", h=8),
                      r=[f"pac{u}"], w=[f"VA{c}"])
            vak = [f"VA{c}" for c in range(NC_)] + ["VA1"]
            ui = [0]

            def proj(h):
                hp = h % 2
                for (a0, sz) in seq_tiles:
                    u = ui[0] % 2
                    ui[0] += 1
                    for kc in range(3):
                        mk.mm(pa[u][0:96, :sz], wq[:, kc, h * 96:(h + 1) * 96], aq[:, kc, a0:a0 + sz], kc == 0, kc == 2,
                              r=["wq", "aq"], w=["pa0"])
                    for kc in range(3):
                        mk.mm(pb_[u][0:96, :sz], wq[:, kc, 768 + h * 96:768 + (h + 1) * 96], aq[:, kc, a0:a0 + sz],
                              kc == 0, kc == 2, r=["wq", "aq"], w=["pb0"])
                    mk.tt("dve", t1[u][0:96, :sz], pa[u][0:96, :sz], cc[0:96, a0:a0 + sz], ALU.mult,
                          r=["pa0", "cc"], w=[f"t1{u}"])
                    mk.tt("dve", t2[u][0:96, :sz], pb_[u][0:96, :sz], ss[0:96, a0:a0 + sz], ALU.mult,
                          r=["pb0", "ss"], w=[f"t2{u}"])
                    mk.tt("pool", QT[hp][0:96, a0:a0 + sz], t1[u][0:96, :sz], t2[u][0:96, :sz], ALU.add,
                          r=[f"t1{u}", f"t2{u}"], w=[f"QT{hp}_{a0}"])
                    for kc in range(2):
                        mk.mm(pb_[u][0:64, :sz], wkv[:, kc, h * 64:(h + 1) * 64], akv[:, kc, a0:a0 + sz], kc == 0, kc == 1,
                              r=["wkv", "akv"], w=["pb0"])
                    mk.cp("dve", KT[hp][0:64, a0:a0 + sz], pb_[u][0:64, :sz], r=["pb0"], w=[f"KT{hp}_{a0}"])
                mk.cp("pool", KT[hp][64:96, :], kr[64:96, :], r=["kr"], w=[f"KTr{hp}"])

            units = []
            for h in range(8):
                for qi_, (a0, sz) in enumerate(seq_tiles):
                    nkb = 2 if a0 == 0 else NC_
                    for kb in range(nkb):
                        units.append((h, qi_, a0, sz, kb, nkb))

            def emit_qk(i):
                h, qi_, a0, sz, kb, nkb = units[i]
                hp = h % 2
                v = i % 4
                ktk = [f"KT{hp}_{a0_}" for (a0_, sz_) in seq_tiles] + [f"KTr{hp}"]
                mk.mm(psc[v][:, :sz], KT[hp][0:96, kb * 128:(kb + 1) * 128], QT[hp][0:96, a0:a0 + sz], True, True,
                      r=ktk + [f"QT{hp}_{a0}"], w=[f"psc{v}"])

            LA = 3
            proj(0)
            proj(1)
            for j in range(LA):
                emit_qk(j)
            for i, (h, qi_, a0, sz, kb, nkb) in enumerate(units):
                if qi_ == 1 and kb == 0 and 1 <= h and h + 1 < 8:
                    proj(h + 1)
                v = i % 4
                v3 = i % 4
                u = (h * len(seq_tiles) + qi_) % 2
                if i + LA < len(units):
                    emit_qk(i + LA)
                mk.act(pT[v3][:, :sz], psc[v][:, :sz], AF.Exp, scale=ATT_SCALE, r=[f"psc{v}"], w=[f"pT{v3}"])
                mk.mm(pac[u][:, :sz], VA[:, kb, h, :], pT[v3][:, :sz], kb == 0, kb == nkb - 1,
                      r=vak + [f"pT{v3}"], w=[f"pac{u}"])
                if kb == nkb - 1:
                    mk.recip(rec[u][:, :sz], pac[u][64:128, :sz], r=[f"pac{u}"], w=[f"rec{u}"])
                    po = (h % 2) * 64
                    mk.tt("dve", yb[po:po + 64, h // 2, a0:a0 + sz], pac[u][0:64, :sz], rec[u][:, :sz], ALU.mult,
                          r=[f"pac{u}", f"rec{u}"], w=[f"yb{h}_{a0}"])
            mk.dma("sp", out=YB[:, :, s0:s0 + L].rearrange("j p t -> p j t"), in_=yb[:],
                   r=[f"yb{h}_{a0}" for h in range(8) for (a0, sz) in seq_tiles])
            sg.close()

        def stage_gmlp(l):
            sg = Stage(mk, f"gm{l}")
            gv = [sg.sb(f"gv{i}", [128, 4, 512], BF16) for i in range(2)]
            gu = [sg.sb(f"gu{i}", [128, 4, 512], BF16) for i in range(2)]
            gsq = sg.sb("gsq", [128, 4, 512], F32)
            gss = [sg.sb(f"gss{i}", [128, 16], F32) for i in range(2)]
            gvn = [sg.sb(f"gvn{i}", [128, 4, 4, 128], BF16) for i in range(2)]
            gtmp = [sg.sb(f"gtmp{i}", [128, 4, 4, 128], F32) for i in range(2)]
            vnb = sg.sb("vnb", [128, 512], F32)
            bsb = sg.sb("bsb", [128, 512], F32)
            wsT = sg.sb("wsT", [128, 512], BF16)
            yc = [sg.sb(f"yc{i}", [128, 4, 512], BF16) for i in range(2)]
            pg = [sg.ps(f"pg{i}", [128, 2048], F32) for i in range(2)]
            mk.dma("sp", out=vnb[:], in_=rows[l, 0:1, R_VN:R_VN + 512].to_broadcast([128, 512]), w=["vnb"])
            mk.dma("sp", out=bsb[:], in_=rows[l, 0:1, R_BS:R_BS + 512].to_broadcast([128, 512]), w=["bsb"])
            mk.dma("pool", out=wsT[:], in_=gws_r[l], w=["wsT"])

            TL = [t_ for t_ in TILES if not (l == DEPTH - 1 and t_[2] == 2)]

            def ldg_(i):
                n0_, sz_, vi_, b_ = TL[i]
                p_ = i % 2
                mk.dma("sp", out=gv[p_][:, :sz_ // 128, :], in_=ZTGV[n0_:n0_ + sz_, :].rearrange("(b p) f -> p b f", p=128),
                       w=[f"gv{p_}"])
                mk.dma("sp", out=gu[p_][:, :, :sz_], in_=ZF[C_GU:C_GU + 4, :, n0_:n0_ + sz_].rearrange("j p t -> p j t"),
                       w=[f"gu{p_}"])
            ldg_(0)
            for i, (n0, sz, vi, b) in enumerate(TL):
                p = i % 2
                nb = sz // 128
                if i + 1 < len(TL):
                    ldg_(i + 1)
                GV = gv[p][:, :nb, :]
                mk.tt("dve", gsq[:, :nb, :], GV, GV, ALU.mult, r=[f"gv{p}"], w=["gsq"])
                mk.op("dve", lambda e: e.tensor_reduce(out=gss[p][:, :nb * 4],
                                                       in_=gsq[:, :nb, :].rearrange("p b (g c) -> p (b g) c", g=4),
                                                       axis=AX.X, op=ALU.add), r=["gsq"], w=[f"gss{p}"])
                mk.act(gss[p][:, :nb * 4], gss[p][:, :nb * 4], AF.Sqrt, bias=EPS, scale=1.0 / 128, r=[f"gss{p}"],
                       w=[f"gss{p}"])
                mk.recip(gss[p][:, :nb * 4], gss[p][:, :nb * 4], r=[f"gss{p}"], w=[f"gss{p}"])
                mk.tt("dve", gsq[:, :nb, :], GV, vnb[:].unsqueeze(1).to_broadcast([128, nb, 512]), ALU.mult,
                      r=[f"gv{p}", "vnb"], w=["gsq"])
                mk.tt("pool", gvn[p][:, :nb, :, :].rearrange("p b g c -> p (b g) c"),
                      gsq[:, :nb, :].rearrange("p b (g c) -> p (b g) c", g=4),
                      gss[p][:, :nb * 4].unsqueeze(2).to_broadcast([128, nb * 4, 128]), ALU.mult,
                      r=["gsq", f"gss{p}"], w=[f"gvn{p}"])
                for bb in range(nb):
                    for g in range(4):
                        mk.mm(pg[p][:, bb * 512 + g * 128:bb * 512 + (g + 1) * 128], gvn[p][:, bb, g, :],
                              wsT[:, g * 128:(g + 1) * 128], True, True, r=[f"gvn{p}", "wsT"], w=[f"pg{p}"])
                mk.tt("dve", gtmp[p][:, :nb, :, :].rearrange("p b g t -> p b (g t)"),
                      pg[p][:, :nb * 512].rearrange("p (b f) -> p b f", f=512),
                      bsb[:].unsqueeze(1).to_broadcast([128, nb, 512]), ALU.add, r=[f"pg{p}", "bsb"], w=[f"gtmp{p}"])
                mk.tt("pool", yc[p][:, :, :sz].rearrange("p g (b t) -> p g b t", t=128),
                      gtmp[p][:, :nb, :, :].rearrange("p b g t -> p g b t"),
                      gu[p][:, :, :sz].rearrange("p g (b t) -> p g b t", t=128), ALU.mult,
                      r=[f"gtmp{p}", f"gu{p}"], w=[f"yc{p}"])
                mk.dma("act", out=YC[:, :, n0:n0 + sz].rearrange("j p t -> p j t"), in_=yc[p][:, :, :sz], r=[f"yc{p}"])
            sg.close()

        def stage_out(l):
            sg = Stage(mk, f"o{l}")
            wp = [sg.sb(f"wp{i}", [128, 4, D], BF16) for i in range(3)]
            wo = sg.sb("wo", [128, 8, D], BF16)
            yin = [[sg.sb(f"yin{i}{k}", [128, 4, 512], BF16) for k in range(3)] for i in range(2)]
            gts = [sg.sb(f"gts{i}", [128, 24, 512], BF16) for i in range(2)]
            xt = [sg.sb(f"xt{i}", [128, 8, 512], F32) for i in range(2)]
            ym = [sg.sb(f"ym{i}", [128, 8, 512], BF16) for i in range(2)]
            ta = [sg.sb(f"ta{i}", [128, 512], F32) for i in range(2)]
            tb = [sg.sb(f"tb{i}", [128, 512], F32) for i in range(2)]
            tc_ = [sg.sb(f"tc{i}", [128, 512], F32) for i in range(2)]
            pp = [[sg.ps(f"pp{i}{k}", [128, 512], F32) for k in range(3)] for i in range(2)]
            po = [sg.ps(f"po{i}", [128, 512], F32) for i in range(2)]
            for k, wsrc in enumerate((w_pa, w_pb, w_pc)):
                mk.dma("pool", out=wp[k][:], in_=wsrc[l].rearrange("(kc p) n -> p kc n", p=128), w=[f"wp{k}"])
            mk.dma("pool", out=wo[:], in_=w_out[l].rearrange("(kc p) n -> p kc n", p=128), w=["wo"])
            oi = [0]
            TL = [t_ for t_ in TILES if not (l == DEPTH - 1 and t_[2] == 2)]

            def loads(i):
                n0, sz, vi, b = TL[i]
                p = i % 2
                for k, src in enumerate((YA, YB, YC)):
                    mk.dma("sp", out=yin[p][k][:, :, :sz], in_=src[:, :, n0:n0 + sz].rearrange("j p t -> p j t"),
                           w=[f"yin{p}{k}"])
                mk.dma("sp", out=gts[p][:, :, :sz], in_=ZF[C_BRA:C_BRA + 24, :, n0:n0 + sz].rearrange("j p t -> p j t"),
                       w=[f"gts{p}"])
                mk.dma("sp", out=xt[p][:, :, :sz], in_=XT[:, :, n0:n0 + sz].rearrange("j p t -> p j t"), w=[f"xt{p}"])

            def merge(i):
                n0, sz, vi, b = TL[i]
                p = i % 2
                for oc in range(8):
                    u = oi[0] % 2
                    oi[0] += 1
                    for k in range(3):
                        for kc in range(4):
                            mk.mm(pp[u][k][:, :sz], wp[k][:, kc, oc * 128:(oc + 1) * 128], yin[p][k][:, kc, :sz],
                                  kc == 0, kc == 3, r=[f"wp{k}", f"yin{p}{k}"], w=[f"pp{u}{k}"])
                    mk.tt("dve", ta[u][:, :sz], pp[u][0][:, :sz], gts[p][:, oc, :sz], ALU.mult,
                          r=[f"pp{u}0", f"gts{p}"], w=[f"ta{u}"])
                    mk.tt("dve", tb[u][:, :sz], pp[u][1][:, :sz], gts[p][:, 8 + oc, :sz], ALU.mult,
                          r=[f"pp{u}1", f"gts{p}"], w=[f"tb{u}"])
                    mk.tt("dve", tc_[u][:, :sz], pp[u][2][:, :sz], gts[p][:, 16 + oc, :sz], ALU.mult,
                          r=[f"pp{u}2", f"gts{p}"], w=[f"tc{u}"])
                    mk.tt("pool", ta[u][:, :sz], ta[u][:, :sz], tb[u][:, :sz], ALU.add, r=[f"ta{u}", f"tb{u}"],
                          w=[f"ta{u}"])
                    mk.tt("pool", ym[p][:, oc, :sz], ta[u][:, :sz], tc_[u][:, :sz], ALU.add, r=[f"ta{u}", f"tc{u}"],
                          w=[f"ym{p}{oc}"])

            def outproj(i):
                n0, sz, vi, b = TL[i]
                p = i % 2
                for oc in range(8):
                    u = oc % 2
                    for kc in range(8):
                        mk.mm(po[u][:, :sz], wo[:, kc, oc * 128:(oc + 1) * 128], ym[p][:, kc, :sz], kc == 0, kc == 7,
                              r=["wo", f"ym{p}{kc}"], w=[f"po{u}"])
                    mk.stt(xt[p][:, oc, :sz], po[u][:, :sz], MOD[:, l, 16 + oc, vi:vi + 1], xt[p][:, oc, :sz],
                           ALU.mult, ALU.add, r=[f"po{u}", f"xt{p}"], w=[f"xt{p}"])
                mk.dma("act", out=XT[:, :, n0:n0 + sz].rearrange("j p t -> p j t"), in_=xt[p][:, :, :sz], r=[f"xt{p}"])

            nT = len(TL)
            loads(0)
            merge(0)
            for i in range(nT):
                if i + 1 < nT:
                    loads(i + 1)
                    merge(i + 1)
                outproj(i)
            sg.close()

        def stage_moe(l, b):
            sg = Stage(mk, f"moe{l}{b}")
            NC_ = L // 128
            s0 = b * L
            seq_tiles = ([] if l == DEPTH - 1 else [(0, LC)]) + [(LC + i * 512, 512) for i in range(4)]
            acc = sg.sb("acc", [128, 8, L], F32)
            h2 = sg.sb("h2", [128, 8, L], BF16)
            cmf = sg.sb("cmf", [128, NC_, 16], F32)
            cmb = sg.sb("cmb", [128, NC_, 16], BF16)
            w1 = [sg.sb(f"w1{i}", [128, 8, 512], BF16) for i in range(2)]
            w3 = [sg.sb(f"w3{i}", [128, 8, 512], BF16) for i in range(2)]
            w2 = [sg.sb(f"w2{i}", [128, 4, D], BF16) for i in range(2)]
            cbs = [sg.sb(f"cbs{i}", [128, 512], F32) for i in range(2)]
            s1 = [sg.sb(f"s1{i}", [128, 512], F32) for i in range(2)]
            tm = [sg.sb(f"tm{i}", [128, 512], F32) for i in range(2)]
            hid = [sg.sb(f"hid{i}", [128, 4, 512], BF16) for i in range(2)]
            pcb = sg.ps("pcb", [128, 512], F32)
            p1 = [sg.ps(f"p1{i}", [128, 512], F32) for i in range(2)]
            p3 = [sg.ps(f"p3{i}", [128, 512], F32) for i in range(2)]
            po = [sg.ps(f"po{i}", [128, 512], F32) for i in range(3)]
            for (a0, sz) in seq_tiles:
                mk.dma("sp", out=h2[:, :, a0:a0 + sz], in_=HT[:, :, s0 + a0:s0 + a0 + sz].rearrange("j p t -> p j t"),
                       w=[f"h2_{a0}"])
            mk.dma("sp", out=cmf[:], in_=COMB[s0:s0 + L, :].rearrange("(c p) e -> p c e", p=128), w=["cmf"])
            mk.cp("dve", cmb[:], cmf[:], r=["cmf"], w=["cmb"])
            items = [(e, ti_, a0, sz) for e in range(16) for ti_, (a0, sz) in enumerate(seq_tiles)]

            def load_w(e):
                p = e % 2
                mk.dma("pool", out=w1[p][:], in_=e_w1[l, e].rearrange("(kc p) n -> p kc n", p=128), w=[f"w1{p}"])
                mk.dma("pool", out=w3[p][:], in_=e_w3[l, e].rearrange("(kc p) n -> p kc n", p=128), w=[f"w3{p}"])
                mk.dma("pool", out=w2[p][:], in_=e_w2[l, e].rearrange("(kc p) n -> p kc n", p=128), w=[f"w2{p}"])

            def phase_a(i):
                e, ti_, a0, sz = items[i]
                p = e % 2
                t = i % 2
                nb = sz // 128
                for bb in range(nb):
                    cblk = a0 // 128 + bb
                    mk.mm(pcb[:, bb * 128:(bb + 1) * 128], cmb[:, cblk, e:e + 1].to_broadcast([128, 128]),
                          ident_b[:], True, True, r=["cmb", "ident_b"], w=["pcb"])
                mk.cp("act", cbs[t][:, :sz], pcb[:, :sz], r=["pcb"], w=[f"cbs{t}"])
                for jc in range(4):
                    u = jc % 2
                    for kc in range(8):
                        mk.mm(p1[u][:, :sz], w1[p][:, kc, jc * 128:(jc + 1) * 128], h2[:, kc, a0:a0 + sz],
                              kc == 0, kc == 7, r=[f"w1{p}", f"h2_{a0}"], w=[f"p1{u}"])
                    for kc in range(8):
                        mk.mm(p3[u][:, :sz], w3[p][:, kc, jc * 128:(jc + 1) * 128], h2[:, kc, a0:a0 + sz],
                              kc == 0, kc == 7, r=[f"w3{p}", f"h2_{a0}"], w=[f"p3{u}"])
                    mk.act(s1[u][:, :sz], p1[u][:, :sz], AF.Silu, r=[f"p1{u}"], w=[f"s1{u}"])
                    mk.tt("dve", tm[u][:, :sz], p3[u][:, :sz], s1[u][:, :sz], ALU.mult, r=[f"p3{u}", f"s1{u}"],
                          w=[f"tm{u}"])
                    mk.tt("pool", hid[t][:, jc, :sz], tm[u][:, :sz], cbs[t][:, :sz], ALU.mult,
                          r=[f"tm{u}", f"cbs{t}"], w=[f"hid{t}_{jc}"])

            def phase_b(i):
                e, ti_, a0, sz = items[i]
                p = e % 2
                t = i % 2
                for oc in range(8):
                    u = (i * 8 + oc) % 3
                    for jc in range(4):
                        mk.mm(po[u][:, :sz], w2[p][:, jc, oc * 128:(oc + 1) * 128], hid[t][:, jc, :sz],
                              jc == 0, jc == 3, r=[f"w2{p}", f"hid{t}_{jc}"], w=[f"po{u}"])
                    if e == 0:
                        mk.cp("dve", acc[:, oc, a0:a0 + sz], po[u][:, :sz], r=[f"po{u}"], w=[f"acc{ti_}_{oc}"])
                    else:
                        mk.tt("dve", acc[:, oc, a0:a0 + sz], po[u][:, :sz], acc[:, oc, a0:a0 + sz], ALU.add,
                              r=[f"po{u}", f"acc{ti_}_{oc}"], w=[f"acc{ti_}_{oc}"])

            load_w(0)
            load_w(1)
            nt_ = len(seq_tiles)
            for i in range(len(items) + 1):
                if i < len(items):
                    phase_a(i)
                if i >= 1:
                    phase_b(i - 1)
                    e_prev, ti_prev = items[i - 1][0], items[i - 1][1]
                    if ti_prev == nt_ - 1 and e_prev + 2 < 16:
                        load_w(e_prev + 2)
            xtv = h2[:].rearrange("p j t -> p (j t)").bitcast(F32)
            h2keys = [f"h2_{a0_}" for (a0_, sz_) in seq_tiles]

            def xt_(i):
                return xtv[:, (i % 2) * 4096:(i % 2 + 1) * 4096].rearrange("p (j t) -> p j t", j=8)

            def ldx_(i):
                a0_, sz_ = seq_tiles[i]
                mk.dma("sp", out=xt_(i)[:, :, :sz_], in_=XT[:, :, s0 + a0_:s0 + a0_ + sz_].rearrange("j p t -> p j t"),
                       w=[f"xr{i % 2}"] + (h2keys if i < 2 else []))
            ldx_(0)
            for ti_, (a0, sz) in enumerate(seq_tiles):
                vi = 2 if a0 == 0 else b
                if ti_ + 1 < len(seq_tiles):
                    ldx_(ti_ + 1)
                xk = f"xr{ti_ % 2}"
                for oc in range(8):
                    mk.stt(xt_(ti_)[:, oc, :sz], acc[:, oc, a0:a0 + sz], MOD[:, l, 40 + oc, vi:vi + 1], xt_(ti_)[:, oc, :sz],
                           ALU.mult, ALU.add, r=[f"acc{ti_}_{oc}", xk], w=[xk])
                mk.dma("act", out=XT[:, :, s0 + a0:s0 + a0 + sz].rearrange("j p t -> p j t"), in_=xt_(ti_)[:, :, :sz],
                       r=[xk])
            sg.close()

        def stage_final():
            sg = Stage(mk, "fin")
            xt = [sg.sb(f"xt{i}", [128, 8, 512], F32) for i in range(2)]
            sq = [sg.sb(f"sq{i}", [128, 8, 512], BF16) for i in range(2)]
            rt = [sg.sb(f"rt{i}", [128, 512], F32) for i in range(2)]
            ot = [sg.sb(f"ot{i}", [128, D], F32) for i in range(2)]
            pss = [sg.ps(f"pss{i}", [128, 512], F32) for i in range(2)]
            pt = [[sg.ps(f"pt{i}{h}", [128, 512], F32) for h in range(2)] for i in range(2)]
            bi = 0
            lat = [i for i, tl_ in enumerate(TILES) if tl_[2] != 2]

            def ldf_(li_):
                n0_, sz_, vi_, b_ = TILES[lat[li_]]
                mk.dma("sp", out=xt[li_ % 2][:, :, :sz_], in_=XT[:, :, n0_:n0_ + sz_].rearrange("j p t -> p j t"),
                       w=[f"xt{li_ % 2}"])
            ldf_(0)
            for li, i in enumerate(lat):
                n0, sz, vi, b = TILES[i]
                p = li % 2
                if li + 1 < len(lat):
                    ldf_(li + 1)
                mk.act(sq[p][:, :, :sz], xt[p][:, :, :sz], AF.Square, r=[f"xt{p}"], w=[f"sq{p}"])
                for j in range(8):
                    mk.mm(pss[p][:, :sz], ones_b[:], sq[p][:, j, :sz], j == 0, j == 7, r=[f"sq{p}"], w=[f"pss{p}"])
                mk.act(rt[p][:, :sz], pss[p][:, :sz], AF.Sqrt, bias=EPS, scale=1.0 / D, r=[f"pss{p}"], w=[f"rt{p}"])
                mk.recip(rt[p][:, :sz], rt[p][:, :sz], r=[f"rt{p}"], w=[f"rt{p}"])
                for j in range(8):
                    mk.stt(xt[p][:, j, :sz], xt[p][:, j, :sz], VEC[:, 0, V_FN + j:V_FN + j + 1], rt[p][:, :sz],
                           ALU.mult, ALU.mult, r=[f"xt{p}", f"rt{p}"], w=[f"xt{p}"])
                for bb in range(sz // 128):
                    q = bi % 2
                    bi += 1
                    for h in range(2):
                        for jj in range(4):
                            j = h * 4 + jj
                            mk.tr(pt[q][h][:, jj * 128:(jj + 1) * 128], xt[p][:, j, bb * 128:(bb + 1) * 128], ident_f[:],
                                  r=[f"xt{p}"], w=[f"pt{q}{h}"])
                        mk.cp("dve" if h == 0 else "act", ot[q][:, h * 512:(h + 1) * 512], pt[q][h][:],
                              r=[f"pt{q}{h}"], w=[f"ot{q}{h}"])
                    tpos = n0 - b * L - LC + bb * 128
                    mk.dma("sp", out=out[b, tpos:tpos + 128, :], in_=ot[q][:], r=[f"ot{q}0", f"ot{q}1"])
            sg.close()

        def program():
            stage_load_x()
            stage_ada()
            if check_stop(-1, "ada"):
                return
            for l in range(nlayers):
                stage_norm(l, 0, False)
                if check_stop(l, "norm1"):
                    return
                stage_z(l)
                if check_stop(l, "z"):
                    return
                for b in range(NB):
                    stage_mlstm(l, b)
                if check_stop(l, "mlstm"):
                    return
                for b in range(NB):
                    stage_mla(l, b)
                if check_stop(l, "mla"):
                    return
                stage_gmlp(l)
                if check_stop(l, "gmlp"):
                    return
                stage_out(l)
                if check_stop(l, "out"):
                    return
                stage_norm(l, 1, True)
                if check_stop(l, "norm2"):
                    return
                for b in range(NB):
                    stage_moe(l, b)
                if check_stop(l, "moe"):
                    return
            stage_final()

        program()
        mk.finish()
        build.last_ninst = mk.ninst
    return nc


def _fm(v, nchunks):
    return np.ascontiguousarray(v.reshape(nchunks, 128).T)


def prep_shared(inp):
    f32 = np.float32
    vecs = np.zeros((DEPTH, 128, NV), f32)
    rows = np.zeros((DEPTH, 1, NR), f32)
    for l in range(DEPTH):
        vecs[l, :, V_N1:V_N1 + 8] = _fm(inp["norm1"][l], 8)
        vecs[l, :, V_N2:V_N2 + 8] = _fm(inp["norm2"][l], 8)
        vecs[l, :, V_BADA:V_BADA + 48] = _fm(inp["b_ada"][l], 48)
        cw = inp["m_conv"][l]
        for tap in range(3):
            vecs[l, :, V_CONV + tap:V_CONV + 24:3] = _fm(cw[tap], 8)
        vecs[l, :, V_MNORM:V_MNORM + 4] = _fm(inp["m_norm"][l], 4)
        vecs[l, :, V_QN:V_QN + 3] = _fm(inp["a_qnorm"][l], 3)
        vecs[l, :, V_KVN:V_KVN + 2] = _fm(inp["a_kvnorm"][l], 2)
        vecs[l, :, V_FN:V_FN + 8] = _fm(inp["final_norm"], 8)
        rows[l, 0, R_GB:R_GB + 16] = inp["m_gate_b"][l]
        rows[l, 0, R_VN:R_VN + 512] = inp["g_vnorm"][l]
        rows[l, 0, R_RB:R_RB + 4] = inp["r_group_b"][l]
        rows[l, 0, R_RB + 4:R_RB + 20] = inp["r_expert_b"][l]
        rows[l, 0, R_BS:R_BS + 512] = inp["g_bs"][l].reshape(512)
    off = np.cumsum([0, 512, 512, 512, 512, 16, 384, 256, 32, 512, 512, 1024, 1024, 1024])
    o_mq, o_mk, o_mv, o_mo, o_mg, o_aq, o_akv, o_akr, o_gu, o_gv, o_bra, o_brb, o_brc = off[:13]
    ar = np.arange
    akr = o_akr + ar(32)
    akr_sw = o_akr + np.concatenate([ar(16, 32), ar(0, 16)])
    padA = np.concatenate([np.tile(akr, 2), akr, akr])
    padB = np.concatenate([np.tile(akr, 2), akr_sw, akr])
    idx = np.concatenate([o_mq + ar(512), o_mk + ar(512), o_mo + ar(512), o_aq + ar(384), o_akv + ar(256), padA, padB,
                          o_gu + ar(512), o_bra + ar(1024), o_brb + ar(1024), o_brc + ar(1024),
                          o_mv + ar(512), o_mg + ar(16), o_gv + ar(512)])
    assert idx.shape[0] == NFM * 128 + NTMC
    w_in_r = np.ascontiguousarray(inp["w_in"][:, :, idx])
    sw = np.concatenate([h * 96 + np.concatenate([ar(64), 64 + ar(16, 32), 64 + ar(0, 16)]) for h in range(8)])
    wuq_r = np.ascontiguousarray(np.concatenate([inp["a_wuq"], inp["a_wuq"][:, :, sw]], axis=2))
    kidx = np.concatenate([h * 128 + ar(64) for h in range(8)])
    vidx = np.concatenate([h * 128 + 64 + ar(64) for h in range(8)])
    wukv_r = np.ascontiguousarray(inp["a_wukv"][:, :, np.concatenate([kidx, vidx])])
    gws_r = np.ascontiguousarray(inp["g_ws"].transpose(0, 3, 1, 2).reshape(DEPTH, 128, 512))
    rwc = np.ascontiguousarray(np.concatenate([inp["r_group"], inp["r_expert"]], axis=2))
    s_ = np.arange(128)
    ident = np.eye(128, dtype=f32)
    trif = (s_[:, None] <= s_[None, :]).astype(f32)
    trib = (s_[:, None] >= s_[None, :]).astype(f32)
    half = 16
    r_ = np.repeat(np.arange(T // 64, dtype=f32), 64)
    col = np.tile(np.arange(64, dtype=f32), T // 64)
    inv = (np.float32(10000.0) ** (-np.arange(0, half, 2, dtype=f32) / np.float32(half))).astype(f32)
    ang = np.concatenate([r_[:, None] * inv, col[:, None] * inv], axis=-1).astype(f32)
    cos, sin = np.cos(ang).astype(f32), np.sin(ang).astype(f32)
    cc = np.ones((128, L), f32)
    ss = np.zeros((128, L), f32)
    cc[64:80, LC:] = cos.T
    cc[80:96, LC:] = cos.T
    ss[64:80, LC:] = -sin.T
    ss[80:96, LC:] = sin.T
    sh = dict(vecs=vecs, rows=rows, w_ada=np.ascontiguousarray(inp["w_ada"]), w_in_r=w_in_r, wuq_r=wuq_r, wukv_r=wukv_r,
              gws_r=gws_r, w_pa=np.ascontiguousarray(inp["w_pa"]), w_pb=np.ascontiguousarray(inp["w_pb"]),
              w_pc=np.ascontiguousarray(inp["w_pc"]), w_out=np.ascontiguousarray(inp["w_out"]), rw=rwc,
              e_w1=np.ascontiguousarray(inp["e_w1"]), e_w3=np.ascontiguousarray(inp["e_w3"]),
              e_w2=np.ascontiguousarray(inp["e_w2"]), c_ident=ident, c_trif=trif, c_trib=trib,
              c_maskf=(trif * np.float32(QS)).astype(f32), c_maskb=(trib * np.float32(QS)).astype(f32), c_cc=cc, c_ss=ss)
    return sh


def prep_core(inp, core):
    f32 = np.float32
    bs = [core * NB + i for i in range(NB)]
    xin = np.concatenate([np.concatenate([inp["ctx"][b], inp["x"][b]], axis=0) for b in bs], axis=0).astype(f32)
    vs = [inp["c"][bs[0]], inp["c"][bs[1]], inp["c_ctx"], inp["c_ctx"]]
    cv = np.stack([_fm(v, 8) for v in vs], axis=-1).astype(f32)
    return dict(xin=np.ascontiguousarray(xin), cv=np.ascontiguousarray(cv))


def kernel(**inputs):
    inp = {k: np.asarray(v) for k, v in inputs.items()}
    sh = prep_shared(inp)
    nc = build()
    in_maps = []
    for core in range(NCORES):
        m = dict(sh)
        m.update(prep_core(inp, core))
        in_maps.append(m)
    res = run_bass_kernel_spmd(nc, in_maps, core_ids=list(range(NCORES)))
    outs = [np.asarray(r["out"]) for r in res.results]
    return np.concatenate(outs, axis=0).astype(np.float32)
```
